# Optimizing a Trainium2 kernel written in Bass

```python
import jax
import jax.numpy as jnp
from jax import lax
import numpy as np

D_MODEL = 1024
BATCH = 2
SEQ = 16384
DEPTH = 2

GRID_W = 64
CTX_LEN = 256
HEAD_DIM = 64
ROPE_THETA = 10000.0
RMS_EPS = 1e-6
A_Q_HEADS = 8
A_KV_HEADS = 2
B_Q_HEADS = 8
B_KV_HEADS = 2
WINDOW = 128
Q_BLOCK = 128
ATTN_IN_DIM = (A_Q_HEADS + 2 * A_KV_HEADS + B_Q_HEADS + 2 * B_KV_HEADS) * HEAD_DIM
ATTN_OUT_DIM = (A_Q_HEADS + B_Q_HEADS) * HEAD_DIM
RWKV_HEADS = D_MODEL // HEAD_DIM
DECAY_LORA = 64
ICLR_LORA = 64
GATE_LORA = 160
GN_EPS = 64e-5
N_EXPERTS = 16
N_GROUPS = 4
EXPERTS_PER_GROUP = N_EXPERTS // N_GROUPS
TOP_K = 2
D_EXPERT = 1024
MOE_BLOCK = 256
N_ATTN_LAYERS = (DEPTH + 1) // 2
N_RWKV_LAYERS = DEPTH // 2

kernel_name = 'hybrid_dit_swa_axial_rwkv7_groupmoe'


def rmsnorm(x, g):
    xf = x.astype(jnp.float32)
    y = xf * lax.rsqrt(jnp.mean(xf * xf, axis=-1, keepdims=True) + RMS_EPS)
    return (y * g.astype(jnp.float32)).astype(x.dtype)


def modulate(h, shift, scale):
    return h * (1 + scale) + shift


def axial_rope_tables(n_tokens):
    rows = n_tokens // GRID_W
    row = jnp.repeat(jnp.arange(rows, dtype=jnp.float32), GRID_W)
    col = (jnp.arange(rows * GRID_W) % GRID_W).astype(jnp.float32)
    n_freq = HEAD_DIM // 4
    inv = ROPE_THETA ** (-jnp.arange(n_freq, dtype=jnp.float32) / n_freq)
    ang = jnp.stack([row, col], axis=-1)[..., None] * inv
    ang = ang[:, None, :, None, :]
    return jnp.cos(ang), jnp.sin(ang)


def apply_rope(x, cos, sin):
    xs = x.reshape(*x.shape[:-1], 2, 2, HEAD_DIM // 4).astype(jnp.float32)
    rot = jnp.stack([-xs[..., 1, :], xs[..., 0, :]], axis=-2)
    return (xs * cos + rot * sin).reshape(x.shape).astype(x.dtype)


def attend(q, k, v, sink=None, mask=None):
    s = jnp.einsum('bqhgd,bshd->bhgqs', q, k, preferred_element_type=jnp.float32)
    if mask is not None:
        s = jnp.where(mask, s, -jnp.inf)
    if sink is not None:
        sk = jnp.broadcast_to(sink.astype(jnp.float32)[None, :, :, None, None], s.shape[:-1] + (1,))
        p = jax.nn.softmax(jnp.concatenate([sk, s], axis=-1), axis=-1)[..., 1:]
    else:
        p = jax.nn.softmax(s, axis=-1)
    return jnp.einsum('bhgqs,bshd->bqhgd', p.astype(v.dtype), v)


def window_attention(q, k, v, kc, vc, sink):
    B, S = q.shape[:2]
    L = kc.shape[1]
    nb = S // Q_BLOCK
    span = Q_BLOCK + 2 * WINDOW
    pad = ((0, 0), (WINDOW, WINDOW), (0, 0), (0, 0))
    kp, vp = jnp.pad(k, pad), jnp.pad(v, pad)
    qb = jnp.swapaxes(q.reshape(B, nb, Q_BLOCK, *q.shape[2:]), 0, 1)
    ctx_ok = jnp.ones((Q_BLOCK, L), dtype=bool)

    def one_block(args):
        q_blk, i = args
        start = i * Q_BLOCK
        qpos = start + jnp.arange(Q_BLOCK)
        kpos = start - WINDOW + jnp.arange(span)
        band = (jnp.abs(qpos[:, None] - kpos[None, :]) <= WINDOW) & (kpos >= 0)[None, :] & (kpos < S)[None, :]
        k_blk = jnp.concatenate([kc, lax.dynamic_slice_in_dim(kp, start, span, axis=1)], axis=1)
        v_blk = jnp.concatenate([vc, lax.dynamic_slice_in_dim(vp, start, span, axis=1)], axis=1)
        return attend(q_blk, k_blk, v_blk, sink=sink, mask=jnp.concatenate([ctx_ok, band], axis=1))

    o = lax.map(one_block, (qb, jnp.arange(nb)))
    return jnp.swapaxes(o, 0, 1).reshape(q.shape)


def block_attention(q, k, v):
    B, S = q.shape[:2]
    nb = S // Q_BLOCK
    qb = jnp.swapaxes(q.reshape(B, nb, Q_BLOCK, *q.shape[2:]), 0, 1)
    o = lax.map(lambda q_blk: attend(q_blk, k, v), qb)
    return jnp.swapaxes(o, 0, 1).reshape(q.shape)


def attn_mixer(hc, hl, w_in, w_out, sink, q_norm_g, k_norm_g, cos, sin, with_ctx):
    B, S, _ = hl.shape
    qscale = HEAD_DIM ** -0.5
    sizes = [A_Q_HEADS, A_KV_HEADS, A_KV_HEADS, B_Q_HEADS, B_KV_HEADS, B_KV_HEADS]
    cuts = [int(n) * HEAD_DIM for n in np.cumsum(sizes)[:-1]]

    def project(h):
        T = h.shape[1]
        parts = jnp.split(h @ w_in, cuts, axis=-1)
        qa, ka, va, qb, kb, vb = [p.reshape(B, T, -1, HEAD_DIM) for p in parts]
        return qa, ka, va, rmsnorm(qb, q_norm_g), rmsnorm(kb, k_norm_g), vb

    def group(q, n_kv):
        return (q * qscale).reshape(q.shape[0], q.shape[1], n_kv, -1, HEAD_DIM)

    sink_g = sink.reshape(A_KV_HEADS, -1)
    cqa, cka, cva, cqb, ckb, cvb = project(hc)
    qa, ka, va, qb, kb, vb = project(hl)
    qa, ka, qb, kb = (apply_rope(t, cos, sin) for t in (qa, ka, qb, kb))
    oa = window_attention(group(qa, A_KV_HEADS), ka, va, cka, cva, sink_g)
    ob = block_attention(group(qb, B_KV_HEADS), jnp.concatenate([ckb, kb], axis=1), jnp.concatenate([cvb, vb], axis=1))
    yl = jnp.concatenate([oa.reshape(B, S, -1), ob.reshape(B, S, -1)], axis=-1) @ w_out
    yc = None
    if with_ctx:
        L = hc.shape[1]
        oca = attend(group(cqa, A_KV_HEADS), cka, cva, sink=sink_g)
        ocb = attend(group(cqb, B_KV_HEADS), ckb, cvb)
        yc = jnp.concatenate([oca.reshape(B, L, -1), ocb.reshape(B, L, -1)], axis=-1) @ w_out
    return yc, yl


def centred_shift(h):
    hp = jnp.pad(h, ((0, 0), (1, 1), (0, 0)))
    return 0.5 * (hp[:, :-2] + hp[:, 2:])


def wkv_scan(state, r, decay, k, v, a, b, reverse):
    def step(s, inp):
        r_t, w_t, k_t, v_t, a_t, b_t = inp
        sa = jnp.einsum('bhvk,bhk->bhv', s, a_t)
        s = s * w_t[:, :, None, :] + sa[..., None] * b_t[:, :, None, :] + v_t[..., None] * k_t[:, :, None, :]
        return s, jnp.einsum('bhvk,bhk->bhv', s, r_t)
    xs = tuple(jnp.swapaxes(t, 0, 1) for t in (r, decay, k, v, a, b))
    state, y = lax.scan(step, state, xs, reverse=reverse)
    return state, jnp.swapaxes(y, 0, 1)


def rwkv_mixer(hc, hl, x_mix, w_r, w_k, w_v, w_o, w0, w1, w2, a0, a1, a2, g1, g2, k_k, k_a, r_k, ln_g, ln_b, with_ctx):
    B, L, D = hc.shape
    f32 = jnp.float32
    h = jnp.concatenate([hc, hl], axis=1)
    T = h.shape[1]
    xx = jnp.concatenate([centred_shift(hc), centred_shift(hl)], axis=1) - h
    mix = lambda j: h + xx * x_mix[j]
    heads = lambda t: t.reshape(*t.shape[:-1], RWKV_HEADS, HEAD_DIM).astype(f32)
    r = heads(mix(0) @ w_r)
    k = heads(mix(2) @ w_k)
    v = heads(mix(3) @ w_v)
    g = jax.nn.sigmoid(mix(5) @ g1) @ g2
    kk = k * heads(k_k)
    kk = kk * lax.rsqrt(jnp.maximum(jnp.sum(kk * kk, axis=-1, keepdims=True), 1e-24))
    y = jnp.zeros_like(v)
    bonus = jnp.zeros(v.shape[:-1] + (1,), f32)
    for d in range(2):
        reverse = d == 1
        w_log = -jax.nn.softplus(-heads(w0[d] + jnp.tanh(mix(1) @ w1[d]) @ w2[d])) - 0.5
        decay = jnp.exp(-jnp.exp(w_log))
        iclr = jax.nn.sigmoid(heads(a0[d] + (mix(4) @ a1[d]) @ a2[d]))
        k_d = k * (1.0 + (iclr - 1.0) * heads(k_a))
        ins = (r, decay, k_d, v, -kk, kk * iclr)
        zero = jnp.zeros((B, RWKV_HEADS, HEAD_DIM, HEAD_DIM), f32)
        s_ctx, y_c = wkv_scan(zero, *(t[:, :L] for t in ins), reverse=reverse)
        _, y_l = wkv_scan(s_ctx, *(t[:, L:] for t in ins), reverse=reverse)
        y = y + jnp.concatenate([y_c, y_l], axis=1)
        bonus = bonus + jnp.sum(r * k_d * r_k.astype(f32), axis=-1, keepdims=True)
    mu = jnp.mean(y, axis=-1, keepdims=True)
    yn = (y - mu) * lax.rsqrt(jnp.mean(jnp.square(y - mu), axis=-1, keepdims=True) + GN_EPS)
    o = yn.reshape(B, T, D) * ln_g.astype(f32) + ln_b.astype(f32) + (bonus * v).reshape(B, T, D)
    o = o.astype(h.dtype) * g
    yl = o[:, L:] @ w_o
    yc = o[:, :L] @ w_o if with_ctx else None
    return yc, yl


def route(h, router_w, router_bias):
    n = h.shape[0]
    probs = jax.nn.softmax((h @ router_w).astype(jnp.float32), axis=-1)
    sel = (probs + router_bias.astype(jnp.float32)).reshape(n, N_GROUPS, EXPERTS_PER_GROUP)
    group_score = jnp.sum(lax.top_k(sel, TOP_K)[0], axis=-1)
    g_idx = jnp.argmax(group_score, axis=-1)
    in_group = jnp.take_along_axis(sel, g_idx[:, None, None], axis=1)[:, 0]
    local = lax.top_k(in_group, TOP_K)[1]
    expert_idx = g_idx[:, None] * EXPERTS_PER_GROUP + local
    w = jnp.take_along_axis(probs, expert_idx, axis=1)
    return expert_idx, w / jnp.sum(w, axis=-1, keepdims=True)


def moe_ffn(h, expert_idx, gate_w, w1, w3, w2):
    N, D = h.shape
    NK = N * TOP_K
    flat_e = expert_idx.reshape(NK)
    flat_tok = jnp.repeat(jnp.arange(N, dtype=jnp.int32), TOP_K)
    flat_w = gate_w.reshape(NK)
    order = jnp.argsort(flat_e)
    e_sorted = flat_e[order]
    counts = jnp.bincount(flat_e, length=N_EXPERTS)
    padded = (counts + MOE_BLOCK - 1) // MOE_BLOCK * MOE_BLOCK
    pad_end = jnp.cumsum(padded)
    pad_start = pad_end - padded
    start = jnp.cumsum(counts) - counts
    dest = pad_start[e_sorted] + jnp.arange(NK) - start[e_sorted]
    n_blocks = -(-NK // MOE_BLOCK) + N_EXPERTS
    n_slots = n_blocks * MOE_BLOCK
    slot_tok = jnp.full((n_slots,), N, dtype=jnp.int32).at[dest].set(flat_tok[order])
    slot_w = jnp.zeros((n_slots,), gate_w.dtype).at[dest].set(flat_w[order])
    block_expert = jnp.minimum(jnp.searchsorted(pad_end, jnp.arange(n_blocks) * MOE_BLOCK, side='right'), N_EXPERTS - 1)
    h_pad = jnp.concatenate([h, jnp.zeros((1, D), h.dtype)], axis=0)
    xb = h_pad[slot_tok].reshape(n_blocks, MOE_BLOCK, D)

    def expert_block(args):
        x_blk, e = args
        return (jax.nn.silu(x_blk @ w1[e]) * (x_blk @ w3[e])) @ w2[e]

    yb = lax.map(expert_block, (xb, block_expert)).reshape(n_slots, D)
    out = jnp.zeros((N + 1, D), h.dtype).at[slot_tok].add(yb * slot_w[:, None].astype(h.dtype))
    return out[:N]


def setup_inputs(seed: int = 0) -> dict:
    key = jax.random.key(seed)
    ks = iter(jax.random.split(key, 64))
    nrm = lambda shape, scale: jax.random.normal(next(ks), shape, jnp.float32) * scale
    D = D_MODEL
    NA, NR = N_ATTN_LAYERS, N_RWKV_LAYERS
    H, N = RWKV_HEADS, HEAD_DIM
    E, F = N_EXPERTS, D_EXPERT
    return {
        'x': nrm((BATCH, SEQ, D), 1.0),
        'c': nrm((BATCH, D), 1.0),
        'ctx': nrm((BATCH, CTX_LEN, D), 1.0),
        'c_ctx': nrm((D,), 1.0),
        'ada_w': nrm((DEPTH, D, 6 * D), 0.5 * D ** -0.5),
        'ada_b': nrm((DEPTH, 6 * D), 0.02),
        'norm_mix_g': 1.0 + nrm((DEPTH, D), 0.02),
        'norm_ffn_g': 1.0 + nrm((DEPTH, D), 0.02),
        'attn_w_in': nrm((NA, D, ATTN_IN_DIM), D ** -0.5),
        'attn_w_out': nrm((NA, ATTN_OUT_DIM, D), ATTN_OUT_DIM ** -0.5),
        'attn_sink': nrm((NA, A_Q_HEADS), 1.0),
        'attn_q_norm_g': 1.0 + nrm((NA, HEAD_DIM), 0.02),
        'attn_k_norm_g': 1.0 + nrm((NA, HEAD_DIM), 0.02),
        'rwkv_x_mix': jax.random.uniform(next(ks), (NR, 6, D), jnp.float32),
        'rwkv_w_r': nrm((NR, D, D), D ** -0.5),
        'rwkv_w_k': nrm((NR, D, D), D ** -0.5),
        'rwkv_w_v': nrm((NR, D, D), D ** -0.5),
        'rwkv_w_o': nrm((NR, D, D), D ** -0.5),
        'rwkv_decay_w0': jax.random.uniform(next(ks), (NR, 2, D), jnp.float32, minval=-6.0, maxval=-1.0),
        'rwkv_decay_w1': nrm((NR, 2, D, DECAY_LORA), 0.5 * D ** -0.5),
        'rwkv_decay_w2': nrm((NR, 2, DECAY_LORA, D), 0.5 * DECAY_LORA ** -0.5),
        'rwkv_iclr_a0': nrm((NR, 2, D), 0.1),
        'rwkv_iclr_a1': nrm((NR, 2, D, ICLR_LORA), 0.5 * D ** -0.5),
        'rwkv_iclr_a2': nrm((NR, 2, ICLR_LORA, D), 0.5 * ICLR_LORA ** -0.5),
        'rwkv_gate_g1': nrm((NR, D, GATE_LORA), D ** -0.5),
        'rwkv_gate_g2': nrm((NR, GATE_LORA, D), GATE_LORA ** -0.5),
        'rwkv_k_k': 0.85 + nrm((NR, D), 0.02),
        'rwkv_k_a': 1.0 + nrm((NR, D), 0.02),
        'rwkv_r_k': nrm((NR, H, N), 0.1),
        'rwkv_ln_g': 1.0 + nrm((NR, D), 0.02),
        'rwkv_ln_b': nrm((NR, D), 0.02),
        'router_w': nrm((D, E), D ** -0.5),
        'router_bias': nrm((E,), 0.01),
        'moe_w1': nrm((DEPTH, E, D, F), D ** -0.5),
        'moe_w3': nrm((DEPTH, E, D, F), D ** -0.5),
        'moe_w2': nrm((DEPTH, E, F, D), F ** -0.5),
        'final_norm_g': 1.0 + nrm((D,), 0.02),
    }


def reference(x, c, ctx, c_ctx, ada_w, ada_b, norm_mix_g, norm_ffn_g, attn_w_in, attn_w_out, attn_sink,
              attn_q_norm_g, attn_k_norm_g, rwkv_x_mix, rwkv_w_r, rwkv_w_k, rwkv_w_v, rwkv_w_o,
              rwkv_decay_w0, rwkv_decay_w1, rwkv_decay_w2, rwkv_iclr_a0, rwkv_iclr_a1, rwkv_iclr_a2,
              rwkv_gate_g1, rwkv_gate_g2, rwkv_k_k, rwkv_k_a, rwkv_r_k, rwkv_ln_g, rwkv_ln_b,
              router_w, router_bias, moe_w1, moe_w3, moe_w2, final_norm_g):
    B, S, D = x.shape
    L = ctx.shape[1]
    cos, sin = axial_rope_tables(S)
    xl, xc = x, ctx
    for i in range(DEPTH):
        with_ctx = i < DEPTH - 1
        j = i // 2
        ml = jnp.moveaxis((jax.nn.silu(c) @ ada_w[i] + ada_b[i]).reshape(B, 6, D), 1, 0)[:, :, None, :]
        mc = (jax.nn.silu(c_ctx) @ ada_w[i] + ada_b[i]).reshape(6, D)
        hl = modulate(rmsnorm(xl, norm_mix_g[i]), ml[0], ml[1])
        hc = modulate(rmsnorm(xc, norm_mix_g[i]), mc[0], mc[1])
        if i % 2 == 0:
            yc, yl = attn_mixer(hc, hl, attn_w_in[j], attn_w_out[j], attn_sink[j], attn_q_norm_g[j],
                                attn_k_norm_g[j], cos, sin, with_ctx)
        else:
            yc, yl = rwkv_mixer(hc, hl, rwkv_x_mix[j], rwkv_w_r[j], rwkv_w_k[j], rwkv_w_v[j], rwkv_w_o[j],
                                rwkv_decay_w0[j], rwkv_decay_w1[j], rwkv_decay_w2[j],
                                rwkv_iclr_a0[j], rwkv_iclr_a1[j], rwkv_iclr_a2[j],
                                rwkv_gate_g1[j], rwkv_gate_g2[j], rwkv_k_k[j], rwkv_k_a[j], rwkv_r_k[j],
                                rwkv_ln_g[j], rwkv_ln_b[j], with_ctx)
        xl = xl + ml[2] * yl
        hl = modulate(rmsnorm(xl, norm_ffn_g[i]), ml[3], ml[4]).reshape(B * S, D)
        if with_ctx:
            xc = xc + mc[2] * yc
            hc = modulate(rmsnorm(xc, norm_ffn_g[i]), mc[3], mc[4]).reshape(B * L, D)
            tokens = jnp.concatenate([hc, hl], axis=0)
        else:
            tokens = hl
        expert_idx, gate_w = route(tokens, router_w, router_bias)
        f = moe_ffn(tokens, expert_idx, gate_w, moe_w1[i], moe_w3[i], moe_w2[i])
        if with_ctx:
            xc = xc + mc[5] * f[:B * L].reshape(B, L, D)
            f = f[B * L:]
        xl = xl + ml[5] * f.reshape(B, S, D)
    return rmsnorm(xl, final_norm_g)
```

```python
import numpy as np
from contextlib import ExitStack
import concourse.bass as bass
import concourse.mybir as mybir
from concourse.bass_utils import run_bass_kernel_spmd


F32 = mybir.dt.float32
BF16 = mybir.dt.bfloat16
I32 = mybir.dt.int32
U32 = mybir.dt.uint32
AF = mybir.ActivationFunctionType
ALU = mybir.AluOpType
AX = mybir.AxisListType

EPOCH = 20000
NDMA = 6


class Prog:
    ENG = ('pe', 'act', 'dve', 'pool', 'sp')

    def __init__(self, nc):
        self.nc = nc
        self.streams = {e: [] for e in self.ENG}
        self.cnt = {e: 0 for e in self.ENG}
        self.last_w = {}
        self.readers = {}
        self.known = {e: {} for e in self.ENG}
        self.dma_rr = {e: 0 for e in self.ENG}
        self.dma_cnt = {}
        self.extra = {e: [] for e in self.ENG}
        self.nops = 0
        self.psum_keys = set()

    def _deps(self, reads, writes):
        deps = []
        for k in reads:
            w = self.last_w.get(k)
            if w is not None:
                deps.append(w)
        for k in writes:
            w = self.last_w.get(k)
            if w is not None:
                deps.append(w)
            deps.extend(self.readers.get(k, ()))
        return deps

    def _resolve(self, eng, deps):
        need = {}
        for t in deps:
            if t[0] == 'c':
                if t[1] == 'pe' and eng == 'pe':
                    continue
                key = ('c', t[1])
                val = t[2]
            else:
                key = ('d', t[1], t[2])
                val = t[3]
            if val > need.get(key, -1):
                need[key] = val
        out = []
        kn = self.known[eng]
        for key, val in need.items():
            if kn.get(key, -1) >= val:
                continue
            kn[key] = val
            out.append((key, val))
        return out

    def _record(self, tok, reads, writes):
        for k in reads:
            self.readers.setdefault(k, []).append(tok)
        for k in writes:
            self.last_w[k] = tok
            self.readers[k] = []

    def op(self, eng, fn, reads=(), writes=()):
        pr = [k for k in reads if k in self.psum_keys]
        if pr:
            writes = list(writes) + pr
        deps = self._deps(reads, writes) + self.extra[eng]
        self.extra[eng] = []
        waits = self._resolve(eng, deps)
        idx = self.cnt[eng]
        self.cnt[eng] += 1
        tok = ('c', eng, idx)
        self.streams[eng].append((waits, fn, tok))
        self._record(tok, reads, writes)
        self.nops += 1
        return tok

    def dma(self, q, fn, reads=(), writes=()):
        k = self.dma_rr[q]
        self.dma_rr[q] = (k + 1) % NDMA
        m = self.dma_cnt.get((q, k), 0)
        deps = self._deps(reads, writes) + self.extra[q]
        self.extra[q] = []
        if m > 0:
            deps.append(('d', q, k, m - 1))
        waits = self._resolve(q, deps)
        self.dma_cnt[(q, k)] = m + 1
        tok = ('d', q, k, m)
        self.streams[q].append((waits, fn, tok))
        self._record(tok, reads, writes)
        self.nops += 1
        return tok

    def barrier(self):
        snap = []
        for e in self.ENG:
            if self.cnt[e] > 0:
                snap.append(('c', e, self.cnt[e] - 1))
        for (q, k), m in self.dma_cnt.items():
            snap.append(('d', q, k, m - 1))
        for e in self.ENG:
            self.extra[e] = self.extra[e] + snap

    def final_wait(self, eng='sp'):
        self.barrier()
        waits = self._resolve(eng, self.extra[eng])
        self.extra[eng] = []
        self.streams[eng].append((waits, None, None))

    def emit(self, stack):
        nc = self.nc
        csem = {}
        for e in self.ENG:
            ne = (self.cnt[e] + EPOCH - 1) // EPOCH
            for j in range(ne):
                csem[(e, j)] = stack.enter_context(nc.semaphore(f"c_{e}_{j}"))
        dsem = {}
        for (q, k) in self.dma_cnt:
            dsem[(q, k)] = stack.enter_context(nc.semaphore(f"d_{q}_{k}"))
        block = stack.enter_context(nc.Block())

        def run(ename, engine):
            for waits, fn, tok in self.streams[ename]:
                for key, val in waits:
                    if key[0] == 'c':
                        engine.wait_ge(csem[(key[1], val // EPOCH)], val % EPOCH + 1)
                    else:
                        engine.wait_ge(dsem[(key[1], key[2])], 16 * (val + 1))
                if fn is None:
                    continue
                ins = fn(engine)
                if tok[0] == 'c':
                    ins.then_inc(csem[(tok[1], tok[2] // EPOCH)], 1)
                else:
                    ins.then_inc(dsem[(tok[1], tok[2])], 16)

        @block.sync
        def _(e):
            run('sp', e)

        @block.tensor
        def _(e):
            run('pe', e)

        @block.scalar
        def _(e):
            run('act', e)

        @block.vector
        def _(e):
            run('dve', e)

        @block.gpsimd
        def _(e):
            run('pool', e)


D = 1024
RMS_EPS = 1e-6


class StopBuild(Exception):
    pass

STOP = [10 ** 9]

def CHK(n):
    if n >= STOP[0]:
        raise StopBuild()


class Scope:
    def __enter__(self):
        self.st = ExitStack()
        self.st.__enter__()
        self.stopped = False
        return self

    def __exit__(self, et, ev, tb):
        if et is StopBuild:
            self.st.__exit__(None, None, None)
            self.stopped = True
            return True
        return self.st.__exit__(et, ev, tb)


class Cfg:
    def __init__(self, S):
        self.S = S
        self.OWN = S // 4
        self.TO = 256 + 128 + self.OWN + 128
        self.TR = S - self.OWN
        assert self.TO % 512 == 0 and self.TR % 512 == 0
        self.NSO = self.TO // 512
        self.NSR = self.TR // 512
        self.NTO = self.TO // 128
        self.NKT = (self.TO + self.TR) // 128
        self.TQ = 256 + self.OWN
        self.NTQ = self.TQ // 128


def _sb(nc, st):
    return lambda name, shape, dt: st.enter_context(nc.sbuf_tensor("s_" + name, shape, dt))


PSUM_KEYS = set()


def _ps(nc, st):
    def f(name, shape, dt):
        PSUM_KEYS.add(name)
        return st.enter_context(nc.psum_tensor("p_" + name, shape, dt))
    return f


def make_consts(P, nc, sb):
    C = {}
    C['ident'] = sb("ident", [128, 128], F32)
    C['identb'] = sb("identb", [128, 128], BF16)
    C['bones'] = sb("bones", [128, 128], BF16)
    C['ones'] = sb("ones", [128, 128], F32)
    i32 = C['ident']
    P.op('pool', lambda e: e.memset(i32[:], 0.0), writes=['ident'])
    P.op('pool', lambda e: e.affine_select(out=i32[:], in_=i32[:], pattern=[[-1, 128]], base=0,
                                           channel_multiplier=1, compare_op=ALU.not_equal, fill=1.0),
         reads=['ident'], writes=['ident'])
    P.op('pool', lambda e: e.tensor_copy(out=C['identb'][:], in_=i32[:]), reads=['ident'], writes=['identb'])
    P.op('pool', lambda e: e.memset(C['ones'][:], 1.0), writes=['ones'])
    bo = C['bones']
    P.op('pool', lambda e: e.memset(bo[:], 0.0), writes=['bones'])
    P.op('pool', lambda e: e.memset(bo[0:64, 0:64], 1.0), reads=['bones'], writes=['bones'])
    P.op('pool', lambda e: e.memset(bo[64:128, 64:128], 1.0), reads=['bones'], writes=['bones'])
    return C


def mod_setup(P, nc, st_outer, C, cT, ada_w, ada_bT, adab_rep, gmixT, gffnT, g5_dram, tagp=""):
    sbo = _sb(nc, st_outer)
    M = {}
    for n in ('Gm', 'Sm', 'Gf', 'Sf'):
        M[n] = sbo(tagp + n, [128, 8, 2], F32)
    with ExitStack() as st:
        sb = _sb(nc, st)
        ps = _ps(nc, st)
        scT = sb("scT", [128, 8, 2], F32)
        screp = sb("screp", [128, 8, 2, 128], F32)
        modT = sb("modT", [128, 48, 2], F32)
        abT = sb("abT", [128, 48], F32)
        gmix = sb("gmix", [128, 8], F32)
        gffn = sb("gffn", [128, 8], F32)
        abrep = sb("abrep", [128, 2, 1024], F32)
        g5t = sb("g5t", [128, 2, 1024], F32)
        g2t = sb("g2t", [128, 2, 1024], F32)
        wblk = [sb(f"wblk{i}", [128, 8, 512], F32) for i in range(2)]
        pmod_full = ps("pmod", [128, 512], F32)
        pmod = pmod_full[:, 0:96].rearrange("p (a b) -> p a b", b=2)
        pg = [ps(f"pg{i}", [128, 512], F32) for i in range(2)]
        P.dma('sp', lambda e: e.dma_start(out=scT[:], in_=cT[:, :, :]), writes=['scT'])
        P.dma('sp', lambda e: e.dma_start(out=abT[:], in_=ada_bT[:, :]), writes=['abT'])
        P.dma('sp', lambda e: e.dma_start(out=gmix[:], in_=gmixT[:, :]), writes=['gmix'])
        P.dma('sp', lambda e: e.dma_start(out=gffn[:], in_=gffnT[:, :]), writes=['gffn'])
        P.dma('sp', lambda e: e.dma_start(out=abrep[:], in_=adab_rep[:, :, :]), writes=['abrep'])
        P.op('act', lambda e: e.activation(out=scT[:], in_=scT[:], func=AF.Silu), reads=['scT'], writes=['scT'])
        for cl in range(2):
            P.op('dve', lambda e, cl=cl: e.tensor_copy(
                out=screp[:, :, cl, :], in_=scT[:, :, cl:cl + 1].broadcast_to([128, 8, 128])),
                reads=['scT'], writes=['screp'])
        for blk in range(12):
            wb = wblk[blk % 2]
            wk = f"wblk{blk % 2}"
            P.dma('sp', lambda e, wb=wb, blk=blk: e.dma_start(
                out=wb[:], in_=ada_w[:, blk * 512:(blk + 1) * 512].rearrange("(c p) n -> p c n", p=128)),
                writes=[wk])
            j = blk // 2
            half = blk % 2

            def fm(e, wb=wb, j=j, half=half):
                for fc in range(4):
                    for kc in range(8):
                        ins = e.matmul(pmod[:, j * 8 + half * 4 + fc, :], lhsT=wb[:, kc, fc * 128:(fc + 1) * 128],
                                       rhs=scT[:, kc, :], start=(kc == 0), stop=(kc == 7))
                return ins
            P.op('pe', fm, reads=[wk, 'scT'], writes=['pmod'])
            if j in (2, 5):
                gi = 0 if j == 2 else 1
                dst = g2t if j == 2 else g5t
                for cl in range(2):
                    pgt = pg[cl]

                    def rm(e, wb=wb, cl=cl, pgt=pgt):
                        for kc in range(8):
                            ins = e.matmul(pgt[:], lhsT=screp[:, kc, cl, :], rhs=wb[:, kc, :],
                                           start=(kc == 0), stop=(kc == 7))
                        return ins
                    P.op('pe', rm, reads=[wk, 'screp'], writes=[f'pg{cl}'])
                    P.op('dve', lambda e, pgt=pgt, dst=dst, cl=cl, gi=gi, half=half: e.tensor_tensor(
                        out=dst[:, cl, half * 512:(half + 1) * 512], in0=pgt[:],
                        in1=abrep[:, gi, half * 512:(half + 1) * 512], op=ALU.add),
                        reads=[f'pg{cl}', 'abrep'], writes=['grep'])
        P.dma('sp', lambda e: e.dma_start(out=g5_dram[1, :, :, :], in_=g5t[:]), reads=['grep'], writes=['g_dram'])
        P.dma('sp', lambda e: e.dma_start(out=g5_dram[0, :, :, :], in_=g2t[:]), reads=['grep'], writes=['g_dram'])
        P.op('dve', lambda e: e.tensor_tensor(out=modT[:], in0=pmod[:],
                                              in1=abT[:, :].unsqueeze(2).broadcast_to([128, 48, 2]), op=ALU.add),
             reads=['pmod', 'abT'], writes=['modT'])
        for (Gn, Sn, gg, jsh, jsc) in (('Gm', 'Sm', gmix, 0, 1), ('Gf', 'Sf', gffn, 3, 4)):
            Gt = M[Gn]
            St = M[Sn]
            P.op('dve', lambda e, Gt=Gt, jsc=jsc: e.tensor_scalar(
                out=Gt[:], in0=modT[:, jsc * 8:(jsc + 1) * 8, :], scalar1=1.0, scalar2=None, op0=ALU.add),
                reads=['modT'], writes=[tagp + Gn])
            P.op('dve', lambda e, Gt=Gt, gg=gg: e.tensor_tensor(
                out=Gt[:], in0=Gt[:], in1=gg[:, :].unsqueeze(2).broadcast_to([128, 8, 2]), op=ALU.mult),
                reads=[tagp + Gn, 'gmix', 'gffn'], writes=[tagp + Gn])
            P.op('dve', lambda e, St=St, jsh=jsh: e.tensor_copy(out=St[:], in_=modT[:, jsh * 8:(jsh + 1) * 8, :]),
                 reads=['modT'], writes=[tagp + Sn])
        P.barrier()
    return M


class NormT:
    def __init__(self, P, nc, sb, ps, C, tag="n"):
        self.P = P
        self.C = C
        self.tag = tag
        self.junk = sb(tag + "junk", [128, 1024], BF16)
        self.ss = [sb(tag + f"ss{i}", [128, 1], F32) for i in range(2)]
        self.rstd = [sb(tag + f"rstd{i}", [128, 1], F32) for i in range(2)]
        self.xn = [sb(tag + f"xn{i}", [128, 1024], BF16) for i in range(2)]
        self.pT = ps(tag + "pT", [128, 8, 128], BF16)
        self.i = 0

    def run(self, xt, xkey, G, S, cls, hT_out, hkey, mkeys, npart=128, halo=None):
        P = self.P
        t = self.tag
        i = self.i % 2
        self.i += 1
        npq = npart
        ss, rstd, xn = self.ss[i], self.rstd[i], self.xn[i]
        junk = self.junk
        P.op('act', lambda e: e.activation(out=junk[0:npq, :], in_=xt, func=AF.Square, accum_out=ss[0:npq, :]),
             reads=[xkey], writes=[t + 'junk', t + f'ss{i}'])
        P.op('act', lambda e: e.activation(out=rstd[0:npq, :], in_=ss[0:npq, :], func=AF.Ln, scale=1.0 / 1024, bias=RMS_EPS),
             reads=[t + f'ss{i}'], writes=[t + f'rstd{i}'])
        P.op('act', lambda e: e.activation(out=rstd[0:npq, :], in_=rstd[0:npq, :], func=AF.Exp, scale=-0.5),
             reads=[t + f'rstd{i}'], writes=[t + f'rstd{i}'])
        P.op('dve', lambda e: e.tensor_scalar(out=xn[0:npq, :], in0=xt, scalar1=rstd[0:npq, :], scalar2=None, op0=ALU.mult),
             reads=[xkey, t + f'rstd{i}'], writes=[t + f'xn{i}'])
        pT = self.pT
        identb = self.C['identb']

        def tr(e):
            for c in range(8):
                ins = e.transpose(out=pT[:, c, 0:npq], in_=xn[0:npq, c * 128:(c + 1) * 128], identity=identb[0:npq, 0:npq])
            return ins
        P.op('pe', tr, reads=[t + f'xn{i}', 'identb'], writes=[t + 'pT'])
        if halo is not None:
            hTx, n, left_ok, right_ok = halo
            for (ok, src, dst) in ((left_ok, 0, 0), (right_ok, 1, n + 1)):
                if not ok:
                    continue
                P.op('dve', lambda e, src=src, dst=dst: e.tensor_tensor(
                    out=hTx[:, :, dst:dst + 1], in0=pT[:, :, src:src + 1], in1=G[:, :, cls:cls + 1], op=ALU.mult),
                    reads=[t + 'pT'] + mkeys, writes=[hkey])
                P.op('dve', lambda e, dst=dst: e.tensor_tensor(
                    out=hTx[:, :, dst:dst + 1], in0=hTx[:, :, dst:dst + 1], in1=S[:, :, cls:cls + 1], op=ALU.add),
                    reads=mkeys, writes=[hkey])
            return
        for c in range(8):
            if c % 2 == 0:
                P.op('act', lambda e, c=c: e.activation(out=hT_out(c), in_=pT[:, c, :], func=AF.Identity,
                                                        scale=G[:, c, cls:cls + 1], bias=S[:, c, cls:cls + 1]),
                     reads=[t + 'pT'] + mkeys, writes=[hkey])
            else:
                P.op('dve', lambda e, c=c: e.tensor_scalar(out=hT_out(c), in0=pT[:, c, :],
                                                           scalar1=G[:, c, cls:cls + 1], scalar2=S[:, c, cls:cls + 1],
                                                           op0=ALU.mult, op1=ALU.add),
                     reads=[t + 'pT'] + mkeys, writes=[hkey])


def load_cast(P, nc, stage, skeys, dst_ap_fn, src_ap_fn, nparts, dkey, engs=('pool', 'act')):
    for i in range(nparts):
        sbuf = stage[i % len(stage)]
        sk = skeys[i % len(stage)]
        d = dst_ap_fn(i)
        s = src_ap_fn(i)
        shp = d.shape
        sv = sbuf
        P.dma('sp', lambda e, sv=sv, s=s: e.dma_start(out=sv, in_=s), writes=[sk])
        eng = engs[i % len(engs)]
        if eng == 'act':
            P.op('act', lambda e, d=d, sv=sv: e.activation(out=d, in_=sv, func=AF.Copy), reads=[sk], writes=[dkey])
        else:
            P.op(eng, lambda e, d=d, sv=sv: e.tensor_copy(out=d, in_=sv), reads=[sk], writes=[dkey])


def attn_phase(P, nc, cfg, C, M, T):
    with Scope() as sc0:
        st = sc0.st
        sb = _sb(nc, st)
        ps = _ps(nc, st)
        NKT, NTO = cfg.NKT, cfg.NTO
        kbT = sb("kbT", [128, NKT * 128], BF16)
        vb = sb("vb", [128, NKT, 2, 65], BF16)
        kaT = sb("kaT", [128, NTO * 128], BF16)
        va = sb("va", [128, NTO, 2, 65], BF16)
        gqk = sb("gqk", [128, 4], F32)
        g2rep = sb("g2rep", [128, 2, 1024], F32)
        P.dma('sp', lambda e: e.dma_start(out=g2rep[:], in_=T['gdram'][0, :, :, :]), reads=['g_dram'], writes=['grep'])
        masks = sb("masks", [128, 4, 128], BF16)
        sinkrow = sb("sinkrow", [65, 8], F32)
        pm = sb("pm", [128, 128], BF16)
        xt2 = [sb(f"xt2_{i}", [128, 1024], F32) for i in range(2)]
        hT = sb("hT", [128, 8, 512], BF16)
        cos_t = sb("cos", [128, 512], F32)
        sin_t = sb("sin", [128, 512], F32)
        t1 = sb("t1", [128, 512], F32)
        t2 = sb("t2", [128, 512], F32)
        sq = sb("sq", [128, 512], BF16)
        qraw = sb("qraw", [128, 512], BF16)
        rs = sb("rs", [128, 512], F32)
        pA = ps("pA", [128, 512], F32)
        pB = ps("pB", [128, 512], F32)
        pC = ps("pC", [128, 512], F32)
        nrm = NormT(P, nc, sb, ps, C, "n")
        xcount = [0]

        def rope_chunk(mm, normed, gi, dst, dkey, wkeys):
            P.op('pe', lambda e: mm(e, pA), reads=wkeys + ['hT'], writes=['pA'])
            P.op('act', lambda e: e.activation(out=qraw[:], in_=pA[:], func=AF.Copy), reads=['pA'], writes=['qraw'])
            P.op('pe', lambda e: e.matmul(pB[:], lhsT=pm[:], rhs=qraw[:], start=True, stop=True),
                 reads=['qraw', 'pm'], writes=['pB'])
            if not normed:
                P.op('dve', lambda e: e.tensor_tensor(out=t1[:], in0=pA[:], in1=cos_t[:], op=ALU.mult),
                     reads=['pA', 'cos'], writes=['t1'])
                P.op('dve', lambda e: e.tensor_tensor(out=t2[:], in0=pB[:], in1=sin_t[:], op=ALU.mult),
                     reads=['pB', 'sin'], writes=['t2'])
                P.op('dve', lambda e: e.tensor_tensor(out=dst, in0=t1[:], in1=t2[:], op=ALU.add),
                     reads=['t1', 't2'], writes=[dkey])
            else:
                P.op('act', lambda e: e.activation(out=sq[:], in_=pA[:], func=AF.Square), reads=['pA'], writes=['sq'])
                P.op('pe', lambda e: e.matmul(pC[:], lhsT=C['bones'][:], rhs=sq[:], start=True, stop=True),
                     reads=['sq', 'bones'], writes=['pC'])
                P.op('act', lambda e: e.activation(out=rs[:], in_=pC[:], func=AF.Ln, scale=1.0 / 64, bias=RMS_EPS),
                     reads=['pC'], writes=['rs'])
                P.op('act', lambda e: e.activation(out=rs[:], in_=rs[:], func=AF.Exp, scale=-0.5),
                     reads=['rs'], writes=['rs'])
                P.op('dve', lambda e: e.scalar_tensor_tensor(out=t1[:], in0=pA[:], scalar=gqk[:, gi:gi + 1], in1=cos_t[:],
                                                             op0=ALU.mult, op1=ALU.mult),
                     reads=['pA', 'cos', 'gqk'], writes=['t1'])
                P.op('dve', lambda e: e.scalar_tensor_tensor(out=t2[:], in0=pB[:], scalar=gqk[:, gi + 1:gi + 2],
                                                             in1=sin_t[:], op0=ALU.mult, op1=ALU.mult),
                     reads=['pB', 'sin', 'gqk'], writes=['t2'])
                P.op('dve', lambda e: e.tensor_tensor(out=t1[:], in0=t1[:], in1=t2[:], op=ALU.add),
                     reads=['t1', 't2'], writes=['t1'])
                P.op('dve', lambda e: e.tensor_tensor(out=dst, in0=t1[:], in1=rs[:], op=ALU.mult),
                     reads=['t1', 'rs'], writes=[dkey])

        def mm8(wt, c0, n=128):
            def f(e, pt):
                for k in range(8):
                    ins = e.matmul(pt[:], lhsT=wt[:, k, c0:c0 + n], rhs=hT[:, k, :], start=(k == 0), stop=(k == 7))
                return ins
            return f

        def load_norm_supertile(xsrc, s, clsfn, cosd, sind):
            P.dma('sp', lambda e: e.dma_start(out=cos_t[:], in_=cosd[:, s * 512:(s + 1) * 512]), writes=['cos'])
            P.dma('sp', lambda e: e.dma_start(out=sin_t[:], in_=sind[:, s * 512:(s + 1) * 512]), writes=['sin'])
            for tt in range(4):
                ti = s * 4 + tt
                xi = xcount[0] % 2
                xcount[0] += 1
                xt = xt2[xi]
                P.dma('sp', lambda e, xt=xt, ti=ti: e.dma_start(out=xt[:], in_=xsrc[ti * 128:(ti + 1) * 128, :]),
                      writes=[f'xt2_{xi}'])
                cls = clsfn(ti)
                nrm.run(xt[:], f'xt2_{xi}', M['Gm'], M['Sm'], cls,
                        lambda c, tt=tt: hT[:, c, tt * 128:(tt + 1) * 128], 'hT', ['Gm', 'Sm'])

        with Scope() as sc1:
            st1 = sc1.st
            sb1 = _sb(nc, st1)
            stage = [sb1(f"stage{i}", [128, 2048], F32) for i in range(2)]
            skeys = ['stage0', 'stage1']
            wkv = sb1("wkv", [128, 8, 512], BF16)
            P.dma('sp', lambda e: e.dma_start(out=gqk[:], in_=T['gqk'][:, :]), writes=['gqk'])
            P.op('pool', lambda e: e.memset(vb[:], 1.0), writes=['vb'])
            P.op('pool', lambda e: e.memset(va[:], 1.0), writes=['va'])
            for mi in range(4):
                P.dma('sp', lambda e, mi=mi: e.dma_start(out=stage[mi % 2][:, 0:128], in_=T['masks'][mi, :, :]),
                      writes=[skeys[mi % 2]])
                P.op('dve', lambda e, mi=mi: e.tensor_copy(out=masks[:, mi, :], in_=stage[mi % 2][:, 0:128]),
                     reads=[skeys[mi % 2]], writes=['masks'])
            P.dma('sp', lambda e: e.dma_start(out=stage[0][:, 0:128], in_=T['pm'][:, :]), writes=['stage0'])
            P.op('dve', lambda e: e.tensor_copy(out=pm[:], in_=stage[0][:, 0:128]), reads=['stage0'], writes=['pm'])
            P.op('pool', lambda e: e.memset(sinkrow[:], 0.0), writes=['sinkrow'])
            P.dma('sp', lambda e: e.dma_start(out=sinkrow[64:65, :], in_=T['sink'][:, :]),
                  reads=['sinkrow'], writes=['sinkrow'])
            P.op('act', lambda e: e.activation(out=sinkrow[64:65, :], in_=sinkrow[64:65, :], func=AF.Exp),
                 reads=['sinkrow'], writes=['sinkrow'])
            wsrc = T['wkv'].rearrange("(c p) n -> p c n", p=128)
            for k in range(8):
                P.dma('sp', lambda e, k=k: e.dma_start(out=stage[k % 2][:, 0:512], in_=wsrc[:, k, :]),
                      writes=[skeys[k % 2]])
                P.op('pool', lambda e, k=k: e.tensor_copy(out=wkv[:, k, :], in_=stage[k % 2][:, 0:512]),
                     reads=[skeys[k % 2]], writes=['wkv'])
            CHK(1)
            for stream in range(2):
                ns = cfg.NSO if stream == 0 else cfg.NSR
                xsrc = T['xo'] if stream == 0 else T['xr']
                cosd = T['cosO'] if stream == 0 else T['cosR']
                sind = T['sinO'] if stream == 0 else T['sinR']
                tbase = 0 if stream == 0 else NTO
                for s in range(ns):
                    clsfn = (lambda ti: 1 if ti < 2 else 0) if stream == 0 else (lambda ti: 0)
                    load_norm_supertile(xsrc, s, clsfn, cosd, sind)
                    CHK(2)
                    tok0 = (tbase + s * 4) * 128
                    if stream == 0:
                        rope_chunk(mm8(wkv, 0), False, 0, kaT[:, s * 512:(s + 1) * 512], 'kaT', ['wkv'])
                        CHK(3)
                    rope_chunk(mm8(wkv, 128), True, 2, kbT[:, tok0:tok0 + 512], 'kbT', ['wkv'])
                    CHK(4)
                    for tt in range(4):
                        c0, n = (256, 256) if stream == 0 else (384, 128)

                        def vm(e, tt=tt, c0=c0, n=n):
                            for k in range(8):
                                ins = e.matmul(pA[:, 0:n], lhsT=hT[:, k, tt * 128:(tt + 1) * 128], rhs=wkv[:, k, c0:c0 + n],
                                               start=(k == 0), stop=(k == 7))
                            return ins
                        P.op('pe', vm, reads=['hT', 'wkv'], writes=['pA'])
                        if stream == 0:
                            P.op('act', lambda e, s=s, tt=tt: e.activation(
                                out=va[:, s * 4 + tt, :, 0:64], in_=pA[:, 0:128].rearrange("p (g d) -> p g d", g=2),
                                func=AF.Copy), reads=['pA'], writes=['va'])
                            P.op('dve', lambda e, s=s, tt=tt: e.tensor_copy(
                                out=vb[:, s * 4 + tt, :, 0:64], in_=pA[:, 128:256].rearrange("p (g d) -> p g d", g=2)),
                                reads=['pA'], writes=['vb'])
                        else:
                            P.op('act', lambda e, s=s, tt=tt, tbase=tbase: e.activation(
                                out=vb[:, tbase + s * 4 + tt, :, 0:64],
                                in_=pA[:, 0:128].rearrange("p (g d) -> p g d", g=2),
                                func=AF.Copy), reads=['pA'], writes=['vb'])
                    CHK(5)
            P.barrier()
            CHK(6)

        if sc1.stopped:
            raise StopBuild()
        with Scope() as sc2:
            st2 = sc2.st
            sb2 = _sb(nc, st2)
            ps2 = _ps(nc, st2)
            wq = sb2("wq", [128, 8, 1024], BF16)
            wout = sb2("wout", [128, 8, 1024], BF16)
            with ExitStack() as st2a:
                sb2a = _sb(nc, st2a)
                stageB = [sb2a(f"stageb{i}", [128, 2048], F32) for i in range(2)]
                skeysB = ['stageb0', 'stageb1']
                wsrcq = T['wq'].rearrange("(c p) n -> p c n", p=128)
                for k in range(8):
                    P.dma('sp', lambda e, k=k: e.dma_start(out=stageB[k % 2][:, 0:1024], in_=wsrcq[:, k, :]),
                          writes=[skeysB[k % 2]])
                    P.op('pool', lambda e, k=k: e.tensor_copy(out=wq[:, k, :], in_=stageB[k % 2][:, 0:1024]),
                         reads=[skeysB[k % 2]], writes=['wq'])
                for k in range(8):
                    P.dma('sp', lambda e, k=k: e.dma_start(out=stageB[k % 2][:, 0:1024], in_=T['wout'][:, k, :]),
                          writes=[skeysB[k % 2]])
                    P.op('pool', lambda e, k=k: e.tensor_copy(out=wout[:, k, :], in_=stageB[k % 2][:, 0:1024]),
                         reads=[skeysB[k % 2]], writes=['wout'])
                P.barrier()
            CHK(7)
            qT = sb2("qT", [128, 8, 512], BF16)
            o_all = sb2("o_all", [128, 8, 512], BF16)
            Pt = [sb2(f"Pt{i}", [128, 512], BF16) for i in range(3)]
            rden = sb2("rden", [65, 512], F32)
            bcs = sb2("bcs", [64, 512], F32)
            tmpy = sb2("tmpy", [128, 1024], F32)
            xres = sb2("xres", [128, 1024], F32)
            pS = [ps2(f"pS{i}", [128, 512], F32) for i in range(2)]
            pO = ps2("pO", [128, 512], F32)
            pBC = ps2("pBC", [128, 512], F32)
            P.op('pool', lambda e: e.memset(o_all[:], 0.0), writes=['o_all'])
            P.op('pool', lambda e: e.memset(rden[:], 1.0), writes=['rden'])
            pcount = [0]
            ones = C['ones']

            def v3(ap, three):
                return ap.rearrange("p (h q) -> p h q", h=4) if three else ap

            def attend(ktiles, kT, vt, kkey, vkey, g, rhs_fn, ncols, mask_fn, sink_g, out_fn, three):
                nk = len(ktiles)
                for ii, kt in enumerate(ktiles):
                    pi = pcount[0] % 2
                    bi = pcount[0] % 3
                    pcount[0] += 1
                    pSt = pS[pi]
                    Pb = Pt[bi]
                    P.op('pe', lambda e, kt=kt, pSt=pSt: e.matmul(
                        v3(pSt[:, 0:ncols], three), lhsT=kT[g * 64:(g + 1) * 64, kt * 128:(kt + 1) * 128], rhs=rhs_fn(),
                        start=True, stop=True), reads=[kkey, 'qT'], writes=[f'pS{pi}'])
                    P.op('act', lambda e, pSt=pSt, Pb=Pb: e.activation(out=Pb[:, 0:ncols], in_=pSt[:, 0:ncols],
                                                                        func=AF.Exp, scale=0.125),
                         reads=[f'pS{pi}'], writes=[f'Pt{bi}'])
                    mk = mask_fn(kt) if mask_fn is not None else None
                    if mk is not None:
                        P.op('dve', lambda e, Pb=Pb, mk=mk: e.tensor_tensor(
                            out=v3(Pb[:, 0:ncols], True), in0=v3(Pb[:, 0:ncols], True),
                            in1=masks[:, mk, :].unsqueeze(1).broadcast_to([128, 4, 128]),
                            op=ALU.mult), reads=[f'Pt{bi}', 'masks'], writes=[f'Pt{bi}'])
                    P.op('pe', lambda e, kt=kt, Pb=Pb, ii=ii: e.matmul(
                        pO[0:65, 0:ncols], lhsT=vt[:, kt, g, 0:65], rhs=Pb[:, 0:ncols], start=(ii == 0), stop=(ii == nk - 1)),
                        reads=[vkey, f'Pt{bi}'], writes=['pO'])
                if sink_g is not None:
                    P.op('dve', lambda e: e.tensor_tensor(
                        out=v3(rden[64:65, 0:ncols], True), in0=v3(pO[64:65, 0:ncols], True),
                        in1=sinkrow[64:65, sink_g * 4:(sink_g + 1) * 4].unsqueeze(2).broadcast_to([1, 4, 128]),
                        op=ALU.add), reads=['pO', 'sinkrow'], writes=['rden'])
                    P.op('dve', lambda e: e.reciprocal(out=rden[64:65, 0:ncols], in_=rden[64:65, 0:ncols]),
                         reads=['rden'], writes=['rden'])
                else:
                    P.op('dve', lambda e: e.reciprocal(out=rden[64:65, 0:ncols], in_=pO[64:65, 0:ncols]),
                         reads=['pO'], writes=['rden'])
                P.op('pe', lambda e: e.matmul(pBC[0:64, 0:ncols], lhsT=ones[64:65, 0:64], rhs=rden[64:65, 0:ncols],
                                              start=True, stop=True), reads=['rden', 'ones'], writes=['pBC'])
                P.op('act', lambda e: e.activation(out=bcs[:, 0:ncols], in_=pBC[0:64, 0:ncols], func=AF.Copy),
                     reads=['pBC'], writes=['bcs'])
                P.op('dve', lambda e: e.tensor_tensor(out=out_fn(), in0=v3(pO[0:64, 0:ncols], three),
                                                      in1=v3(bcs[:, 0:ncols], three),
                                                      op=ALU.mult), reads=['pO', 'bcs'], writes=['o_all'])

            last_own = NTO - 2
            allk = [kt for kt in range(NKT) if kt != 2 and kt != NTO - 1]
            for s in range(cfg.NSO):
                load_norm_supertile(T['xo'], s, (lambda ti: 1 if ti < 2 else 0), T['cosO'], T['sinO'])
                for c in range(8):
                    rope_chunk(mm8(wq, c * 128), c >= 4, 0, qT[:, c, :], 'qT', ['wq'])
                CHK(8)
                tiles = [s * 4 + tt for tt in range(4)]
                for tt, ti in enumerate(tiles):
                    if ti == 2 or ti == NTO - 1:
                        continue
                    if ti < 2:
                        kts = [0, 1]
                        mfn = None
                    else:
                        kts = [0, 1, ti - 1, ti, ti + 1]

                        def mfn(kt, ti=ti):
                            if kt == ti - 1:
                                return 0 if ti == 3 else 1
                            if kt == ti + 1:
                                return 3 if ti == last_own else 2
                            return None
                    for g in range(2):
                        attend(kts, kaT, va, 'kaT', 'va', g,
                               lambda g=g, tt=tt: qT[g * 64:(g + 1) * 64, 0:4, tt * 128:(tt + 1) * 128],
                               512, mfn, g,
                               lambda g=g, tt=tt: o_all[0:64, g * 4:(g + 1) * 4, tt * 128:(tt + 1) * 128], True)
                CHK(9)
                groups = []
                run0 = None
                for tt, ti in enumerate(tiles):
                    if ti < 2:
                        kind = 'c'
                    elif ti == 2 or ti == NTO - 1:
                        kind = None
                    else:
                        kind = 'o'
                    if run0 is not None and run0[0] == kind:
                        run0[2] += 128
                    else:
                        if run0 is not None and run0[0] is not None:
                            groups.append(tuple(run0))
                        run0 = [kind, tt * 128, 128]
                if run0 is not None and run0[0] is not None:
                    groups.append(tuple(run0))
                for (kind, c0, n) in groups:
                    kts = [0, 1] if kind == 'c' else allk
                    for hb in range(8):
                        g = hb // 4
                        j = hb % 4
                        attend(kts, kbT, vb, 'kbT', 'vb', g,
                               lambda g=g, j=j, c0=c0, n=n: qT[g * 64:(g + 1) * 64, 4 + j, c0:c0 + n],
                               n, None, None,
                               lambda hb=hb, c0=c0, n=n: o_all[64:128, hb, c0:c0 + n], False)
                CHK(10)
                for tt, ti in enumerate(tiles):
                    if ti == 2 or ti == NTO - 1:
                        continue
                    cls = 1 if ti < 2 else 0
                    row0 = ti * 128 if ti < 2 else 256 + (ti - 3) * 128
                    P.dma('sp', lambda e, ti=ti: e.dma_start(out=xres[:], in_=T['xo'][ti * 128:(ti + 1) * 128, :]),
                          writes=['xres'])
                    for half in range(2):
                        def om(e, tt=tt, half=half):
                            for hh in range(8):
                                ins = e.matmul(pA[:], lhsT=o_all[:, hh, tt * 128:(tt + 1) * 128],
                                               rhs=wout[:, hh, half * 512:(half + 1) * 512],
                                               start=(hh == 0), stop=(hh == 7))
                            return ins
                        P.op('pe', om, reads=['o_all', 'wout'], writes=['pA'])
                        P.op('dve', lambda e, half=half, cls=cls: e.tensor_tensor(
                            out=tmpy[:, half * 512:(half + 1) * 512], in0=pA[:],
                            in1=g2rep[:, cls, half * 512:(half + 1) * 512], op=ALU.mult),
                            reads=['pA', 'grep'], writes=['tmpy'])
                    P.op('dve', lambda e: e.tensor_tensor(out=tmpy[:], in0=tmpy[:], in1=xres[:], op=ALU.add),
                         reads=['tmpy', 'xres'], writes=['tmpy'])
                    P.dma('sp', lambda e, row0=row0: e.dma_start(out=T['x1'][row0:row0 + 128, :], in_=tmpy[:]),
                          reads=['tmpy'], writes=['x1'])
            P.barrier()
        if sc2.stopped:
            raise StopBuild()
    if sc0.stopped:
        raise StopBuild()


def moe_phase(P, nc, C, M, T, NT, cls_fn, tag="m", final_norm=False):
    GT = 8
    groups = [list(range(i, min(i + GT, NT))) for i in range(0, NT, GT)]
    with ExitStack() as st:
        sb = _sb(nc, st)
        ps = _ps(nc, st)
        wset = [[sb(f"{tag}w{j}_{i}", [128, 8, 1024], BF16) for j in range(3)] for i in range(2)]
        stage = [sb(f"{tag}stg{i}", [128, 2, 1024], F32) for i in range(2)]
        h2T = sb(tag + "h2T", [128, 8, GT * 128], BF16)
        acc = sb(tag + "acc", [128, GT, 1024], F32)
        gT = sb(tag + "gT", [128, 8, 512], BF16)
        sgt = sb(tag + "sgt", [128, 512], F32)
        xm = sb(tag + "xm", [128, 1024], F32)
        xn32 = sb(tag + "xn32", [128, 1024], F32)
        h32 = sb(tag + "h32", [128, 8, 128], F32)
        g5rep = sb(tag + "g5rep", [128, 2, 1024], F32)
        gate = sb(tag + "gate", [128, GT, 16], F32)
        rw = sb(tag + "rw", [128, 8, 16], F32)
        rbias = sb(tag + "rbias", [128, 16], F32)
        if final_norm:
            fng = sb(tag + "fng", [128, 1024], F32)
            P.dma('sp', lambda e: e.dma_start(out=fng[:], in_=T['fng'][:, :]), writes=[tag + 'fng'])
        junk = sb(tag + "junk", [128, 1024], BF16)
        sm = sb(tag + "sm", [128, 16], F32)
        r16 = [sb(tag + f"r16_{i}", [128, 16], F32) for i in range(5)]
        r4 = [sb(tag + f"r4_{i}", [128, 4], F32) for i in range(4)]
        pA = [ps(f"{tag}pA{i}", [128, 512], F32) for i in range(2)]
        pB = [ps(f"{tag}pB{i}", [128, 512], F32) for i in range(2)]
        pY = [ps(f"{tag}pY{i}", [128, 512], F32) for i in range(2)]
        pT = ps(tag + "pT32", [128, 8, 128], F32)
        K = lambda n: tag + n
        P.dma('sp', lambda e: e.dma_start(out=g5rep[:], in_=T['gdram'][1, :, :, :]), reads=['g_dram'], writes=[K('g5rep')])
        P.dma('sp', lambda e: e.dma_start(out=rw[:], in_=T['rw'][:, :, :]), writes=[K('rw')])
        P.dma('sp', lambda e: e.dma_start(out=rbias[:], in_=T['rbias'][:, :]), writes=[K('rbias')])
        ident = C['ident']
        wcount = [0]

        def load_expert(e_idx):
            ws = wset[e_idx % 2]
            for j, wn in enumerate(('w1', 'w3', 'w2')):
                src = T[wn][e_idx, :, :].rearrange("(c p) n -> p c n", p=128)
                for q in range(4):
                    si = wcount[0] % 2
                    wcount[0] += 1
                    sg = stage[si]
                    P.dma('sp', lambda e, sg=sg, src=src, q=q: e.dma_start(out=sg[:], in_=src[:, 2 * q:2 * q + 2, :]),
                          writes=[K(f'stg{si}')])
                    P.op('pool', lambda e, sg=sg, ws=ws, j=j, q=q: e.tensor_copy(out=ws[j][:, 2 * q:2 * q + 2, :], in_=sg[:]),
                         reads=[K(f'stg{si}')], writes=[K(f'w{j}_{e_idx % 2}')])

        for grp in groups:
            ng = len(grp)
            for tl, t in enumerate(grp):
                cls = cls_fn(t)
                P.dma('sp', lambda e, t=t: e.dma_start(out=xm[:], in_=T['x_in'][t * 128:(t + 1) * 128, :]),
                      reads=['x_in_' + tag], writes=[K('xm')])
                P.op('act', lambda e: e.activation(out=junk[:], in_=xm[:], func=AF.Square, accum_out=sm[:, 0:1]),
                     reads=[K('xm')], writes=[K('junk'), K('sm0')])
                P.op('act', lambda e: e.activation(out=sm[:, 1:2], in_=sm[:, 0:1], func=AF.Ln, scale=1.0 / 1024, bias=RMS_EPS),
                     reads=[K('sm0')], writes=[K('sm1')])
                P.op('act', lambda e: e.activation(out=sm[:, 1:2], in_=sm[:, 1:2], func=AF.Exp, scale=-0.5),
                     reads=[K('sm1')], writes=[K('sm1')])
                P.op('dve', lambda e: e.tensor_scalar(out=xn32[:], in0=xm[:], scalar1=sm[:, 1:2], scalar2=None, op0=ALU.mult),
                     reads=[K('xm'), K('sm1')], writes=[K('xn32')])

                def tr(e):
                    for c in range(8):
                        ins = e.transpose(out=pT[:, c, :], in_=xn32[:, c * 128:(c + 1) * 128], identity=ident[:])
                    return ins
                P.op('pe', tr, reads=[K('xn32'), 'ident'], writes=[K('pT32')])
                for c in range(8):
                    if c % 2 == 0:
                        P.op('act', lambda e, c=c, cls=cls: e.activation(
                            out=h32[:, c, :], in_=pT[:, c, :], func=AF.Identity,
                            scale=M['Gf'][:, c, cls:cls + 1], bias=M['Sf'][:, c, cls:cls + 1]),
                            reads=[K('pT32'), 'Gf', 'Sf'], writes=[K('h32')])
                    else:
                        P.op('dve', lambda e, c=c, cls=cls: e.tensor_scalar(
                            out=h32[:, c, :], in0=pT[:, c, :], scalar1=M['Gf'][:, c, cls:cls + 1],
                            scalar2=M['Sf'][:, c, cls:cls + 1], op0=ALU.mult, op1=ALU.add),
                            reads=[K('pT32'), 'Gf', 'Sf'], writes=[K('h32')])
                P.op('pool', lambda e, tl=tl: e.tensor_copy(out=h2T[:, :, tl * 128:(tl + 1) * 128], in_=h32[:]),
                     reads=[K('h32')], writes=[K('h2T')])
                pR = pY[0]

                def rm(e):
                    for k in range(8):
                        ins = e.matmul(pR[:, 0:16], lhsT=h32[:, k, :], rhs=rw[:, k, :], start=(k == 0), stop=(k == 7))
                    return ins
                P.op('pe', rm, reads=[K('h32'), K('rw')], writes=[K('pY0')])
                lg, ex, probs, sel, sel2 = r16
                top1, top2, gs, gsel = r4
                kk = [K(f'r16_{i}') for i in range(5)]
                k4 = [K(f'r4_{i}') for i in range(4)]
                P.op('dve', lambda e: e.tensor_copy(out=lg[:], in_=pR[:, 0:16]), reads=[K('pY0')], writes=[kk[0]])
                P.op('dve', lambda e: e.tensor_reduce(out=sm[:, 2:3], in_=lg[:], axis=AX.X, op=ALU.max, negate=True),
                     reads=[kk[0]], writes=[K('sm2')])
                P.op('act', lambda e: e.activation(out=ex[:], in_=lg[:], func=AF.Exp, bias=sm[:, 2:3], accum_out=sm[:, 3:4]),
                     reads=[kk[0], K('sm2')], writes=[kk[1], K('sm3')])
                P.op('dve', lambda e: e.reciprocal(out=sm[:, 4:5], in_=sm[:, 3:4]), reads=[K('sm3')], writes=[K('sm4')])
                P.op('dve', lambda e: e.tensor_scalar(out=probs[:], in0=ex[:], scalar1=sm[:, 4:5], scalar2=None, op0=ALU.mult),
                     reads=[kk[1], K('sm4')], writes=[kk[2]])
                P.op('dve', lambda e: e.tensor_tensor(out=sel[:], in0=probs[:], in1=rbias[:], op=ALU.add),
                     reads=[kk[2], K('rbias')], writes=[kk[3]])
                s3 = lambda ap: ap.rearrange("p (g j) -> p g j", g=4)
                b3 = lambda ap: ap.unsqueeze(2).broadcast_to([128, 4, 4])
                P.op('dve', lambda e: e.tensor_reduce(out=top1[:], in_=s3(sel[:]), axis=AX.X, op=ALU.max),
                     reads=[kk[3]], writes=[k4[0]])
                P.op('dve', lambda e: e.tensor_tensor(out=s3(sel2[:]), in0=s3(sel[:]), in1=b3(top1[:]), op=ALU.is_equal),
                     reads=[kk[3], k4[0]], writes=[kk[4]])
                P.op('dve', lambda e: e.scalar_tensor_tensor(out=sel2[:], in0=sel2[:], scalar=-1e30, in1=sel[:],
                                                             op0=ALU.mult, op1=ALU.add),
                     reads=[kk[4], kk[3]], writes=[kk[4]])
                P.op('dve', lambda e: e.tensor_reduce(out=top2[:], in_=s3(sel2[:]), axis=AX.X, op=ALU.max),
                     reads=[kk[4]], writes=[k4[1]])
                P.op('dve', lambda e: e.tensor_tensor(out=gs[:], in0=top1[:], in1=top2[:], op=ALU.add),
                     reads=[k4[0], k4[1]], writes=[k4[2]])
                P.op('dve', lambda e: e.tensor_reduce(out=sm[:, 5:6], in_=gs[:], axis=AX.X, op=ALU.max),
                     reads=[k4[2]], writes=[K('sm5')])
                P.op('dve', lambda e: e.tensor_scalar(out=gsel[:], in0=gs[:], scalar1=sm[:, 5:6], scalar2=None, op0=ALU.is_equal),
                     reads=[k4[2], K('sm5')], writes=[k4[3]])
                P.op('dve', lambda e: e.tensor_tensor(out=s3(sel2[:]), in0=s3(sel[:]), in1=b3(top2[:]), op=ALU.is_ge),
                     reads=[kk[3], k4[1]], writes=[kk[4]])
                P.op('dve', lambda e: e.tensor_tensor(out=s3(sel2[:]), in0=s3(sel2[:]), in1=b3(gsel[:]), op=ALU.mult),
                     reads=[kk[4], k4[3]], writes=[kk[4]])
                P.op('dve', lambda e: e.tensor_tensor(out=ex[:], in0=probs[:], in1=sel2[:], op=ALU.mult),
                     reads=[kk[2], kk[4]], writes=[kk[1]])
                P.op('dve', lambda e: e.tensor_reduce(out=sm[:, 6:7], in_=ex[:], axis=AX.X, op=ALU.add),
                     reads=[kk[1]], writes=[K('sm6')])
                P.op('dve', lambda e: e.reciprocal(out=sm[:, 7:8], in_=sm[:, 6:7]), reads=[K('sm6')], writes=[K('sm7')])
                P.op('dve', lambda e, tl=tl: e.tensor_scalar(out=gate[:, tl, :], in0=ex[:], scalar1=sm[:, 7:8], scalar2=None,
                                                             op0=ALU.mult), reads=[kk[1], K('sm7')], writes=[K('gate')])
            P.op('pool', lambda e: e.memset(acc[:], 0.0), writes=[K('acc')])
            subs = [list(range(i, min(i + 4, ng))) for i in range(0, ng, 4)]
            pc = [0]
            for ex_i in range(16):
                load_expert(ex_i)
                ws = wset[ex_i % 2]
                wk = [K(f'w{j}_{ex_i % 2}') for j in range(3)]
                for sub in subs:
                    c0 = sub[0] * 128
                    ncol = len(sub) * 128
                    for fc in range(8):
                        pi = pc[0] % 2
                        pc[0] += 1
                        pa, pb = pA[pi], pB[pi]

                        def m1(e, pa=pa, fc=fc, w=ws[0], ncol=ncol, c0=c0):
                            for k in range(8):
                                ins = e.matmul(pa[:, 0:ncol], lhsT=w[:, k, fc * 128:(fc + 1) * 128], rhs=h2T[:, k, c0:c0 + ncol],
                                               start=(k == 0), stop=(k == 7))
                            return ins

                        def m3(e, pb=pb, fc=fc, w=ws[1], ncol=ncol, c0=c0):
                            for k in range(8):
                                ins = e.matmul(pb[:, 0:ncol], lhsT=w[:, k, fc * 128:(fc + 1) * 128], rhs=h2T[:, k, c0:c0 + ncol],
                                               start=(k == 0), stop=(k == 7))
                            return ins
                        P.op('pe', m1, reads=[wk[0], K('h2T')], writes=[K(f'pA{pi}')])
                        P.op('pe', m3, reads=[wk[1], K('h2T')], writes=[K(f'pB{pi}')])
                        P.op('act', lambda e, pa=pa, ncol=ncol: e.activation(out=sgt[:, 0:ncol], in_=pa[:, 0:ncol], func=AF.Silu),
                             reads=[K(f'pA{pi}')], writes=[K('sgt')])
                        P.op('dve', lambda e, pb=pb, fc=fc, ncol=ncol: e.tensor_tensor(out=gT[:, fc, 0:ncol], in0=sgt[:, 0:ncol],
                                                                            in1=pb[:, 0:ncol], op=ALU.mult),
                             reads=[K('sgt'), K(f'pB{pi}')], writes=[K('gT')])
                    for tl2, tl in enumerate(sub):
                        for half in range(2):
                            yi = pc[0] % 2
                            pc[0] += 1
                            py = pY[yi]

                            def m2(e, py=py, tl2=tl2, half=half, w=ws[2]):
                                for fc in range(8):
                                    ins = e.matmul(py[:], lhsT=gT[:, fc, tl2 * 128:(tl2 + 1) * 128],
                                                   rhs=w[:, fc, half * 512:(half + 1) * 512], start=(fc == 0), stop=(fc == 7))
                                return ins
                            P.op('pe', m2, reads=[K('gT'), wk[2]], writes=[K(f'pY{yi}')])
                            P.op('dve', lambda e, py=py, tl=tl, half=half, ex_i=ex_i: e.scalar_tensor_tensor(
                                out=acc[:, tl, half * 512:(half + 1) * 512], in0=py[:], scalar=gate[:, tl, ex_i:ex_i + 1],
                                in1=acc[:, tl, half * 512:(half + 1) * 512], op0=ALU.mult, op1=ALU.add),
                                reads=[K(f'pY{yi}'), K('gate'), K('acc')], writes=[K('acc')])
            for tl, t in enumerate(grp):
                cls = cls_fn(t)
                P.dma('sp', lambda e, t=t: e.dma_start(out=xm[:], in_=T['x_in'][t * 128:(t + 1) * 128, :]),
                      reads=['x_in_' + tag], writes=[K('xm')])
                P.op('dve', lambda e, tl=tl, cls=cls: e.tensor_tensor(out=xn32[:], in0=acc[:, tl, :], in1=g5rep[:, cls, :],
                                                                      op=ALU.mult),
                     reads=[K('acc'), K('g5rep')], writes=[K('xn32')])
                P.op('dve', lambda e: e.tensor_tensor(out=xn32[:], in0=xn32[:], in1=xm[:], op=ALU.add),
                     reads=[K('xn32'), K('xm')], writes=[K('xn32')])
                if final_norm:
                    P.op('act', lambda e: e.activation(out=junk[:], in_=xn32[:], func=AF.Square, accum_out=sm[:, 8:9]),
                         reads=[K('xn32')], writes=[K('junk'), K('sm8')])
                    P.op('act', lambda e: e.activation(out=sm[:, 9:10], in_=sm[:, 8:9], func=AF.Ln, scale=1.0 / 1024, bias=RMS_EPS),
                         reads=[K('sm8')], writes=[K('sm9')])
                    P.op('act', lambda e: e.activation(out=sm[:, 9:10], in_=sm[:, 9:10], func=AF.Exp, scale=-0.5),
                         reads=[K('sm9')], writes=[K('sm9')])
                    P.op('dve', lambda e: e.scalar_tensor_tensor(out=xn32[:], in0=xn32[:], scalar=sm[:, 9:10], in1=fng[:],
                                                                 op0=ALU.mult, op1=ALU.mult),
                         reads=[K('xn32'), K('sm9'), K('fng')], writes=[K('xn32')])
                P.dma('sp', lambda e, t=t: e.dma_start(out=T['x_out'][t * 128:(t + 1) * 128, :], in_=xn32[:]),
                      reads=[K('xn32')], writes=['x_out_' + tag])
        P.barrier()


def build_A(cfg, debug_out=True, phases=('attn', 'moe'), n_exp=16):
    nc = bass.Bass("TRN2", target_bir_lowering=False)
    def din(name, shape):
        return nc.dram_tensor(name, shape, F32, kind="ExternalInput").ap()
    T = {}
    T['xo'] = din("xo", [cfg.TO, 1024])
    T['xr'] = din("xr", [cfg.TR, 1024])
    T['cosO'] = din("cosO", [128, cfg.TO])
    T['sinO'] = din("sinO", [128, cfg.TO])
    T['cosR'] = din("cosR", [128, cfg.TR])
    T['sinR'] = din("sinR", [128, cfg.TR])
    T['masks'] = din("masks", [4, 128, 128])
    T['gqk'] = din("gqk", [128, 4])
    T['sink'] = din("sink", [1, 8])
    T['pm'] = din("pm", [128, 128])
    T['wkv'] = din("wkv", [1024, 512])
    T['wq'] = din("wq", [1024, 1024])
    T['wout'] = din("wout", [128, 8, 1024])
    cT = din("cT", [128, 8, 2])
    ada_w = din("ada_w", [1024, 6144])
    ada_bT = din("ada_bT", [128, 48])
    adab_rep = din("adab_rep", [128, 2, 1024])
    gmixT = din("gmixT", [128, 8])
    gffnT = din("gffnT", [128, 8])
    if 'moe' in phases:
        T['rw'] = din("rw", [128, 8, 16])
        T['rbias'] = din("rbias", [128, 16])
        T['w1'] = din("w1", [n_exp, 1024, 1024])
        T['w3'] = din("w3", [n_exp, 1024, 1024])
        T['w2'] = din("w2", [n_exp, 1024, 1024])
        T['x2'] = nc.dram_tensor("x2", [cfg.TQ, 1024], F32, kind="ExternalOutput").ap()
    T['x1'] = nc.dram_tensor("x1", [cfg.TQ, 1024], F32, kind="ExternalOutput").ap()
    T['gdram'] = nc.dram_tensor("gdram", [2, 128, 2, 1024], F32, kind="ExternalOutput").ap()
    P = Prog(nc)
    P.psum_keys = PSUM_KEYS
    with ExitStack() as st:
        sb = _sb(nc, st)
        C = make_consts(P, nc, sb)
        M = mod_setup(P, nc, st, C, cT, ada_w, ada_bT, adab_rep, gmixT, gffnT, T['gdram'])
        if 'attn' in phases:
            try:
                attn_phase(P, nc, cfg, C, M, T)
            except StopBuild:
                P.barrier()
        if 'moe' in phases:
            T['x_in'] = T['x1']
            T['x_out'] = T['x2']
            moe_phase(P, nc, C, M, T, cfg.NTQ, lambda t: 1 if t < 2 else 0, "m")
        P.final_wait('sp')
        P.emit(st)
    print("A ops:", P.nops, {e: len(P.streams[e]) for e in P.ENG})
    return nc


def rope_tab(pos):
    p = np.arange(128)
    d = p % 64
    axis = d // 32
    half = (d % 32) // 16
    f = d % 16
    inv = (10000.0 ** (-(f.astype(np.float32)) / 16.0)).astype(np.float32)
    pos = np.asarray(pos)
    row = (pos // 64).astype(np.float32)
    col = (pos % 64).astype(np.float32)
    pa = np.where(axis[:, None] == 0, row[None, :], col[None, :]).astype(np.float32)
    ang = (pa * inv[:, None]).astype(np.float32)
    cos = np.cos(ang).astype(np.float32)
    sin = np.sin(ang).astype(np.float32) * np.where(half == 0, -1.0, 1.0)[:, None].astype(np.float32)
    nr = pos < 0
    cos[:, nr] = 1.0
    sin[:, nr] = 0.0
    return cos, sin.astype(np.float32)


def partner():
    p = np.arange(128)
    d = p % 64
    pd = np.where((d % 32) < 16, d + 16, d - 16)
    return (p // 64) * 64 + pd


def host_A(inp, cfg, core, layer=0, with_moe=True):
    b, qd = core // 4, core % 4
    S, OWN = cfg.S, cfg.OWN
    x = inp['x'][b, :S]
    ctx = inp['ctx'][b]
    o0 = qd * OWN
    z = np.zeros((128, 1024), np.float32)
    hl = x[o0 - 128:o0] if qd > 0 else z
    hr = x[o0 + OWN:o0 + OWN + 128] if qd < 3 else z
    xo = np.concatenate([ctx, hl, x[o0:o0 + OWN], hr], 0)
    posO = np.concatenate([-np.ones(256, np.int64),
                           np.arange(o0 - 128, o0) if qd > 0 else -np.ones(128, np.int64),
                           np.arange(o0, o0 + OWN),
                           np.arange(o0 + OWN, o0 + OWN + 128) if qd < 3 else -np.ones(128, np.int64)])
    xr = np.concatenate([x[:o0], x[o0 + OWN:]], 0)
    posR = np.concatenate([np.arange(0, o0), np.arange(o0 + OWN, S)])
    cosO, sinO = rope_tab(posO)
    cosR, sinR = rope_tab(posR)
    k = np.arange(128)[:, None]
    q = np.arange(128)[None, :]
    mL = (k >= q).astype(np.float32)
    mR = (k <= q).astype(np.float32)
    masks = np.stack([mL if qd > 0 else 0 * mL, mL, mR, mR if qd < 3 else 0 * mR])
    pt = partner()
    d = np.arange(128) % 64
    gq = inp['attn_q_norm_g'][0]
    gk = inp['attn_k_norm_g'][0]
    gqk = np.stack([gq[d], gq[pt % 64], gk[d], gk[pt % 64]], 1).astype(np.float32)
    pm = np.zeros((128, 128), np.float32)
    pm[pt, np.arange(128)] = 1.0
    w_in = inp['attn_w_in'][0]
    wkv = np.concatenate([w_in[:, 512:640], w_in[:, 1280:1408], w_in[:, 640:768], w_in[:, 1408:1536]], 1)
    cols = []
    for c in range(4):
        cols += [w_in[:, c * 64:(c + 1) * 64], w_in[:, (4 + c) * 64:(5 + c) * 64]]
    for j in range(4):
        cols += [w_in[:, 768 + j * 64:768 + (j + 1) * 64], w_in[:, 768 + (4 + j) * 64:768 + (5 + j) * 64]]
    wq = np.concatenate(cols, 1)
    w_out = inp['attn_w_out'][0]
    wout = np.zeros((128, 8, 1024), np.float32)
    for hh in range(8):
        wout[0:64, hh] = w_out[hh * 64:(hh + 1) * 64]
        wout[64:128, hh] = w_out[512 + hh * 64:512 + (hh + 1) * 64]
    m = {}
    m.update(xo=xo, xr=xr, cosO=cosO, sinO=sinO, cosR=cosR, sinR=sinR, masks=masks, gqk=gqk,
             sink=inp['attn_sink'][0][None, :], pm=pm, wkv=wkv, wq=wq, wout=wout)
    m.update(host_mod(inp, b, layer))
    if with_moe:
        m.update(host_moe(inp, layer))
    return {k_: np.ascontiguousarray(v, dtype=np.float32) for k_, v in m.items()}


def host_mod(inp, b, layer):
    cT = np.stack([inp['c'][b].reshape(8, 128).T, inp['c_ctx'].reshape(8, 128).T], 2)
    ab = inp['ada_b'][layer]
    return dict(cT=cT, ada_w=inp['ada_w'][layer], ada_bT=ab.reshape(48, 128).T,
                adab_rep=np.broadcast_to(np.stack([ab[2048:3072], ab[5120:6144]])[None], (128, 2, 1024)),
                gmixT=inp['norm_mix_g'][layer].reshape(8, 128).T, gffnT=inp['norm_ffn_g'][layer].reshape(8, 128).T)


def host_moe(inp, layer):
    return dict(rw=inp['router_w'].reshape(8, 128, 16).transpose(1, 0, 2),
                rbias=np.broadcast_to(inp['router_bias'][None], (128, 16)),
                w1=inp['moe_w1'][layer], w3=inp['moe_w3'][layer], w2=inp['moe_w2'][layer])


def build_A2(NT, n_exp=16):
    nc = bass.Bass("TRN2", target_bir_lowering=False)
    def din(name, shape):
        return nc.dram_tensor(name, shape, F32, kind="ExternalInput").ap()
    T = {}
    T['x_in'] = din("xin", [NT * 128, 1024])
    cT = din("cT", [128, 8, 2])
    ada_w = din("ada_w", [1024, 6144])
    ada_bT = din("ada_bT", [128, 48])
    adab_rep = din("adab_rep", [128, 2, 1024])
    gmixT = din("gmixT", [128, 8])
    gffnT = din("gffnT", [128, 8])
    T['rw'] = din("rw", [128, 8, 16])
    T['rbias'] = din("rbias", [128, 16])
    T['w1'] = din("w1", [n_exp, 1024, 1024])
    T['w3'] = din("w3", [n_exp, 1024, 1024])
    T['w2'] = din("w2", [n_exp, 1024, 1024])
    T['x_out'] = nc.dram_tensor("x2", [NT * 128, 1024], F32, kind="ExternalOutput").ap()
    T['gdram'] = nc.dram_tensor("gdram", [2, 128, 2, 1024], F32, kind="ExternalOutput").ap()
    P = Prog(nc)
    P.psum_keys = PSUM_KEYS
    with ExitStack() as st:
        sb = _sb(nc, st)
        C = make_consts(P, nc, sb)
        M = mod_setup(P, nc, st, C, cT, ada_w, ada_bT, adab_rep, gmixT, gffnT, T['gdram'])
        moe_phase(P, nc, C, M, T, NT, lambda t: 1 if t < 2 else 0, "m")
        P.final_wait('sp')
        P.emit(st)
    print("A2 ops:", P.nops, {e: len(P.streams[e]) for e in P.ENG})
    return nc


def host_A2(inp, xin, b):
    m = {'xin': xin}
    m.update(host_mod(inp, b, 0))
    m.update(host_moe(inp, 0))
    return {k_: np.ascontiguousarray(v, dtype=np.float32) for k_, v in m.items()}


DEC_C = -0.6065306597126334
INV_DT = BF16


def rwkv_phase(P, nc, C, M, T, NL):
    with ExitStack() as st:
        sb = _sb(nc, st)
        ps = _ps(nc, st)
        wrkv = sb("wrkv", [128, 8, 768], BF16)
        wl = sb("wl", [128, 8, 416], BF16)
        w2c = sb("w2c", [128, 256], BF16)
        a2c = sb("a2c", [128, 256], BF16)
        g2a = sb("g2a", [128, 256], BF16)
        g2b = sb("g2b", [32, 256], BF16)
        xmix = sb("xmix", [128, 8, 6], F32)
        colv = sb("colv", [128, 2, 6], F32)
        rkw = sb("rkw", [128, 2, 2], BF16)
        lnrep = sb("lnrep", [128, 2, 256], F32)
        mk = sb("mk", [128, 10, 128], F32)
        rmask = sb("rmask", [128, 512], F32)
        stg = [sb(f"bstg{i}", [128, 1024], F32) for i in range(2)]
        sk = ['bstg0', 'bstg1']
        cnt = [0]

        def ld(dst, src, shape_cols, dkey, nparts=128):
            i = cnt[0] % 2
            cnt[0] += 1
            P.dma('sp', lambda e: e.dma_start(out=stg[i][0:nparts, 0:shape_cols], in_=src), writes=[sk[i]])
            P.op('pool', lambda e: e.tensor_copy(out=dst, in_=stg[i][0:nparts, 0:shape_cols]), reads=[sk[i]], writes=[dkey])
        wsrc = T['wrkv'].rearrange("(c p) n -> p c n", p=128)
        for k in range(8):
            ld(wrkv[:, k, :], wsrc[:, k, :], 768, 'wrkv')
        for (nm, c0, n) in (('w1cat', 0, 128), ('a1cat', 128, 128), ('g1', 256, 160)):
            src = T[nm].rearrange("(c p) n -> p c n", p=128)
            for k in range(8):
                ld(wl[:, k, c0:c0 + n], src[:, k, :], n, 'wl')
        ld(w2c[:], T['w2cat'][:, :], 256, 'w2c')
        ld(a2c[:], T['a2cat'][:, :], 256, 'a2c')
        ld(g2a[:], T['g2a'][:, :], 256, 'g2a')
        ld(g2b[:], T['g2b'][:, :], 256, 'g2b', nparts=32)
        ld(rkw[:].rearrange("p a b -> p (a b)"), T['rkw'].rearrange("p a b -> p (a b)"), 4, 'rkw')
        P.dma('sp', lambda e: e.dma_start(out=xmix[:], in_=T['xmixT'][:, :, :]), writes=['xmix'])
        P.dma('sp', lambda e: e.dma_start(out=colv[:], in_=T['colvec'][:, :, :]), writes=['colv'])
        P.dma('sp', lambda e: e.dma_start(out=lnrep[:], in_=T['lnrep'][:, :, :]), writes=['lnrep'])
        for i in range(10):
            P.dma('sp', lambda e, i=i: e.dma_start(out=mk[:, i, :], in_=T['mk'][i, :, :]), writes=['mk'])
        P.op('pool', lambda e: e.memset(rmask[:], 1.0), writes=['rmask'])
        P.op('pool', lambda e: e.memset(rmask[:].rearrange("p (c t) -> p c t", t=64)[:, :, 0:1], 0.0),
             reads=['rmask'], writes=['rmask'])
        P.barrier()

        xt = [sb(f"bxt{i}", [128, 1024], F32) for i in range(2)]
        xh = sb("bxh", [2, 1024], F32)
        hTx = sb("hTx", [128, 8, 514], BF16)
        xx = sb("xx", [128, 8, 512], BF16)
        mixb = [sb(f"mix{i}", [128, 8, 512], BF16) for i in range(2)]
        nrm = NormT(P, nc, sb, ps, C, "bn")
        F = lambda name: sb(name, [128, 2, 512], F32)
        rT, kT, lw, icl, kkn, kd, cin, tmpA, tmpB, e2 = [F(n) for n in
                                                         ("rT", "kT", "lw", "icl", "kkn", "kd", "cin", "tmpA", "tmpB", "e2")]
        arT = sb("arT", [128, 2, 2, 512], BF16)
        bhT = sb("bhT", [128, 2, 512], BF16)
        khT = sb("khT", [128, 2, 512], BF16)
        bgT = sb("bgT", [128, 2, 512], BF16)
        kgT = sb("kgT", [128, 2, 512], BF16)
        rkb = sb("rkb", [128, 2, 512], BF16)
        sqb = sb("sqb", [128, 512], BF16)
        th = sb("th", [128, 512], BF16)
        ua = sb("ua", [128, 512], BF16)
        sg = sb("sg", [128, 512], BF16)
        sg2 = sb("sg2", [32, 512], BF16)
        vtok = sb("vtok", [128, 4, 256], BF16)
        gtok = sb("gtok", [128, 4, 256], F32)
        bon = sb("bon", [128, 4, 4], F32)
        gC = sb("gC", [128, 2, 8], F32)
        tot = sb("tot", [128, 2, 8], F32)
        tk = sb("tk", [128, 3, 256], BF16)
        U1 = sb("U1", [128, 4, 2, 128], BF16)
        U2 = sb("U2", [128, 4, 2, 128], BF16)
        AA = [sb(f"AA{i}", [128, 4, 2, 128], F32) for i in range(2)]
        AB = [sb(f"AB{i}", [128, 4, 2, 128], F32) for i in range(2)]
        UoffT = sb("UoffT", [128, 4, 3, 128], F32)
        T32 = sb("T32", [128, 4, 2, 128], F32)
        Zs = sb("Zs", [128, 4, 2, 128], F32)
        Tfin = sb("Tfin", [128, 4, 128], BF16)
        Mtmp = sb("Mtmp", [128, 2, 64], F32)
        XA = sb("XA", [128, 4, 2, 64], BF16)
        WA = sb("WA", [128, 2, 4, 64], BF16)
        RtT = sb("RtT", [128, 2, 128], F32)
        Msb = sb("Msb", [128, 2, 2, 128], F32)
        Sbd = sb("Sbd", [128, 2, 128], F32)
        ysb = sb("ysb", [128, 256], F32)
        y0t = sb("y0t", [128, 256], F32)
        b0t = sb("b0t", [128, 4], F32)
        st8 = sb("st8", [128, 16], F32)
        osb = sb("osb", [128, 256], F32)
        pA = ps("bpA", [128, 512], F32)
        pG = [ps(f"bpG{i}", [128, 2, 2, 128], F32) for i in range(2)]
        pXf = ps("bpX", [128, 512], F32)
        pX = pXf[:].rearrange("p (h a k) -> p h a k", h=4, a=2)
        pB = pXf
        pYf = ps("bpY", [128, 512], F32)
        pY = pYf[:, 0:256].rearrange("p (h v) -> p h v", h=4)
        pSf = ps("bpS", [128, 512], F32)
        pS = pSf[:, 0:128].rearrange("p (c v) -> p c v", c=2)
        pTt_full = ps("bpTt", [128, 4, 256], BF16)
        pTt = pTt_full[:, 0:3, :]
        ident = C['ident']
        identb = C['identb']
        xc_ = [0]

        def proj_fm(w, c0, n, mix, out_ps, wkey):
            def f(e):
                for k in range(8):
                    ins = e.matmul(out_ps, lhsT=w[:, k, c0:c0 + n], rhs=mix[:, k, :], start=(k == 0), stop=(k == 7))
                return ins
            return f

        def do_segment(tok0, n, is_ctx, left_ok, right_ok, d, final):
            nt = n // 128
            nsc = n // 64
            cls = 1 if is_ctx else 0
            for tt in range(nt):
                xi = xc_[0] % 2
                xc_[0] += 1
                P.dma('sp', lambda e, xi=xi, tt=tt: e.dma_start(out=xt[xi][:], in_=T['xs'][tok0 + tt * 128:tok0 + (tt + 1) * 128, :]),
                      writes=[f'bxt{xi}'])
                nrm.run(xt[xi][:], f'bxt{xi}', M['Gm'], M['Sm'], cls,
                        lambda c, tt=tt: hTx[:, c, 1 + tt * 128:1 + (tt + 1) * 128], 'hTx', ['Gm', 'Sm'])
            P.op('dve', lambda e: e.memset(hTx[:, :, 0:1], 0.0), writes=['hTx'])
            P.op('dve', lambda e: e.memset(hTx[:, :, n + 1:n + 2], 0.0), writes=['hTx'])
            if left_ok or right_ok:
                P.op('dve', lambda e: e.memset(xh[:], 1.0), writes=['bxh'])
                if left_ok:
                    P.dma('sp', lambda e: e.dma_start(out=xh[0:1, :], in_=T['xs'][tok0 - 1:tok0, :]), reads=['bxh'], writes=['bxh'])
                if right_ok:
                    P.dma('sp', lambda e: e.dma_start(out=xh[1:2, :], in_=T['xs'][tok0 + n:tok0 + n + 1, :]), reads=['bxh'], writes=['bxh'])
                nrm.run(xh[:], 'bxh', M['Gm'], M['Sm'], cls, None, 'hTx', ['Gm', 'Sm'], npart=2,
                        halo=(hTx, n, left_ok, right_ok))
            P.op('dve', lambda e: e.tensor_tensor(out=xx[:, :, 0:n], in0=hTx[:, :, 0:n], in1=hTx[:, :, 2:n + 2], op=ALU.add),
                 reads=['hTx'], writes=['xx'])
            P.op('dve', lambda e: e.scalar_tensor_tensor(out=xx[:, :, 0:n], in0=xx[:, :, 0:n], scalar=0.5, in1=hTx[:, :, 1:n + 1],
                                                         op0=ALU.mult, op1=ALU.subtract), reads=['xx', 'hTx'], writes=['xx'])
            mc = [0]

            def make_mix(j):
                mi = mc[0] % 2
                mc[0] += 1
                mb = mixb[mi]
                for c in range(8):
                    P.op('dve', lambda e, c=c, mb=mb: e.scalar_tensor_tensor(
                        out=mb[:, c, 0:n], in0=xx[:, c, 0:n], scalar=xmix[:, c, j:j + 1], in1=hTx[:, c, 1:n + 1],
                        op0=ALU.mult, op1=ALU.add), reads=['xx', 'hTx', 'xmix'], writes=[f'mix{mi}'])
                return mb, f'mix{mi}'
            mb, mkey = make_mix(0)
            for cc in range(2):
                P.op('pe', proj_fm(wrkv, cc * 128, 128, mb[:, :, 0:n], pA[:, 0:n], 'wrkv'), reads=['wrkv', mkey], writes=['bpA'])
                P.op('act', lambda e, cc=cc: e.activation(out=rT[:, cc, 0:n], in_=pA[:, 0:n], func=AF.Copy), reads=['bpA'], writes=['rT'])
            mb, mkey = make_mix(2)
            for cc in range(2):
                P.op('pe', proj_fm(wrkv, 256 + cc * 128, 128, mb[:, :, 0:n], pA[:, 0:n], 'wrkv'), reads=['wrkv', mkey], writes=['bpA'])
                P.op('act', lambda e, cc=cc: e.activation(out=kT[:, cc, 0:n], in_=pA[:, 0:n], func=AF.Copy), reads=['bpA'], writes=['kT'])
            mb, mkey = make_mix(3)
            for tt in range(nt):
                def vm(e, tt=tt, mb=mb):
                    for k in range(8):
                        ins = e.matmul(pA[:, 0:256], lhsT=mb[:, k, tt * 128:(tt + 1) * 128], rhs=wrkv[:, k, 512:768],
                                       start=(k == 0), stop=(k == 7))
                    return ins
                P.op('pe', vm, reads=['wrkv', mkey], writes=['bpA'])
                P.op('act', lambda e, tt=tt: e.activation(out=vtok[:, tt, :], in_=pA[:, 0:256], func=AF.Copy), reads=['bpA'], writes=['vtok'])
            mb, mkey = make_mix(1)
            dp = d * 64
            P.op('pe', proj_fm(wl, d * 64, 64, mb[:, :, 0:n], pB[dp:dp + 64, 0:n], 'wl'), reads=['wl', mkey], writes=['bpX'])
            P.op('act', lambda e: e.activation(out=th[dp:dp + 64, 0:n], in_=pB[dp:dp + 64, 0:n], func=AF.Tanh), reads=['bpX'], writes=['th'])
            for cc in range(2):
                P.op('pe', lambda e, cc=cc: e.matmul(pA[:, 0:n], lhsT=w2c[dp:dp + 64, cc * 128:(cc + 1) * 128], rhs=th[dp:dp + 64, 0:n],
                                                     start=True, stop=True), reads=['w2c', 'th'], writes=['bpA'])
                P.op('act', lambda e, cc=cc: e.activation(out=lw[:, cc, 0:n], in_=pA[:, 0:n], func=AF.Sigmoid, bias=colv[:, cc, d:d + 1]),
                     reads=['bpA', 'colv'], writes=['lw'])
            P.op('dve', lambda e: e.tensor_scalar(out=lw[:, :, 0:n], in0=lw[:, :, 0:n], scalar1=DEC_C, scalar2=None, op0=ALU.mult),
                 reads=['lw'], writes=['lw'])
            mb, mkey = make_mix(4)
            P.op('pe', proj_fm(wl, 128 + d * 64, 64, mb[:, :, 0:n], pB[dp:dp + 64, 0:n], 'wl'), reads=['wl', mkey], writes=['bpX'])
            P.op('act', lambda e: e.activation(out=ua[dp:dp + 64, 0:n], in_=pB[dp:dp + 64, 0:n], func=AF.Copy), reads=['bpX'], writes=['ua'])
            for cc in range(2):
                P.op('pe', lambda e, cc=cc: e.matmul(pA[:, 0:n], lhsT=a2c[dp:dp + 64, cc * 128:(cc + 1) * 128], rhs=ua[dp:dp + 64, 0:n],
                                                     start=True, stop=True), reads=['a2c', 'ua'], writes=['bpA'])
                P.op('act', lambda e, cc=cc: e.activation(out=icl[:, cc, 0:n], in_=pA[:, 0:n], func=AF.Sigmoid, bias=colv[:, cc, 2 + d:3 + d]),
                     reads=['bpA', 'colv'], writes=['icl'])
            if final and not is_ctx:
                mb, mkey = make_mix(5)
                P.op('pe', proj_fm(wl, 256, 128, mb[:, :, 0:n], pA[:, 0:n], 'wl'), reads=['wl', mkey], writes=['bpA'])
                P.op('act', lambda e: e.activation(out=sg[:, 0:n], in_=pA[:, 0:n], func=AF.Sigmoid), reads=['bpA'], writes=['sg'])
                P.op('pe', proj_fm(wl, 384, 32, mb[:, :, 0:n], pB[0:32, 0:n], 'wl'), reads=['wl', mkey], writes=['bpX'])
                P.op('act', lambda e: e.activation(out=sg2[:, 0:n], in_=pB[0:32, 0:n], func=AF.Sigmoid), reads=['bpX'], writes=['sg2'])
                for tt in range(nt):
                    def gm(e, tt=tt):
                        e.matmul(pA[:, 0:256], lhsT=sg[:, tt * 128:(tt + 1) * 128], rhs=g2a[:, :], start=True, stop=False)
                        return e.matmul(pA[:, 0:256], lhsT=sg2[:, tt * 128:(tt + 1) * 128], rhs=g2b[:, :], start=False, stop=True)
                    P.op('pe', gm, reads=['sg', 'sg2', 'g2a', 'g2b'], writes=['bpA'])
                    P.op('act', lambda e, tt=tt: e.activation(out=gtok[:, tt, :], in_=pA[:, 0:256], func=AF.Copy), reads=['bpA'], writes=['gtok'])
            V3 = lambda t_: t_[:, :, 0:n]
            for cc in range(2):
                P.op('dve', lambda e, cc=cc: e.tensor_scalar(out=kkn[:, cc, 0:n], in0=kT[:, cc, 0:n], scalar1=colv[:, cc, 4:5], scalar2=None,
                                                             op0=ALU.mult), reads=['kT', 'colv'], writes=['kkn'])
                P.op('act', lambda e, cc=cc: e.activation(out=sqb[:, 0:n], in_=kkn[:, cc, 0:n], func=AF.Square), reads=['kkn'], writes=['sqb'])
                P.op('pe', lambda e: e.matmul(pB[:, 0:n], lhsT=C['bones'][:], rhs=sqb[:, 0:n], start=True, stop=True),
                     reads=['sqb', 'bones'], writes=['bpX'])
                P.op('dve', lambda e, cc=cc: e.tensor_scalar(out=tmpA[:, cc, 0:n], in0=pB[:, 0:n], scalar1=1e-18, scalar2=None, op0=ALU.max),
                     reads=['bpX'], writes=['tmpA'])
            P.op('act', lambda e: e.activation(out=V3(tmpA), in_=V3(tmpA), func=AF.Ln), reads=['tmpA'], writes=['tmpA'])
            P.op('act', lambda e: e.activation(out=V3(tmpA), in_=V3(tmpA), func=AF.Exp, scale=-0.5), reads=['tmpA'], writes=['tmpA'])
            P.op('dve', lambda e: e.tensor_tensor(out=V3(kkn), in0=V3(kkn), in1=V3(tmpA), op=ALU.mult), reads=['kkn', 'tmpA'], writes=['kkn'])
            for cc in range(2):
                P.op('dve', lambda e, cc=cc: e.tensor_scalar(out=kd[:, cc, 0:n], in0=icl[:, cc, 0:n], scalar1=-1.0, scalar2=colv[:, cc, 5:6],
                                                             op0=ALU.add, op1=ALU.mult), reads=['icl', 'colv'], writes=['kd'])
            P.op('dve', lambda e: e.scalar_tensor_tensor(out=V3(kd), in0=V3(kd), scalar=1.0, in1=V3(kT), op0=ALU.add, op1=ALU.mult),
                 reads=['kd', 'kT'], writes=['kd'])
            P.op('dve', lambda e: e.tensor_tensor(out=V3(rkb), in0=V3(rT), in1=V3(kd), op=ALU.mult), reads=['rT', 'kd'], writes=['rkb'])
            for tt in range(nt):
                def bm(e, tt=tt):
                    for cc in range(2):
                        ins = e.matmul(pB[:, cc * 2:cc * 2 + 2], lhsT=rkb[:, cc, tt * 128:(tt + 1) * 128], rhs=rkw[:, cc, :], start=True, stop=True)
                    return ins
                P.op('pe', bm, reads=['rkb', 'rkw'], writes=['bpX'])
                P.op('dve', lambda e, tt=tt: e.tensor_copy(out=bon[:, tt, :], in_=pB[:, 0:4]), reads=['bpX'], writes=['bon'])
            for cc in range(2):
                P.op('dve', lambda e, cc=cc: e.tensor_tensor_scan(out=cin[:, cc, 0:n], data0=rmask[:, 0:n], data1=lw[:, cc, 0:n], initial=0.0,
                                                                  op0=ALU.mult, op1=ALU.add), reads=['rmask', 'lw'], writes=['cin'])
            c4 = lambda t_: t_[:, :, 0:n].rearrange("p c (k t) -> p c k t", t=64)
            P.op('dve', lambda e: e.tensor_copy(out=tot[:, :, 0:nsc], in_=c4(cin)[:, :, :, 63]), reads=['cin'], writes=['tot'])
            totb = lambda: tot[:, :, 0:nsc].unsqueeze(3).broadcast_to([128, 2, nsc, 64])
            if d == 1:
                P.op('dve', lambda e: e.tensor_tensor(out=V3(cin), in0=V3(lw), in1=V3(cin), op=ALU.subtract), reads=['lw', 'cin'], writes=['cin'])
                P.op('dve', lambda e: e.tensor_tensor(out=c4(cin), in0=c4(cin), in1=totb(), op=ALU.add), reads=['cin', 'tot'], writes=['cin'])
            P.op('act', lambda e: e.activation(out=gC[:, :, 0:nsc], in_=tot[:, :, 0:nsc], func=AF.Exp), reads=['tot'], writes=['gC'])
            P.op('dve', lambda e: e.tensor_tensor(out=c4(e2), in0=totb(), in1=c4(cin), op=ALU.subtract), reads=['cin', 'tot'], writes=['e2'])
            P.op('act', lambda e: e.activation(out=V3(e2), in_=V3(e2), func=AF.Exp), reads=['e2'], writes=['e2'])
            P.op('act', lambda e: e.activation(out=V3(tmpA), in_=V3(cin), func=AF.Exp), reads=['cin'], writes=['tmpA'])
            P.op('dve', lambda e: e.tensor_tensor(out=arT[:, :, 1, 0:n], in0=V3(rT), in1=V3(tmpA), op=ALU.mult), reads=['rT', 'tmpA'], writes=['arT'])
            P.op('dve', lambda e: e.tensor_tensor(out=V3(tmpB), in0=V3(cin), in1=V3(lw), op=ALU.subtract), reads=['cin', 'lw'], writes=['tmpB'])
            P.op('act', lambda e: e.activation(out=V3(tmpB), in_=V3(tmpB), func=AF.Exp), reads=['tmpB'], writes=['tmpB'])
            P.op('dve', lambda e: e.scalar_tensor_tensor(out=arT[:, :, 0, 0:n], in0=V3(kkn), scalar=-1.0, in1=V3(tmpB), op0=ALU.mult, op1=ALU.mult),
                 reads=['kkn', 'tmpB'], writes=['arT'])
            P.op('act', lambda e: e.activation(out=V3(tmpA), in_=V3(cin), func=AF.Exp, scale=-1.0), reads=['cin', 'arT'], writes=['tmpA'])
            P.op('dve', lambda e: e.tensor_tensor(out=V3(tmpB), in0=V3(kkn), in1=V3(icl), op=ALU.mult), reads=['kkn', 'icl', 'arT'], writes=['tmpB'])
            P.op('dve', lambda e: e.tensor_tensor(out=V3(bhT), in0=V3(tmpB), in1=V3(tmpA), op=ALU.mult), reads=['tmpB', 'tmpA'], writes=['bhT'])
            P.op('dve', lambda e: e.tensor_tensor(out=V3(khT), in0=V3(kd), in1=V3(tmpA), op=ALU.mult), reads=['kd', 'tmpA'], writes=['khT'])
            P.op('dve', lambda e: e.tensor_tensor(out=V3(bgT), in0=V3(tmpB), in1=V3(e2), op=ALU.mult), reads=['tmpB', 'e2'], writes=['bgT'])
            P.op('dve', lambda e: e.tensor_tensor(out=V3(kgT), in0=V3(kd), in1=V3(e2), op=ALU.mult), reads=['kd', 'e2'], writes=['kgT'])
            if d == 0:
                m_s, mA, mAt, mO1, mO1T, mO2T = 0, 4, 5, 6, 7, 9
            else:
                m_s, mA, mAt, mO1, mO1T, mO2T = 2, 5, 4, 7, 6, 8
            order = list(range(nt)) if d == 0 else list(range(nt - 1, -1, -1))
            corder = [0, 1] if d == 0 else [1, 0]
            for tt in order:
                cs = slice(tt * 128, (tt + 1) * 128)
                def trs(e, cs=cs):
                    for qi, src in enumerate((None, bgT, kgT)):
                        for cc in range(2):
                            s_ap = arT[:, cc, 0, cs] if qi == 0 else src[:, cc, cs]
                            ins = e.transpose(out=pTt[:, qi, cc * 128:(cc + 1) * 128], in_=s_ap, identity=identb[:])
                    return ins
                P.op('pe', trs, reads=['arT', 'bgT', 'kgT', 'identb'], writes=['bpTt'])
                P.op('act', lambda e: e.activation(out=tk[:], in_=pTt, func=AF.Copy), reads=['bpTt'], writes=['tk'])
                P.op('dve', lambda e: e.tensor_copy(out=XA[:, :, 1, :], in_=tk[:, 0, :].rearrange("p (h k) -> p h k", h=4)),
                     reads=['tk'], writes=['XA'])
                for hh in range(2):
                    hp = hh * 64
                    hs = slice(hh, 4, 2)

                    def gr(e, hp=hp, cs=cs, src=bhT, gp=pG[0]):
                        for cc in range(2):
                            ins = e.matmul(gp[:, cc, :, :], lhsT=src[hp:hp + 64, cc, cs], rhs=arT[hp:hp + 64, cc, :, cs], start=True, stop=True)
                        return ins
                    P.op('pe', gr, reads=['bhT', 'arT'], writes=['bpG0'])
                    P.op('dve', lambda e, hs=hs: e.tensor_tensor(
                        out=U1[:, hs, :, :], in0=pG[0][:],
                        in1=mk[:, m_s:m_s + 2, :].unsqueeze(1).broadcast_to([128, 2, 2, 128]), op=ALU.mult),
                        reads=['bpG0', 'mk'], writes=['U1'])
                    P.op('dve', lambda e, hs=hs: e.tensor_tensor(
                        out=AA[0][:, hs, 0, :], in0=pG[0][:, :, 0, :],
                        in1=mk[:, mA, :].unsqueeze(1).broadcast_to([128, 2, 128]), op=ALU.mult),
                        reads=['bpG0', 'mk'], writes=['AA0'])
                    P.op('dve', lambda e, hs=hs: e.tensor_tensor(
                        out=UoffT[:, hs, 1, :], in0=pG[0][:, :, 0, :],
                        in1=mk[:, mO1, :].unsqueeze(1).broadcast_to([128, 2, 128]), op=ALU.mult),
                        reads=['bpG0', 'mk'], writes=['UoffT'])
                    P.op('pe', lambda e, hp=hp, cs=cs: gr(e, hp, cs, khT, pG[1]), reads=['khT', 'arT'], writes=['bpG1'])
                    P.op('dve', lambda e, hs=hs: e.tensor_tensor(
                        out=U2[:, hs, :, :], in0=pG[1][:],
                        in1=mk[:, m_s:m_s + 2, :].unsqueeze(1).broadcast_to([128, 2, 2, 128]), op=ALU.mult),
                        reads=['bpG1', 'mk'], writes=['U2'])

                    def gt(e, hp=hp, cs=cs):
                        for cc in range(2):
                            ins = e.matmul(pG[0][:, cc, 0, :], lhsT=arT[hp:hp + 64, cc, 0, cs], rhs=bhT[hp:hp + 64, cc, cs], start=True, stop=True)
                        return ins
                    P.op('pe', gt, reads=['bhT', 'arT'], writes=['bpG0'])
                    P.op('dve', lambda e, hs=hs: e.tensor_tensor(
                        out=AB[0][:, hs, 0, :], in0=pG[0][:, :, 0, :],
                        in1=mk[:, mAt, :].unsqueeze(1).broadcast_to([128, 2, 128]), op=ALU.mult),
                        reads=['bpG0', 'mk'], writes=['AB0'])
                    P.op('dve', lambda e, hs=hs: e.tensor_tensor(
                        out=UoffT[:, hs, 0, :], in0=pG[0][:, :, 0, :],
                        in1=mk[:, mO1T, :].unsqueeze(1).broadcast_to([128, 2, 128]), op=ALU.mult),
                        reads=['bpG0', 'mk'], writes=['UoffT'])
                    P.op('dve', lambda e, hs=hs: e.tensor_tensor(
                        out=UoffT[:, hs, 2, :], in0=pG[0][:, :, 0, :],
                        in1=mk[:, mO2T, :].unsqueeze(1).broadcast_to([128, 2, 128]), op=ALU.mult),
                        reads=['bpG0', 'mk'], writes=['UoffT'])
                P.op('dve', lambda e: e.tensor_copy(out=AA[0][:, :, 1, :], in_=ident[:].unsqueeze(1).broadcast_to([128, 4, 128])),
                     reads=['ident'], writes=['AA0'])
                P.op('dve', lambda e: e.tensor_copy(out=AB[0][:, :, 1, :], in_=ident[:].unsqueeze(1).broadcast_to([128, 4, 128])),
                     reads=['ident'], writes=['AB0'])
                cur = 0
                NIT = 4
                for it in range(NIT):
                    nxt = 1 - cur
                    last = (it == NIT - 1)
                    for cc in range(2):
                        gp = pG[cc]
                        hs = slice(cc * 2, cc * 2 + 2)

                        def im(e, cc=cc, cur=cur, gp=gp, last=last):
                            for hh in range(2):
                                h = cc * 2 + hh
                                if last:
                                    ins = e.matmul(gp[:, hh, 1, :], lhsT=AB[cur][:, h, 0, :], rhs=AA[cur][:, h, 1, :], start=True, stop=True)
                                else:
                                    ins = e.matmul(gp[:, hh, :, :], lhsT=AB[cur][:, h, 0, :], rhs=AA[cur][:, h, :, :], start=True, stop=True)
                            return ins
                        P.op('pe', im, reads=[f'AB{cur}', f'AA{cur}'], writes=[f'bpG{cc}'])
                        P.op('dve', lambda e, hs=hs, cur=cur, nxt=nxt, gp=gp: e.tensor_tensor(
                            out=AA[nxt][:, hs, 1, :], in0=gp[:, :, 1, :], in1=AA[cur][:, hs, 1, :], op=ALU.add),
                            reads=[f'bpG{cc}', f'AA{cur}'], writes=[f'AA{nxt}'])
                        if not last:
                            P.op('act', lambda e, hs=hs, nxt=nxt, gp=gp: e.activation(
                                out=AA[nxt][:, hs, 0, :], in_=gp[:, :, 0, :], func=AF.Copy),
                                reads=[f'bpG{cc}'], writes=[f'AA{nxt}'])

                        def im2(e, cc=cc, cur=cur, gp=gp, last=last):
                            for hh in range(2):
                                h = cc * 2 + hh
                                if last:
                                    ins = e.matmul(gp[:, hh, 1, :], lhsT=AA[cur][:, h, 0, :], rhs=AB[cur][:, h, 1, :], start=True, stop=True)
                                else:
                                    ins = e.matmul(gp[:, hh, :, :], lhsT=AA[cur][:, h, 0, :], rhs=AB[cur][:, h, :, :], start=True, stop=True)
                            return ins
                        P.op('pe', im2, reads=[f'AB{cur}', f'AA{cur}'], writes=[f'bpG{cc}'])
                        P.op('dve', lambda e, hs=hs, cur=cur, nxt=nxt, gp=gp: e.tensor_tensor(
                            out=AB[nxt][:, hs, 1, :], in0=gp[:, :, 1, :], in1=AB[cur][:, hs, 1, :], op=ALU.add),
                            reads=[f'bpG{cc}', f'AB{cur}'], writes=[f'AB{nxt}'])
                        if not last:
                            P.op('act', lambda e, hs=hs, nxt=nxt, gp=gp: e.activation(
                                out=AB[nxt][:, hs, 0, :], in_=gp[:, :, 0, :], func=AF.Copy),
                                reads=[f'bpG{cc}'], writes=[f'AB{nxt}'])
                    cur = nxt
                for cc in range(2):
                    gp = pG[cc]
                    hs = slice(cc * 2, cc * 2 + 2)

                    def z1(e, cc=cc, cur=cur, gp=gp):
                        for hh in range(2):
                            h = cc * 2 + hh
                            e.matmul(gp[:, hh, 0, :], lhsT=UoffT[:, h, 0, :], rhs=AA[cur][:, h, 1, :], start=True, stop=True)
                            ins = e.matmul(gp[:, hh, 1, :], lhsT=UoffT[:, h, 1, :], rhs=AB[cur][:, h, 1, :], start=True, stop=True)
                        return ins
                    P.op('pe', z1, reads=['UoffT', f'AA{cur}', f'AB{cur}'], writes=[f'bpG{cc}'])
                    P.op('act', lambda e, hs=hs, gp=gp: e.activation(out=Zs[:, hs, :, :], in_=gp[:], func=AF.Copy),
                         reads=[f'bpG{cc}'], writes=['Zs'])

                    def t1(e, cc=cc, cur=cur, gp=gp):
                        for hh in range(2):
                            h = cc * 2 + hh
                            e.matmul(gp[:, hh, 0, :], lhsT=AB[cur][:, h, 1, :], rhs=Zs[:, h, 0, :], start=True, stop=True)
                            ins = e.matmul(gp[:, hh, 1, :], lhsT=AA[cur][:, h, 1, :], rhs=Zs[:, h, 1, :], start=True, stop=True)
                        return ins
                    P.op('pe', t1, reads=['Zs', f'AA{cur}', f'AB{cur}'], writes=[f'bpG{cc}'])
                    P.op('dve', lambda e, hs=hs, cur=cur, gp=gp: e.tensor_tensor(
                        out=T32[:, hs, 0, :], in0=gp[:, :, 0, :], in1=AA[cur][:, hs, 1, :], op=ALU.add),
                        reads=[f'bpG{cc}', f'AA{cur}'], writes=['T32'])
                    P.op('dve', lambda e, hs=hs, cur=cur, gp=gp: e.tensor_tensor(
                        out=T32[:, hs, 1, :], in0=gp[:, :, 1, :], in1=AB[cur][:, hs, 1, :], op=ALU.add),
                        reads=[f'bpG{cc}', f'AB{cur}'], writes=['T32'])

                    def z2(e, cc=cc, gp=gp):
                        for hh in range(2):
                            h = cc * 2 + hh
                            ins = e.matmul(gp[:, hh, 0, :], lhsT=UoffT[:, h, 2, :], rhs=T32[:, h, 0, :], start=True, stop=True)
                        return ins
                    P.op('pe', z2, reads=['UoffT', 'T32'], writes=[f'bpG{cc}'])
                    P.op('act', lambda e, hs=hs, gp=gp: e.activation(out=Zs[:, hs, 0, :], in_=gp[:, :, 0, :], func=AF.Copy),
                         reads=[f'bpG{cc}'], writes=['Zs'])

                    def t2(e, cc=cc, gp=gp):
                        for hh in range(2):
                            h = cc * 2 + hh
                            ins = e.matmul(gp[:, hh, 1, :], lhsT=T32[:, h, 1, :], rhs=Zs[:, h, 0, :], start=True, stop=True)
                        return ins
                    P.op('pe', t2, reads=['Zs', 'T32'], writes=[f'bpG{cc}'])
                    P.op('dve', lambda e, hs=hs, gp=gp: e.tensor_tensor(
                        out=Tfin[:, hs, :], in0=gp[:, :, 1, :], in1=T32[:, hs, 0, :], op=ALU.add),
                        reads=[f'bpG{cc}', 'T32'], writes=['Tfin'])
                def xm(e, tt=tt):
                    for h in range(4):
                        ins = e.matmul(pX[:, h, 0, :], lhsT=U2[:, h, 0, :], rhs=vtok[:, tt, h * 64:(h + 1) * 64], start=True, stop=True)
                    return ins
                P.op('pe', xm, reads=['U2', 'vtok'], writes=['bpX'])
                P.op('act', lambda e: e.activation(out=XA[:, :, 0, :], in_=pX[:, :, 0, :], func=AF.Copy), reads=['bpX'], writes=['XA'])

                def wm(e):
                    for h in range(4):
                        ins = e.matmul(pX[:, h, :, :], lhsT=Tfin[:, h, :], rhs=XA[:, h, :, :], start=True, stop=True)
                    return ins
                P.op('pe', wm, reads=['Tfin', 'XA'], writes=['bpX'])
                P.op('act', lambda e: e.activation(out=WA[:].rearrange("p a h k -> p h a k"), in_=pX, func=AF.Copy), reads=['bpX'], writes=['WA'])

                def rm(e):
                    for cc in range(2):
                        for hh in range(2):
                            h = cc * 2 + hh
                            hp = hh * 64
                            ins = e.matmul(pG[0][hp:hp + 64, cc, 0, :], lhsT=WA[:, 1, h, :], rhs=U1[:, h, 1, :], start=True, stop=True)
                    return ins
                P.op('pe', rm, reads=['WA', 'U1'], writes=['bpG0'])
                P.op('dve', lambda e, cs=cs: e.tensor_tensor(out=RtT[:], in0=pG[0][:, :, 0, :], in1=arT[:, :, 1, cs], op=ALU.add),
                     reads=['bpG0', 'arT'], writes=['RtT'])
                for c in range(2):
                    pc = slice(c * 64, (c + 1) * 64)
                    pMc = pG[1 - c][:].rearrange("p a b t -> p (a b) t")

                    def mmm(e, c=c, pc=pc, pMc=pMc):
                        for cc in range(2):
                            ins = e.matmul(pMc[:, cc, :], lhsT=WA[pc, 1, cc * 2:cc * 2 + 2, :].rearrange("p h k -> p (h k)"),
                                           rhs=tk[pc, 1, cc * 128:(cc + 1) * 128], start=True, stop=True)
                        return ins
                    P.op('pe', mmm, reads=['WA', 'tk'], writes=[f'bpG{1 - c}'])
                    for cc in range(2):
                        P.op('dve', lambda e, c=c, cc=cc, tt=tt, pMc=pMc: e.scalar_tensor_tensor(
                            out=Msb[:, cc, c, :], in0=ident[:], scalar=gC[:, cc, tt * 2 + c:tt * 2 + c + 1], in1=pMc[:, cc, :],
                            op0=ALU.mult, op1=ALU.add), reads=[f'bpG{1 - c}', 'gC', 'ident'], writes=['Msb'])
                if not is_ctx:
                    def y0m(e, tt=tt):
                        for h in range(4):
                            e.matmul(pY[:, h, :], lhsT=U1[:, h, 1, :], rhs=WA[:, 0, h, :], start=(h == 0), stop=False, skip_group_check=True)
                            ins = e.matmul(pY[:, h, :], lhsT=U2[:, h, 1, :], rhs=vtok[:, tt, h * 64:(h + 1) * 64], start=False, stop=False,
                                           skip_group_check=True)
                        return ins
                    P.op('pe', y0m, reads=['U1', 'U2', 'WA', 'vtok'], writes=['bpY'])
                for ci, c in enumerate(corder):
                    pc = slice(c * 64, (c + 1) * 64)
                    if not is_ctx:
                        def ycm(e, pc=pc, ci=ci):
                            for cc in range(2):
                                ins = e.matmul(pYf[pc, cc * 128:(cc + 1) * 128], lhsT=RtT[:, cc, pc], rhs=Sbd[:, cc, :], start=False,
                                               stop=(ci == 1), skip_group_check=True)
                            return ins
                        P.op('pe', ycm, reads=['RtT', 'Sbd'], writes=['bpY'])

                    def scm(e, pc=pc, c=c, tt=tt):
                        for cc in range(2):
                            o_ = pSf[:, cc * 128:(cc + 1) * 128]
                            e.matmul(o_, lhsT=tk[pc, 1, cc * 128:(cc + 1) * 128], rhs=WA[pc, 0, cc * 2:cc * 2 + 2, :].rearrange("p h k -> p (h k)"),
                                     start=(cc == 0), stop=False, skip_group_check=True)
                            e.matmul(o_, lhsT=tk[pc, 2, cc * 128:(cc + 1) * 128], rhs=vtok[pc, tt, cc * 128:(cc + 1) * 128],
                                     start=False, stop=False, skip_group_check=True)
                            ins = e.matmul(o_, lhsT=Msb[:, cc, c, :], rhs=Sbd[:, cc, :], start=False, stop=True, skip_group_check=True)
                        return ins
                    P.op('pe', scm, reads=['tk', 'WA', 'vtok', 'Msb', 'Sbd'], writes=['bpS'])
                    pS3 = pSf[:, 0:256].rearrange("p (c x) -> p c x", c=2)
                    P.op('act', lambda e, pS3=pS3: e.activation(out=Sbd[0:64, :, 0:64], in_=pS3[0:64, :, 0:64], func=AF.Copy),
                         reads=['bpS'], writes=['Sbd'])
                    P.op('dve', lambda e, pS3=pS3: e.tensor_copy(out=Sbd[64:128, :, 64:128], in_=pS3[64:128, :, 64:128]),
                         reads=['bpS'], writes=['Sbd'])
                if is_ctx:
                    continue
                row0 = tok0 - 256 + tt * 128
                if not final:
                    P.op('dve', lambda e: e.tensor_copy(out=ysb[:], in_=pYf[:, 0:256]), reads=['bpY'], writes=['ysb'])
                    P.dma('sp', lambda e, row0=row0: e.dma_start(out=T['y0'][row0:row0 + 128, :], in_=ysb[:]), reads=['ysb'], writes=['y0'])
                    P.dma('sp', lambda e, row0=row0, tt=tt: e.dma_start(out=T['bon0'][row0:row0 + 128, :], in_=bon[:, tt, :]), reads=['bon'], writes=['bon0'])
                else:
                    P.dma('sp', lambda e, row0=row0: e.dma_start(out=y0t[:], in_=T['y0'][row0:row0 + 128, :]), reads=['y0'], writes=['y0t'])
                    P.dma('sp', lambda e, row0=row0: e.dma_start(out=b0t[:], in_=T['bon0'][row0:row0 + 128, :]), reads=['bon0'], writes=['b0t'])
                    P.op('dve', lambda e: e.tensor_tensor(out=ysb[:], in0=pYf[:, 0:256], in1=y0t[:], op=ALU.add),
                         reads=['bpY', 'y0t'], writes=['ysb'])
                    P.op('dve', lambda e, tt=tt: e.tensor_tensor(out=b0t[:], in0=b0t[:], in1=bon[:, tt, :], op=ALU.add), reads=['b0t', 'bon'], writes=['b0t'])
                    y3 = ysb[:].rearrange("p (h v) -> p h v", h=4)
                    P.op('dve', lambda e: e.tensor_reduce(out=st8[:, 0:4], in_=y3, axis=AX.X, op=ALU.add), reads=['ysb'], writes=['st8'])
                    P.op('dve', lambda e: e.tensor_scalar(out=st8[:, 0:4], in0=st8[:, 0:4], scalar1=1.0 / 64, scalar2=None, op0=ALU.mult),
                         reads=['st8'], writes=['st8'])
                    P.op('dve', lambda e: e.tensor_tensor(out=y3, in0=y3, in1=st8[:, 0:4].unsqueeze(2).broadcast_to([128, 4, 64]), op=ALU.subtract),
                         reads=['ysb', 'st8'], writes=['ysb'])
                    P.op('dve', lambda e: e.tensor_tensor(out=osb[:], in0=ysb[:], in1=ysb[:], op=ALU.mult), reads=['ysb'], writes=['osb'])
                    P.op('dve', lambda e: e.tensor_reduce(out=st8[:, 4:8], in_=osb[:].rearrange("p (h v) -> p h v", h=4), axis=AX.X, op=ALU.add),
                         reads=['osb'], writes=['st8'])
                    P.op('act', lambda e: e.activation(out=st8[:, 4:8], in_=st8[:, 4:8], func=AF.Ln, scale=1.0 / 64, bias=64e-5), reads=['st8'], writes=['st8'])
                    P.op('act', lambda e: e.activation(out=st8[:, 4:8], in_=st8[:, 4:8], func=AF.Exp, scale=-0.5), reads=['st8'], writes=['st8'])
                    P.op('dve', lambda e: e.tensor_tensor(out=y3, in0=y3, in1=st8[:, 4:8].unsqueeze(2).broadcast_to([128, 4, 64]), op=ALU.mult),
                         reads=['ysb', 'st8'], writes=['ysb'])
                    P.op('dve', lambda e: e.tensor_tensor(out=ysb[:], in0=ysb[:], in1=lnrep[:, 0, :], op=ALU.mult), reads=['ysb', 'lnrep'], writes=['ysb'])
                    P.op('dve', lambda e: e.tensor_tensor(out=ysb[:], in0=ysb[:], in1=lnrep[:, 1, :], op=ALU.add), reads=['ysb', 'lnrep'], writes=['ysb'])
                    P.op('dve', lambda e, tt=tt: e.tensor_tensor(
                        out=osb[:].rearrange("p (h v) -> p h v", h=4), in0=vtok[:, tt, :].rearrange("p (h v) -> p h v", h=4),
                        in1=b0t[:, 0:4].unsqueeze(2).broadcast_to([128, 4, 64]), op=ALU.mult), reads=['vtok', 'b0t'], writes=['osb'])
                    P.op('dve', lambda e: e.tensor_tensor(out=osb[:], in0=osb[:], in1=ysb[:], op=ALU.add), reads=['osb', 'ysb'], writes=['osb'])
                    P.op('dve', lambda e, tt=tt: e.tensor_tensor(out=osb[:], in0=osb[:], in1=gtok[:, tt, :], op=ALU.mult), reads=['osb', 'gtok'], writes=['osb'])
                    P.dma('sp', lambda e, row0=row0: e.dma_start(out=T['og'][row0:row0 + 128, :], in_=osb[:]), reads=['osb'], writes=['og'])

        for d in range(2):
            P.op('dve', lambda e: e.memset(Sbd[:], 0.0), writes=['Sbd'])
            do_segment(0, 256, True, False, False, d, d == 1)
            segs = list(range(NL)) if d == 0 else list(range(NL - 1, -1, -1))
            for si in segs:
                do_segment(256 + si * 512, 512, False, si > 0, si < NL - 1, d, d == 1)
        P.barrier()


def build_B(NL):
    nc = bass.Bass("TRN2", target_bir_lowering=False)
    def din(name, shape):
        return nc.dram_tensor(name, shape, F32, kind="ExternalInput").ap()
    T = {}
    NTOK = 256 + NL * 512
    T['xs'] = din("xs", [NTOK, 1024])
    T['xmixT'] = din("xmixT", [128, 8, 6])
    T['wrkv'] = din("wrkv", [1024, 768])
    T['w1cat'] = din("w1cat", [1024, 128])
    T['a1cat'] = din("a1cat", [1024, 128])
    T['g1'] = din("g1", [1024, 160])
    T['w2cat'] = din("w2cat", [128, 256])
    T['a2cat'] = din("a2cat", [128, 256])
    T['g2a'] = din("g2a", [128, 256])
    T['g2b'] = din("g2b", [32, 256])
    T['colvec'] = din("colvec", [128, 2, 6])
    T['rkw'] = din("rkw", [128, 2, 2])
    T['lnrep'] = din("lnrep", [128, 2, 256])
    T['mk'] = din("mk", [10, 128, 128])
    cT = din("cT", [128, 8, 2])
    ada_w = din("ada_w", [1024, 6144])
    ada_bT = din("ada_bT", [128, 48])
    adab_rep = din("adab_rep", [128, 2, 1024])
    gmixT = din("gmixT", [128, 8])
    gffnT = din("gffnT", [128, 8])
    dout = lambda name, shape: nc.dram_tensor(name, shape, F32, kind="ExternalOutput").ap()
    T['y0'] = dout("y0", [NL * 512, 256])
    T['bon0'] = dout("bon0", [NL * 512, 4])
    T['og'] = dout("og", [NL * 512, 256])
    T['gdram'] = dout("gdram", [2, 128, 2, 1024])
    P = Prog(nc)
    P.psum_keys = PSUM_KEYS
    with ExitStack() as st:
        sb = _sb(nc, st)
        C = make_consts(P, nc, sb)
        M = mod_setup(P, nc, st, C, cT, ada_w, ada_bT, adab_rep, gmixT, gffnT, T['gdram'])
        rwkv_phase(P, nc, C, M, T, NL)
        P.final_wait('sp')
        P.emit(st)
    print("B ops:", P.nops, {e: len(P.streams[e]) for e in P.ENG})
    return nc


def host_B(inp, xs_b, core):
    b, hg = core // 4, core % 4
    cols = slice(hg * 256, (hg + 1) * 256)
    m = {}
    m['xs'] = xs_b
    m['xmixT'] = inp['rwkv_x_mix'][0].reshape(6, 8, 128).transpose(2, 1, 0)
    m['wrkv'] = np.concatenate([inp['rwkv_w_r'][0][:, cols], inp['rwkv_w_k'][0][:, cols], inp['rwkv_w_v'][0][:, cols]], 1)
    m['w1cat'] = np.concatenate([inp['rwkv_decay_w1'][0, 0], inp['rwkv_decay_w1'][0, 1]], 1)
    m['a1cat'] = np.concatenate([inp['rwkv_iclr_a1'][0, 0], inp['rwkv_iclr_a1'][0, 1]], 1)
    m['g1'] = inp['rwkv_gate_g1'][0]
    m['w2cat'] = np.concatenate([inp['rwkv_decay_w2'][0, 0][:, cols], inp['rwkv_decay_w2'][0, 1][:, cols]], 0)
    m['a2cat'] = np.concatenate([inp['rwkv_iclr_a2'][0, 0][:, cols], inp['rwkv_iclr_a2'][0, 1][:, cols]], 0)
    g2 = inp['rwkv_gate_g2'][0][:, cols]
    m['g2a'] = g2[0:128]
    m['g2b'] = g2[128:160]
    vecs = [inp['rwkv_decay_w0'][0, 0], inp['rwkv_decay_w0'][0, 1], inp['rwkv_iclr_a0'][0, 0], inp['rwkv_iclr_a0'][0, 1],
            inp['rwkv_k_k'][0], inp['rwkv_k_a'][0]]
    cv = np.stack([v[cols].reshape(2, 128) for v in vecs], 2)
    m['colvec'] = cv.transpose(1, 0, 2)
    rkw = np.zeros((128, 2, 2), np.float32)
    for cc in range(2):
        for hh in range(2):
            rkw[hh * 64:(hh + 1) * 64, cc, hh] = inp['rwkv_r_k'][0][hg * 4 + cc * 2 + hh]
    m['rkw'] = rkw
    m['lnrep'] = np.broadcast_to(np.stack([inp['rwkv_ln_g'][0][cols], inp['rwkv_ln_b'][0][cols]])[None], (128, 2, 256))
    r = np.arange(128)[:, None]
    c = np.arange(128)[None, :]
    b64 = (r // 64) == (c // 64)
    b32 = (r // 32) == (c // 32)
    b16 = (r // 16) == (c // 16)
    S64 = (r < c) & b64
    I64 = (r <= c) & b64
    S16 = (r < c) & b16
    O1 = (r < c) & b32 & ~b16
    O2 = (r < c) & b64 & ~b32
    m['mk'] = np.stack([S64, I64, S64.T, I64.T, S16, S16.T, O1, O1.T, O2, O2.T]).astype(np.float32)
    m.update(host_mod(inp, b, 1))
    return {k_: np.ascontiguousarray(v, dtype=np.float32) for k_, v in m.items()}


def wo_phase(P, nc, C, T, NT):
    with ExitStack() as st:
        sb = _sb(nc, st)
        ps = _ps(nc, st)
        wo = sb("wo", [128, 8, 1024], BF16)
        stg = [sb(f"cstg{i}", [128, 1024], F32) for i in range(2)]
        g2rep = sb("cg2rep", [128, 1024], F32)
        ot32 = [sb(f"ot32_{i}", [128, 8, 128], F32) for i in range(2)]
        otb = [sb(f"otb_{i}", [128, 8, 128], BF16) for i in range(2)]
        xt = [sb(f"cxt{i}", [128, 1024], F32) for i in range(2)]
        ty = [sb(f"cty{i}", [128, 1024], F32) for i in range(2)]
        pA = [ps(f"cpA{i}", [128, 512], F32) for i in range(2)]
        P.dma('sp', lambda e: e.dma_start(out=g2rep[:], in_=T['gdram'][0, :, 0, :]), reads=['g_dram'], writes=['cg2rep'])
        wsrc = T['wo'].rearrange("(c p) n -> p c n", p=128)
        for k in range(8):
            P.dma('sp', lambda e, k=k: e.dma_start(out=stg[k % 2][:], in_=wsrc[:, k, :]), writes=[f'cstg{k % 2}'])
            P.op('pool', lambda e, k=k: e.tensor_copy(out=wo[:, k, :], in_=stg[k % 2][:]), reads=[f'cstg{k % 2}'], writes=['wo'])
        osrc = T['oT'].rearrange("(c p) n -> p c n", p=128)
        for t in range(NT):
            i = t % 2
            P.dma('sp', lambda e, t=t, i=i: e.dma_start(out=ot32[i][:], in_=osrc[:, :, t * 128:(t + 1) * 128]), writes=[f'ot32_{i}'])
            P.dma('sp', lambda e, t=t, i=i: e.dma_start(out=xt[i][:], in_=T['xin'][t * 128:(t + 1) * 128, :]), writes=[f'cxt{i}'])
            P.op('pool', lambda e, i=i: e.tensor_copy(out=otb[i][:], in_=ot32[i][:]), reads=[f'ot32_{i}'], writes=[f'otb_{i}'])
            for half in range(2):
                pa = pA[half]

                def om(e, i=i, half=half, pa=pa):
                    for k in range(8):
                        ins = e.matmul(pa[:], lhsT=otb[i][:, k, :], rhs=wo[:, k, half * 512:(half + 1) * 512], start=(k == 0), stop=(k == 7))
                    return ins
                P.op('pe', om, reads=[f'otb_{i}', 'wo'], writes=[f'cpA{half}'])
                P.op('dve', lambda e, i=i, half=half, pa=pa: e.tensor_tensor(
                    out=ty[i][:, half * 512:(half + 1) * 512], in0=pa[:], in1=g2rep[:, half * 512:(half + 1) * 512], op=ALU.mult),
                    reads=[f'cpA{half}', 'cg2rep'], writes=[f'cty{i}'])
            P.op('dve', lambda e, i=i: e.tensor_tensor(out=ty[i][:], in0=ty[i][:], in1=xt[i][:], op=ALU.add),
                 reads=[f'cty{i}', f'cxt{i}'], writes=[f'cty{i}'])
            P.dma('sp', lambda e, t=t, i=i: e.dma_start(out=T['x3'][t * 128:(t + 1) * 128, :], in_=ty[i][:]), reads=[f'cty{i}'], writes=['x3'])
        P.barrier()


def build_C(NT, n_exp=16):
    nc = bass.Bass("TRN2", target_bir_lowering=False)
    def din(name, shape):
        return nc.dram_tensor(name, shape, F32, kind="ExternalInput").ap()
    dout = lambda name, shape: nc.dram_tensor(name, shape, F32, kind="ExternalOutput").ap()
    T = {}
    T['oT'] = din("oT", [1024, NT * 128])
    T['wo'] = din("wo", [1024, 1024])
    T['xin'] = din("xin", [NT * 128, 1024])
    cT = din("cT", [128, 8, 2])
    ada_w = din("ada_w", [1024, 6144])
    ada_bT = din("ada_bT", [128, 48])
    adab_rep = din("adab_rep", [128, 2, 1024])
    gmixT = din("gmixT", [128, 8])
    gffnT = din("gffnT", [128, 8])
    T['rw'] = din("rw", [128, 8, 16])
    T['rbias'] = din("rbias", [128, 16])
    T['w1'] = din("w1", [n_exp, 1024, 1024])
    T['w3'] = din("w3", [n_exp, 1024, 1024])
    T['w2'] = din("w2", [n_exp, 1024, 1024])
    T['fng'] = din("fng", [128, 1024])
    T['x3'] = dout("x3", [NT * 128, 1024])
    T['out'] = dout("out", [NT * 128, 1024])
    T['gdram'] = dout("gdram", [2, 128, 2, 1024])
    P = Prog(nc)
    P.psum_keys = PSUM_KEYS
    with ExitStack() as st:
        sb = _sb(nc, st)
        C = make_consts(P, nc, sb)
        M = mod_setup(P, nc, st, C, cT, ada_w, ada_bT, adab_rep, gmixT, gffnT, T['gdram'])
        wo_phase(P, nc, C, T, NT)
        T['x_in'] = T['x3']
        T['x_out'] = T['out']
        moe_phase(P, nc, C, M, T, NT, lambda t: 0, "m", final_norm=True)
        P.final_wait('sp')
        P.emit(st)
    print("C ops:", P.nops, {e: len(P.streams[e]) for e in P.ENG})
    return nc


def host_C(inp, o_own, xl2_own, b):
    m = {}
    m['oT'] = o_own.T
    m['wo'] = inp['rwkv_w_o'][0]
    m['xin'] = xl2_own
    m['fng'] = np.broadcast_to(inp['final_norm_g'][None], (128, 1024))
    m.update(host_mod(inp, b, 1))
    m.update(host_moe(inp, 1))
    return {k_: np.ascontiguousarray(v, dtype=np.float32) for k_, v in m.items()}


def kernel(**inp):
    inp = {k: np.asarray(v) for k, v in inp.items()}
    S = 16384
    cfg = Cfg(S)
    OWN = cfg.OWN
    cores = list(range(8))
    two = [0, 1]
    ncA = build_A(cfg, phases=('attn',))
    mapsA = [host_A(inp, cfg, c, with_moe=False) for c in cores]
    resA = run_bass_kernel_spmd(ncA, mapsA, core_ids=cores).results
    x1 = [np.asarray(resA[c]['x1']) for c in cores]
    del mapsA, resA
    x1b = [np.concatenate([x1[b * 4][:256]] + [x1[b * 4 + q][256:] for q in range(4)], 0) for b in range(2)]
    NT2 = (256 + S) // 128
    ncA2 = build_A2(NT2)
    mapsA2 = [host_A2(inp, x1b[b], b) for b in two]
    resA2 = run_bass_kernel_spmd(ncA2, mapsA2, core_ids=two).results
    xs = [np.asarray(resA2[b]['x2']) for b in two]
    del mapsA2, resA2
    ncB = build_B(S // 512)
    mapsB = [host_B(inp, xs[c // 4], c) for c in cores]
    resB = run_bass_kernel_spmd(ncB, mapsB, core_ids=cores).results
    o = [np.concatenate([np.asarray(resB[b * 4 + hg]['og']) for hg in range(4)], 1) for b in range(2)]
    del mapsB, resB
    ncC = build_C(S // 128)
    mapsC = [host_C(inp, o[b], xs[b][256:], b) for b in two]
    resC = run_bass_kernel_spmd(ncC, mapsC, core_ids=two).results
    out = np.stack([np.asarray(resC[b]['out']) for b in two])
    return np.ascontiguousarray(out, dtype=np.float32)
```

```python
import numpy as np
from contextlib import ExitStack
import concourse.bass as bass
import concourse.mybir as mybir
from concourse.bass_utils import run_bass_kernel_spmd


F32 = mybir.dt.float32
BF16 = mybir.dt.bfloat16
I32 = mybir.dt.int32
U32 = mybir.dt.uint32
AF = mybir.ActivationFunctionType
ALU = mybir.AluOpType
AX = mybir.AxisListType

EPOCH = 20000
NDMA = 6


class Prog:
    ENG = ('pe', 'act', 'dve', 'pool', 'sp')

    def __init__(self, nc):
        self.nc = nc
        self.streams = {e: [] for e in self.ENG}
        self.cnt = {e: 0 for e in self.ENG}
        self.last_w = {}
        self.readers = {}
        self.known = {e: {} for e in self.ENG}
        self.dma_rr = {e: 0 for e in self.ENG}
        self.dma_cnt = {}
        self.extra = {e: [] for e in self.ENG}
        self.nops = 0
        self.psum_keys = set()

    def _deps(self, reads, writes):
        deps = []
        for k in reads:
            w = self.last_w.get(k)
            if w is not None:
                deps.append(w)
        for k in writes:
            w = self.last_w.get(k)
            if w is not None:
                deps.append(w)
            deps.extend(self.readers.get(k, ()))
        return deps

    def _resolve(self, eng, deps):
        need = {}
        for t in deps:
            if t[0] == 'c':
                if t[1] == 'pe' and eng == 'pe':
                    continue
                key = ('c', t[1])
                val = t[2]
            else:
                key = ('d', t[1], t[2])
                val = t[3]
            if val > need.get(key, -1):
                need[key] = val
        out = []
        kn = self.known[eng]
        for key, val in need.items():
            if kn.get(key, -1) >= val:
                continue
            kn[key] = val
            out.append((key, val))
        return out

    def _record(self, tok, reads, writes):
        for k in reads:
            self.readers.setdefault(k, []).append(tok)
        for k in writes:
            self.last_w[k] = tok
            self.readers[k] = []

    def op(self, eng, fn, reads=(), writes=()):
        pr = [k for k in reads if k in self.psum_keys]
        if pr:
            writes = list(writes) + pr
        deps = self._deps(reads, writes) + self.extra[eng]
        self.extra[eng] = []
        waits = self._resolve(eng, deps)
        idx = self.cnt[eng]
        self.cnt[eng] += 1
        tok = ('c', eng, idx)
        self.streams[eng].append((waits, fn, tok))
        self._record(tok, reads, writes)
        self.nops += 1
        return tok

    def dma(self, q, fn, reads=(), writes=()):
        k = self.dma_rr[q]
        self.dma_rr[q] = (k + 1) % NDMA
        m = self.dma_cnt.get((q, k), 0)
        deps = self._deps(reads, writes) + self.extra[q]
        self.extra[q] = []
        if m > 0:
            deps.append(('d', q, k, m - 1))
        waits = self._resolve(q, deps)
        self.dma_cnt[(q, k)] = m + 1
        tok = ('d', q, k, m)
        self.streams[q].append((waits, fn, tok))
        self._record(tok, reads, writes)
        self.nops += 1
        return tok

    def barrier(self):
        snap = []
        for e in self.ENG:
            if self.cnt[e] > 0:
                snap.append(('c', e, self.cnt[e] - 1))
        for (q, k), m in self.dma_cnt.items():
            snap.append(('d', q, k, m - 1))
        for e in self.ENG:
            self.extra[e] = self.extra[e] + snap

    def final_wait(self, eng='sp'):
        self.barrier()
        waits = self._resolve(eng, self.extra[eng])
        self.extra[eng] = []
        self.streams[eng].append((waits, None, None))

    def emit(self, stack):
        nc = self.nc
        csem = {}
        for e in self.ENG:
            ne = (self.cnt[e] + EPOCH - 1) // EPOCH
            for j in range(ne):
                csem[(e, j)] = stack.enter_context(nc.semaphore(f"c_{e}_{j}"))
        dsem = {}
        for (q, k) in self.dma_cnt:
            dsem[(q, k)] = stack.enter_context(nc.semaphore(f"d_{q}_{k}"))
        block = stack.enter_context(nc.Block())

        def run(ename, engine):
            for waits, fn, tok in self.streams[ename]:
                for key, val in waits:
                    if key[0] == 'c':
                        engine.wait_ge(csem[(key[1], val // EPOCH)], val % EPOCH + 1)
                    else:
                        engine.wait_ge(dsem[(key[1], key[2])], 16 * (val + 1))
                if fn is None:
                    continue
                ins = fn(engine)
                if tok[0] == 'c':
                    ins.then_inc(csem[(tok[1], tok[2] // EPOCH)], 1)
                else:
                    ins.then_inc(dsem[(tok[1], tok[2])], 16)

        @block.sync
        def _(e):
            run('sp', e)

        @block.tensor
        def _(e):
            run('pe', e)

        @block.scalar
        def _(e):
            run('act', e)

        @block.vector
        def _(e):
            run('dve', e)

        @block.gpsimd
        def _(e):
            run('pool', e)


D = 1024
RMS_EPS = 1e-6


class StopBuild(Exception):
    pass

STOP = [10 ** 9]

def CHK(n):
    if n >= STOP[0]:
        raise StopBuild()


class Scope:
    def __enter__(self):
        self.st = ExitStack()
        self.st.__enter__()
        self.stopped = False
        return self

    def __exit__(self, et, ev, tb):
        if et is StopBuild:
            self.st.__exit__(None, None, None)
            self.stopped = True
            return True
        return self.st.__exit__(et, ev, tb)


class Cfg:
    def __init__(self, S):
        self.S = S
        self.OWN = S // 4
        self.TO = 256 + 128 + self.OWN + 128
        self.TR = S - self.OWN
        assert self.TO % 512 == 0 and self.TR % 512 == 0
        self.NSO = self.TO // 512
        self.NSR = self.TR // 512
        self.NTO = self.TO // 128
        self.NKT = (self.TO + self.TR) // 128
        self.TQ = 256 + self.OWN
        self.NTQ = self.TQ // 128


def _sb(nc, st):
    return lambda name, shape, dt: st.enter_context(nc.sbuf_tensor("s_" + name, shape, dt))


PSUM_KEYS = set()


def _ps(nc, st):
    def f(name, shape, dt):
        PSUM_KEYS.add(name)
        return st.enter_context(nc.psum_tensor("p_" + name, shape, dt))
    return f


def make_consts(P, nc, sb):
    C = {}
    C['ident'] = sb("ident", [128, 128], F32)
    C['identb'] = sb("identb", [128, 128], BF16)
    C['bones'] = sb("bones", [128, 128], BF16)
    C['ones'] = sb("ones", [128, 128], F32)
    i32 = C['ident']
    P.op('pool', lambda e: e.memset(i32[:], 0.0), writes=['ident'])
    P.op('pool', lambda e: e.affine_select(out=i32[:], in_=i32[:], pattern=[[-1, 128]], base=0,
                                           channel_multiplier=1, compare_op=ALU.not_equal, fill=1.0),
         reads=['ident'], writes=['ident'])
    P.op('pool', lambda e: e.tensor_copy(out=C['identb'][:], in_=i32[:]), reads=['ident'], writes=['identb'])
    P.op('pool', lambda e: e.memset(C['ones'][:], 1.0), writes=['ones'])
    bo = C['bones']
    P.op('pool', lambda e: e.memset(bo[:], 0.0), writes=['bones'])
    P.op('pool', lambda e: e.memset(bo[0:64, 0:64], 1.0), reads=['bones'], writes=['bones'])
    P.op('pool', lambda e: e.memset(bo[64:128, 64:128], 1.0), reads=['bones'], writes=['bones'])
    return C


def mod_setup(P, nc, st_outer, C, cT, ada_w, ada_bT, adab_rep, gmixT, gffnT, g5_dram, tagp=""):
    sbo = _sb(nc, st_outer)
    M = {}
    for n in ('Gm', 'Sm', 'Gf', 'Sf'):
        M[n] = sbo(tagp + n, [128, 8, 2], F32)
    with ExitStack() as st:
        sb = _sb(nc, st)
        ps = _ps(nc, st)
        scT = sb("scT", [128, 8, 2], F32)
        screp = sb("screp", [128, 8, 2, 128], F32)
        modT = sb("modT", [128, 48, 2], F32)
        abT = sb("abT", [128, 48], F32)
        gmix = sb("gmix", [128, 8], F32)
        gffn = sb("gffn", [128, 8], F32)
        abrep = sb("abrep", [128, 2, 1024], F32)
        g5t = sb("g5t", [128, 2, 1024], F32)
        g2t = sb("g2t", [128, 2, 1024], F32)
        wblk = [sb(f"wblk{i}", [128, 8, 512], F32) for i in range(2)]
        pmod_full = ps("pmod", [128, 512], F32)
        pmod = pmod_full[:, 0:96].rearrange("p (a b) -> p a b", b=2)
        pg = [ps(f"pg{i}", [128, 512], F32) for i in range(2)]
        P.dma('sp', lambda e: e.dma_start(out=scT[:], in_=cT[:, :, :]), writes=['scT'])
        P.dma('sp', lambda e: e.dma_start(out=abT[:], in_=ada_bT[:, :]), writes=['abT'])
        P.dma('sp', lambda e: e.dma_start(out=gmix[:], in_=gmixT[:, :]), writes=['gmix'])
        P.dma('sp', lambda e: e.dma_start(out=gffn[:], in_=gffnT[:, :]), writes=['gffn'])
        P.dma('sp', lambda e: e.dma_start(out=abrep[:], in_=adab_rep[:, :, :]), writes=['abrep'])
        P.op('act', lambda e: e.activation(out=scT[:], in_=scT[:], func=AF.Silu), reads=['scT'], writes=['scT'])
        for cl in range(2):
            P.op('dve', lambda e, cl=cl: e.tensor_copy(
                out=screp[:, :, cl, :], in_=scT[:, :, cl:cl + 1].broadcast_to([128, 8, 128])),
                reads=['scT'], writes=['screp'])
        for blk in range(12):
            wb = wblk[blk % 2]
            wk = f"wblk{blk % 2}"
            P.dma('sp', lambda e, wb=wb, blk=blk: e.dma_start(
                out=wb[:], in_=ada_w[:, blk * 512:(blk + 1) * 512].rearrange("(c p) n -> p c n", p=128)),
                writes=[wk])
            j = blk // 2
            half = blk % 2

            def fm(e, wb=wb, j=j, half=half):
                for fc in range(4):
                    for kc in range(8):
                        ins = e.matmul(pmod[:, j * 8 + half * 4 + fc, :], lhsT=wb[:, kc, fc * 128:(fc + 1) * 128],
                                       rhs=scT[:, kc, :], start=(kc == 0), stop=(kc == 7))
                return ins
            P.op('pe', fm, reads=[wk, 'scT'], writes=['pmod'])
            if j in (2, 5):
                gi = 0 if j == 2 else 1
                dst = g2t if j == 2 else g5t
                for cl in range(2):
                    pgt = pg[cl]

                    def rm(e, wb=wb, cl=cl, pgt=pgt):
                        for kc in range(8):
                            ins = e.matmul(pgt[:], lhsT=screp[:, kc, cl, :], rhs=wb[:, kc, :],
                                           start=(kc == 0), stop=(kc == 7))
                        return ins
                    P.op('pe', rm, reads=[wk, 'screp'], writes=[f'pg{cl}'])
                    P.op('dve', lambda e, pgt=pgt, dst=dst, cl=cl, gi=gi, half=half: e.tensor_tensor(
                        out=dst[:, cl, half * 512:(half + 1) * 512], in0=pgt[:],
                        in1=abrep[:, gi, half * 512:(half + 1) * 512], op=ALU.add),
                        reads=[f'pg{cl}', 'abrep'], writes=['grep'])
        P.dma('sp', lambda e: e.dma_start(out=g5_dram[1, :, :, :], in_=g5t[:]), reads=['grep'], writes=['g_dram'])
        P.dma('sp', lambda e: e.dma_start(out=g5_dram[0, :, :, :], in_=g2t[:]), reads=['grep'], writes=['g_dram'])
        P.op('dve', lambda e: e.tensor_tensor(out=modT[:], in0=pmod[:],
                                              in1=abT[:, :].unsqueeze(2).broadcast_to([128, 48, 2]), op=ALU.add),
             reads=['pmod', 'abT'], writes=['modT'])
        for (Gn, Sn, gg, jsh, jsc) in (('Gm', 'Sm', gmix, 0, 1), ('Gf', 'Sf', gffn, 3, 4)):
            Gt = M[Gn]
            St = M[Sn]
            P.op('dve', lambda e, Gt=Gt, jsc=jsc: e.tensor_scalar(
                out=Gt[:], in0=modT[:, jsc * 8:(jsc + 1) * 8, :], scalar1=1.0, scalar2=None, op0=ALU.add),
                reads=['modT'], writes=[tagp + Gn])
            P.op('dve', lambda e, Gt=Gt, gg=gg: e.tensor_tensor(
                out=Gt[:], in0=Gt[:], in1=gg[:, :].unsqueeze(2).broadcast_to([128, 8, 2]), op=ALU.mult),
                reads=[tagp + Gn, 'gmix', 'gffn'], writes=[tagp + Gn])
            P.op('dve', lambda e, St=St, jsh=jsh: e.tensor_copy(out=St[:], in_=modT[:, jsh * 8:(jsh + 1) * 8, :]),
                 reads=['modT'], writes=[tagp + Sn])
        P.barrier()
    return M


class NormT:
    def __init__(self, P, nc, sb, ps, C, tag="n"):
        self.P = P
        self.C = C
        self.tag = tag
        self.junk = sb(tag + "junk", [128, 1024], BF16)
        self.ss = [sb(tag + f"ss{i}", [128, 1], F32) for i in range(2)]
        self.rstd = [sb(tag + f"rstd{i}", [128, 1], F32) for i in range(2)]
        self.xn = [sb(tag + f"xn{i}", [128, 1024], BF16) for i in range(2)]
        self.pT = ps(tag + "pT", [128, 8, 128], BF16)
        self.i = 0

    def run(self, xt, xkey, G, S, cls, hT_out, hkey, mkeys, npart=128, halo=None):
        P = self.P
        t = self.tag
        i = self.i % 2
        self.i += 1
        npq = npart
        ss, rstd, xn = self.ss[i], self.rstd[i], self.xn[i]
        junk = self.junk
        P.op('act', lambda e: e.activation(out=junk[0:npq, :], in_=xt, func=AF.Square, accum_out=ss[0:npq, :]),
             reads=[xkey], writes=[t + 'junk', t + f'ss{i}'])
        P.op('act', lambda e: e.activation(out=rstd[0:npq, :], in_=ss[0:npq, :], func=AF.Ln, scale=1.0 / 1024, bias=RMS_EPS),
             reads=[t + f'ss{i}'], writes=[t + f'rstd{i}'])
        P.op('act', lambda e: e.activation(out=rstd[0:npq, :], in_=rstd[0:npq, :], func=AF.Exp, scale=-0.5),
             reads=[t + f'rstd{i}'], writes=[t + f'rstd{i}'])
        P.op('dve', lambda e: e.tensor_scalar(out=xn[0:npq, :], in0=xt, scalar1=rstd[0:npq, :], scalar2=None, op0=ALU.mult),
             reads=[xkey, t + f'rstd{i}'], writes=[t + f'xn{i}'])
        pT = self.pT
        identb = self.C['identb']

        def tr(e):
            for c in range(8):
                ins = e.transpose(out=pT[:, c, 0:npq], in_=xn[0:npq, c * 128:(c + 1) * 128], identity=identb[0:npq, 0:npq])
            return ins
        P.op('pe', tr, reads=[t + f'xn{i}', 'identb'], writes=[t + 'pT'])
        if halo is not None:
            hTx, n, left_ok, right_ok = halo
            for (ok, src, dst) in ((left_ok, 0, 0), (right_ok, 1, n + 1)):
                if not ok:
                    continue
                P.op('dve', lambda e, src=src, dst=dst: e.tensor_tensor(
                    out=hTx[:, :, dst:dst + 1], in0=pT[:, :, src:src + 1], in1=G[:, :, cls:cls + 1], op=ALU.mult),
                    reads=[t + 'pT'] + mkeys, writes=[hkey])
                P.op('dve', lambda e, dst=dst: e.tensor_tensor(
                    out=hTx[:, :, dst:dst + 1], in0=hTx[:, :, dst:dst + 1], in1=S[:, :, cls:cls + 1], op=ALU.add),
                    reads=mkeys, writes=[hkey])
            return
        for c in range(8):
            if c % 2 == 0:
                P.op('act', lambda e, c=c: e.activation(out=hT_out(c), in_=pT[:, c, :], func=AF.Identity,
                                                        scale=G[:, c, cls:cls + 1], bias=S[:, c, cls:cls + 1]),
                     reads=[t + 'pT'] + mkeys, writes=[hkey])
            else:
                P.op('dve', lambda e, c=c: e.tensor_scalar(out=hT_out(c), in0=pT[:, c, :],
                                                           scalar1=G[:, c, cls:cls + 1], scalar2=S[:, c, cls:cls + 1],
                                                           op0=ALU.mult, op1=ALU.add),
                     reads=[t + 'pT'] + mkeys, writes=[hkey])


def load_cast(P, nc, stage, skeys, dst_ap_fn, src_ap_fn, nparts, dkey, engs=('pool', 'act')):
    for i in range(nparts):
        sbuf = stage[i % len(stage)]
        sk = skeys[i % len(stage)]
        d = dst_ap_fn(i)
        s = src_ap_fn(i)
        shp = d.shape
        sv = sbuf
        P.dma('sp', lambda e, sv=sv, s=s: e.dma_start(out=sv, in_=s), writes=[sk])
        eng = engs[i % len(engs)]
        if eng == 'act':
            P.op('act', lambda e, d=d, sv=sv: e.activation(out=d, in_=sv, func=AF.Copy), reads=[sk], writes=[dkey])
        else:
            P.op(eng, lambda e, d=d, sv=sv: e.tensor_copy(out=d, in_=sv), reads=[sk], writes=[dkey])


def attn_phase(P, nc, cfg, C, M, T):
    with Scope() as sc0:
        st = sc0.st
        sb = _sb(nc, st)
        ps = _ps(nc, st)
        NKT, NTO = cfg.NKT, cfg.NTO
        kbT = sb("kbT", [128, NKT * 128], BF16)
        vb = sb("vb", [128, NKT, 2, 65], BF16)
        kaT = sb("kaT", [128, NTO * 128], BF16)
        va = sb("va", [128, NTO, 2, 65], BF16)
        gqk = sb("gqk", [128, 4], F32)
        g2rep = sb("g2rep", [128, 2, 1024], F32)
        P.dma('sp', lambda e: e.dma_start(out=g2rep[:], in_=T['gdram'][0, :, :, :]), reads=['g_dram'], writes=['grep'])
        masks = sb("masks", [128, 4, 128], BF16)
        sinkrow = sb("sinkrow", [65, 8], F32)
        pm = sb("pm", [128, 128], BF16)
        xt2 = [sb(f"xt2_{i}", [128, 1024], F32) for i in range(2)]
        hT = sb("hT", [128, 8, 512], BF16)
        cos_t = sb("cos", [128, 512], F32)
        sin_t = sb("sin", [128, 512], F32)
        t1 = sb("t1", [128, 512], F32)
        t2 = sb("t2", [128, 512], F32)
        sq = sb("sq", [128, 512], BF16)
        qraw = sb("qraw", [128, 512], BF16)
        rs = sb("rs", [128, 512], F32)
        pA = ps("pA", [128, 512], F32)
        pB = ps("pB", [128, 512], F32)
        pC = ps("pC", [128, 512], F32)
        nrm = NormT(P, nc, sb, ps, C, "n")
        xcount = [0]

        def rope_chunk(mm, normed, gi, dst, dkey, wkeys):
            P.op('pe', lambda e: mm(e, pA), reads=wkeys + ['hT'], writes=['pA'])
            P.op('act', lambda e: e.activation(out=qraw[:], in_=pA[:], func=AF.Copy), reads=['pA'], writes=['qraw'])
            P.op('pe', lambda e: e.matmul(pB[:], lhsT=pm[:], rhs=qraw[:], start=True, stop=True),
                 reads=['qraw', 'pm'], writes=['pB'])
            if not normed:
                P.op('dve', lambda e: e.tensor_tensor(out=t1[:], in0=pA[:], in1=cos_t[:], op=ALU.mult),
                     reads=['pA', 'cos'], writes=['t1'])
                P.op('dve', lambda e: e.tensor_tensor(out=t2[:], in0=pB[:], in1=sin_t[:], op=ALU.mult),
                     reads=['pB', 'sin'], writes=['t2'])
                P.op('dve', lambda e: e.tensor_tensor(out=dst, in0=t1[:], in1=t2[:], op=ALU.add),
                     reads=['t1', 't2'], writes=[dkey])
            else:
                P.op('act', lambda e: e.activation(out=sq[:], in_=pA[:], func=AF.Square), reads=['pA'], writes=['sq'])
                P.op('pe', lambda e: e.matmul(pC[:], lhsT=C['bones'][:], rhs=sq[:], start=True, stop=True),
                     reads=['sq', 'bones'], writes=['pC'])
                P.op('act', lambda e: e.activation(out=rs[:], in_=pC[:], func=AF.Ln, scale=1.0 / 64, bias=RMS_EPS),
                     reads=['pC'], writes=['rs'])
                P.op('act', lambda e: e.activation(out=rs[:], in_=rs[:], func=AF.Exp, scale=-0.5),
                     reads=['rs'], writes=['rs'])
                P.op('dve', lambda e: e.scalar_tensor_tensor(out=t1[:], in0=pA[:], scalar=gqk[:, gi:gi + 1], in1=cos_t[:],
                                                             op0=ALU.mult, op1=ALU.mult),
                     reads=['pA', 'cos', 'gqk'], writes=['t1'])
                P.op('dve', lambda e: e.scalar_tensor_tensor(out=t2[:], in0=pB[:], scalar=gqk[:, gi + 1:gi + 2],
                                                             in1=sin_t[:], op0=ALU.mult, op1=ALU.mult),
                     reads=['pB', 'sin', 'gqk'], writes=['t2'])
                P.op('dve', lambda e: e.tensor_tensor(out=t1[:], in0=t1[:], in1=t2[:], op=ALU.add),
                     reads=['t1', 't2'], writes=['t1'])
                P.op('dve', lambda e: e.tensor_tensor(out=dst, in0=t1[:], in1=rs[:], op=ALU.mult),
                     reads=['t1', 'rs'], writes=[dkey])

        def mm8(wt, c0, n=128):
            def f(e, pt):
                for k in range(8):
                    ins = e.matmul(pt[:], lhsT=wt[:, k, c0:c0 + n], rhs=hT[:, k, :], start=(k == 0), stop=(k == 7))
                return ins
            return f

        def load_norm_supertile(xsrc, s, clsfn, cosd, sind):
            P.dma('sp', lambda e: e.dma_start(out=cos_t[:], in_=cosd[:, s * 512:(s + 1) * 512]), writes=['cos'])
            P.dma('sp', lambda e: e.dma_start(out=sin_t[:], in_=sind[:, s * 512:(s + 1) * 512]), writes=['sin'])
            for tt in range(4):
                ti = s * 4 + tt
                xi = xcount[0] % 2
                xcount[0] += 1
                xt = xt2[xi]
                P.dma('sp', lambda e, xt=xt, ti=ti: e.dma_start(out=xt[:], in_=xsrc[ti * 128:(ti + 1) * 128, :]),
                      writes=[f'xt2_{xi}'])
                cls = clsfn(ti)
                nrm.run(xt[:], f'xt2_{xi}', M['Gm'], M['Sm'], cls,
                        lambda c, tt=tt: hT[:, c, tt * 128:(tt + 1) * 128], 'hT', ['Gm', 'Sm'])

        with Scope() as sc1:
            st1 = sc1.st
            sb1 = _sb(nc, st1)
            stage = [sb1(f"stage{i}", [128, 2048], F32) for i in range(2)]
            skeys = ['stage0', 'stage1']
            wkv = sb1("wkv", [128, 8, 512], BF16)
            P.dma('sp', lambda e: e.dma_start(out=gqk[:], in_=T['gqk'][:, :]), writes=['gqk'])
            P.op('pool', lambda e: e.memset(vb[:], 1.0), writes=['vb'])
            P.op('pool', lambda e: e.memset(va[:], 1.0), writes=['va'])
            for mi in range(4):
                P.dma('sp', lambda e, mi=mi: e.dma_start(out=stage[mi % 2][:, 0:128], in_=T['masks'][mi, :, :]),
                      writes=[skeys[mi % 2]])
                P.op('dve', lambda e, mi=mi: e.tensor_copy(out=masks[:, mi, :], in_=stage[mi % 2][:, 0:128]),
                     reads=[skeys[mi % 2]], writes=['masks'])
            P.dma('sp', lambda e: e.dma_start(out=stage[0][:, 0:128], in_=T['pm'][:, :]), writes=['stage0'])
            P.op('dve', lambda e: e.tensor_copy(out=pm[:], in_=stage[0][:, 0:128]), reads=['stage0'], writes=['pm'])
            P.op('pool', lambda e: e.memset(sinkrow[:], 0.0), writes=['sinkrow'])
            P.dma('sp', lambda e: e.dma_start(out=sinkrow[64:65, :], in_=T['sink'][:, :]),
                  reads=['sinkrow'], writes=['sinkrow'])
            P.op('act', lambda e: e.activation(out=sinkrow[64:65, :], in_=sinkrow[64:65, :], func=AF.Exp),
                 reads=['sinkrow'], writes=['sinkrow'])
            wsrc = T['wkv'].rearrange("(c p) n -> p c n", p=128)
            for k in range(8):
                P.dma('sp', lambda e, k=k: e.dma_start(out=stage[k % 2][:, 0:512], in_=wsrc[:, k, :]),
                      writes=[skeys[k % 2]])
                P.op('pool', lambda e, k=k: e.tensor_copy(out=wkv[:, k, :], in_=stage[k % 2][:, 0:512]),
                     reads=[skeys[k % 2]], writes=['wkv'])
            CHK(1)
            for stream in range(2):
                ns = cfg.NSO if stream == 0 else cfg.NSR
                xsrc = T['xo'] if stream == 0 else T['xr']
                cosd = T['cosO'] if stream == 0 else T['cosR']
                sind = T['sinO'] if stream == 0 else T['sinR']
                tbase = 0 if stream == 0 else NTO
                for s in range(ns):
                    clsfn = (lambda ti: 1 if ti < 2 else 0) if stream == 0 else (lambda ti: 0)
                    load_norm_supertile(xsrc, s, clsfn, cosd, sind)
                    CHK(2)
                    tok0 = (tbase + s * 4) * 128
                    if stream == 0:
                        rope_chunk(mm8(wkv, 0), False, 0, kaT[:, s * 512:(s + 1) * 512], 'kaT', ['wkv'])
                        CHK(3)
                    rope_chunk(mm8(wkv, 128), True, 2, kbT[:, tok0:tok0 + 512], 'kbT', ['wkv'])
                    CHK(4)
                    for tt in range(4):
                        c0, n = (256, 256) if stream == 0 else (384, 128)

                        def vm(e, tt=tt, c0=c0, n=n):
                            for k in range(8):
                                ins = e.matmul(pA[:, 0:n], lhsT=hT[:, k, tt * 128:(tt + 1) * 128], rhs=wkv[:, k, c0:c0 + n],
                                               start=(k == 0), stop=(k == 7))
                            return ins
                        P.op('pe', vm, reads=['hT', 'wkv'], writes=['pA'])
                        if stream == 0:
                            P.op('act', lambda e, s=s, tt=tt: e.activation(
                                out=va[:, s * 4 + tt, :, 0:64], in_=pA[:, 0:128].rearrange("p (g d) -> p g d", g=2),
                                func=AF.Copy), reads=['pA'], writes=['va'])
                            P.op('dve', lambda e, s=s, tt=tt: e.tensor_copy(
                                out=vb[:, s * 4 + tt, :, 0:64], in_=pA[:, 128:256].rearrange("p (g d) -> p g d", g=2)),
                                reads=['pA'], writes=['vb'])
                        else:
                            P.op('act', lambda e, s=s, tt=tt, tbase=tbase: e.activation(
                                out=vb[:, tbase + s * 4 + tt, :, 0:64],
                                in_=pA[:, 0:128].rearrange("p (g d) -> p g d", g=2),
                                func=AF.Copy), reads=['pA'], writes=['vb'])
                    CHK(5)
            P.barrier()
            CHK(6)

        if sc1.stopped:
            raise StopBuild()
        with Scope() as sc2:
            st2 = sc2.st
            sb2 = _sb(nc, st2)
            ps2 = _ps(nc, st2)
            wq = sb2("wq", [128, 8, 1024], BF16)
            wout = sb2("wout", [128, 8, 1024], BF16)
            with ExitStack() as st2a:
                sb2a = _sb(nc, st2a)
                stageB = [sb2a(f"stageb{i}", [128, 2048], F32) for i in range(2)]
                skeysB = ['stageb0', 'stageb1']
                wsrcq = T['wq'].rearrange("(c p) n -> p c n", p=128)
                for k in range(8):
                    P.dma('sp', lambda e, k=k: e.dma_start(out=stageB[k % 2][:, 0:1024], in_=wsrcq[:, k, :]),
                          writes=[skeysB[k % 2]])
                    P.op('pool', lambda e, k=k: e.tensor_copy(out=wq[:, k, :], in_=stageB[k % 2][:, 0:1024]),
                         reads=[skeysB[k % 2]], writes=['wq'])
                for k in range(8):
                    P.dma('sp', lambda e, k=k: e.dma_start(out=stageB[k % 2][:, 0:1024], in_=T['wout'][:, k, :]),
                          writes=[skeysB[k % 2]])
                    P.op('pool', lambda e, k=k: e.tensor_copy(out=wout[:, k, :], in_=stageB[k % 2][:, 0:1024]),
                         reads=[skeysB[k % 2]], writes=['wout'])
                P.barrier()
            CHK(7)
            qT = sb2("qT", [128, 8, 512], BF16)
            o_all = sb2("o_all", [128, 8, 512], BF16)
            Pt = [sb2(f"Pt{i}", [128, 512], BF16) for i in range(3)]
            rden = sb2("rden", [65, 512], F32)
            bcs = sb2("bcs", [64, 512], F32)
            tmpy = sb2("tmpy", [128, 1024], F32)
            xres = sb2("xres", [128, 1024], F32)
            pS = [ps2(f"pS{i}", [128, 512], F32) for i in range(2)]
            pO = ps2("pO", [128, 512], F32)
            pBC = ps2("pBC", [128, 512], F32)
            P.op('pool', lambda e: e.memset(o_all[:], 0.0), writes=['o_all'])
            P.op('pool', lambda e: e.memset(rden[:], 1.0), writes=['rden'])
            pcount = [0]
            ones = C['ones']

            def v3(ap, three):
                return ap.rearrange("p (h q) -> p h q", h=4) if three else ap

            def attend(ktiles, kT, vt, kkey, vkey, g, rhs_fn, ncols, mask_fn, sink_g, out_fn, three):
                nk = len(ktiles)
                for ii, kt in enumerate(ktiles):
                    pi = pcount[0] % 2
                    bi = pcount[0] % 3
                    pcount[0] += 1
                    pSt = pS[pi]
                    Pb = Pt[bi]
                    P.op('pe', lambda e, kt=kt, pSt=pSt: e.matmul(
                        v3(pSt[:, 0:ncols], three), lhsT=kT[g * 64:(g + 1) * 64, kt * 128:(kt + 1) * 128], rhs=rhs_fn(),
                        start=True, stop=True), reads=[kkey, 'qT'], writes=[f'pS{pi}'])
                    P.op('act', lambda e, pSt=pSt, Pb=Pb: e.activation(out=Pb[:, 0:ncols], in_=pSt[:, 0:ncols],
                                                                        func=AF.Exp, scale=0.125),
                         reads=[f'pS{pi}'], writes=[f'Pt{bi}'])
                    mk = mask_fn(kt) if mask_fn is not None else None
                    if mk is not None:
                        P.op('dve', lambda e, Pb=Pb, mk=mk: e.tensor_tensor(
                            out=v3(Pb[:, 0:ncols], True), in0=v3(Pb[:, 0:ncols], True),
                            in1=masks[:, mk, :].unsqueeze(1).broadcast_to([128, 4, 128]),
                            op=ALU.mult), reads=[f'Pt{bi}', 'masks'], writes=[f'Pt{bi}'])
                    P.op('pe', lambda e, kt=kt, Pb=Pb, ii=ii: e.matmul(
                        pO[0:65, 0:ncols], lhsT=vt[:, kt, g, 0:65], rhs=Pb[:, 0:ncols], start=(ii == 0), stop=(ii == nk - 1)),
                        reads=[vkey, f'Pt{bi}'], writes=['pO'])
                if sink_g is not None:
                    P.op('dve', lambda e: e.tensor_tensor(
                        out=v3(rden[64:65, 0:ncols], True), in0=v3(pO[64:65, 0:ncols], True),
                        in1=sinkrow[64:65, sink_g * 4:(sink_g + 1) * 4].unsqueeze(2).broadcast_to([1, 4, 128]),
                        op=ALU.add), reads=['pO', 'sinkrow'], writes=['rden'])
                    P.op('dve', lambda e: e.reciprocal(out=rden[64:65, 0:ncols], in_=rden[64:65, 0:ncols]),
                         reads=['rden'], writes=['rden'])
                else:
                    P.op('dve', lambda e: e.reciprocal(out=rden[64:65, 0:ncols], in_=pO[64:65, 0:ncols]),
                         reads=['pO'], writes=['rden'])
                P.op('pe', lambda e: e.matmul(pBC[0:64, 0:ncols], lhsT=ones[64:65, 0:64], rhs=rden[64:65, 0:ncols],
                                              start=True, stop=True), reads=['rden', 'ones'], writes=['pBC'])
                P.op('act', lambda e: e.activation(out=bcs[:, 0:ncols], in_=pBC[0:64, 0:ncols], func=AF.Copy),
                     reads=['pBC'], writes=['bcs'])
                P.op('dve', lambda e: e.tensor_tensor(out=out_fn(), in0=v3(pO[0:64, 0:ncols], three),
                                                      in1=v3(bcs[:, 0:ncols], three),
                                                      op=ALU.mult), reads=['pO', 'bcs'], writes=['o_all'])

            last_own = NTO - 2
            allk = [kt for kt in range(NKT) if kt != 2 and kt != NTO - 1]
            for s in range(cfg.NSO):
                load_norm_supertile(T['xo'], s, (lambda ti: 1 if ti < 2 else 0), T['cosO'], T['sinO'])
                for c in range(8):
                    rope_chunk(mm8(wq, c * 128), c >= 4, 0, qT[:, c, :], 'qT', ['wq'])
                CHK(8)
                tiles = [s * 4 + tt for tt in range(4)]
                for tt, ti in enumerate(tiles):
                    if ti == 2 or ti == NTO - 1:
                        continue
                    if ti < 2:
                        kts = [0, 1]
                        mfn = None
                    else:
                        kts = [0, 1, ti - 1, ti, ti + 1]

                        def mfn(kt, ti=ti):
                            if kt == ti - 1:
                                return 0 if ti == 3 else 1
                            if kt == ti + 1:
                                return 3 if ti == last_own else 2
                            return None
                    for g in range(2):
                        attend(kts, kaT, va, 'kaT', 'va', g,
                               lambda g=g, tt=tt: qT[g * 64:(g + 1) * 64, 0:4, tt * 128:(tt + 1) * 128],
                               512, mfn, g,
                               lambda g=g, tt=tt: o_all[0:64, g * 4:(g + 1) * 4, tt * 128:(tt + 1) * 128], True)
                CHK(9)
                groups = []
                run0 = None
                for tt, ti in enumerate(tiles):
                    if ti < 2:
                        kind = 'c'
                    elif ti == 2 or ti == NTO - 1:
                        kind = None
                    else:
                        kind = 'o'
                    if run0 is not None and run0[0] == kind:
                        run0[2] += 128
                    else:
                        if run0 is not None and run0[0] is not None:
                            groups.append(tuple(run0))
                        run0 = [kind, tt * 128, 128]
                if run0 is not None and run0[0] is not None:
                    groups.append(tuple(run0))
                for (kind, c0, n) in groups:
                    kts = [0, 1] if kind == 'c' else allk
                    for hb in range(8):
                        g = hb // 4
                        j = hb % 4
                        attend(kts, kbT, vb, 'kbT', 'vb', g,
                               lambda g=g, j=j, c0=c0, n=n: qT[g * 64:(g + 1) * 64, 4 + j, c0:c0 + n],
                               n, None, None,
                               lambda hb=hb, c0=c0, n=n: o_all[64:128, hb, c0:c0 + n], False)
                CHK(10)
                for tt, ti in enumerate(tiles):
                    if ti == 2 or ti == NTO - 1:
                        continue
                    cls = 1 if ti < 2 else 0
                    row0 = ti * 128 if ti < 2 else 256 + (ti - 3) * 128
                    P.dma('sp', lambda e, ti=ti: e.dma_start(out=xres[:], in_=T['xo'][ti * 128:(ti + 1) * 128, :]),
                          writes=['xres'])
                    for half in range(2):
                        def om(e, tt=tt, half=half):
                            for hh in range(8):
                                ins = e.matmul(pA[:], lhsT=o_all[:, hh, tt * 128:(tt + 1) * 128],
                                               rhs=wout[:, hh, half * 512:(half + 1) * 512],
                                               start=(hh == 0), stop=(hh == 7))
                            return ins
                        P.op('pe', om, reads=['o_all', 'wout'], writes=['pA'])
                        P.op('dve', lambda e, half=half, cls=cls: e.tensor_tensor(
                            out=tmpy[:, half * 512:(half + 1) * 512], in0=pA[:],
                            in1=g2rep[:, cls, half * 512:(half + 1) * 512], op=ALU.mult),
                            reads=['pA', 'grep'], writes=['tmpy'])
                    P.op('dve', lambda e: e.tensor_tensor(out=tmpy[:], in0=tmpy[:], in1=xres[:], op=ALU.add),
                         reads=['tmpy', 'xres'], writes=['tmpy'])
                    P.dma('sp', lambda e, row0=row0: e.dma_start(out=T['x1'][row0:row0 + 128, :], in_=tmpy[:]),
                          reads=['tmpy'], writes=['x1'])
            P.barrier()
        if sc2.stopped:
            raise StopBuild()
    if sc0.stopped:
        raise StopBuild()


def moe_phase(P, nc, C, M, T, NT, cls_fn, tag="m", final_norm=False):
    GT = 8
    groups = [list(range(i, min(i + GT, NT))) for i in range(0, NT, GT)]
    with ExitStack() as st:
        sb = _sb(nc, st)
        ps = _ps(nc, st)
        wset = [[sb(f"{tag}w{j}_{i}", [128, 8, 1024], BF16) for j in range(3)] for i in range(2)]
        stage = [sb(f"{tag}stg{i}", [128, 2, 1024], F32) for i in range(2)]
        h2T = sb(tag + "h2T", [128, 8, GT * 128], BF16)
        acc = sb(tag + "acc", [128, GT, 1024], F32)
        gT = sb(tag + "gT", [128, 8, 512], BF16)
        sgt = sb(tag + "sgt", [128, 512], F32)
        xm = sb(tag + "xm", [128, 1024], F32)
        xn32 = sb(tag + "xn32", [128, 1024], F32)
        h32 = sb(tag + "h32", [128, 8, 128], F32)
        g5rep = sb(tag + "g5rep", [128, 2, 1024], F32)
        gate = sb(tag + "gate", [128, GT, 16], F32)
        rw = sb(tag + "rw", [128, 8, 16], F32)
        rbias = sb(tag + "rbias", [128, 16], F32)
        if final_norm:
            fng = sb(tag + "fng", [128, 1024], F32)
            P.dma('sp', lambda e: e.dma_start(out=fng[:], in_=T['fng'][:, :]), writes=[tag + 'fng'])
        junk = sb(tag + "junk", [128, 1024], BF16)
        sm = sb(tag + "sm", [128, 16], F32)
        r16 = [sb(tag + f"r16_{i}", [128, 16], F32) for i in range(5)]
        r4 = [sb(tag + f"r4_{i}", [128, 4], F32) for i in range(4)]
        pA = [ps(f"{tag}pA{i}", [128, 512], F32) for i in range(2)]
        pB = [ps(f"{tag}pB{i}", [128, 512], F32) for i in range(2)]
        pY = [ps(f"{tag}pY{i}", [128, 512], F32) for i in range(2)]
        pT = ps(tag + "pT32", [128, 8, 128], F32)
        K = lambda n: tag + n
        P.dma('sp', lambda e: e.dma_start(out=g5rep[:], in_=T['gdram'][1, :, :, :]), reads=['g_dram'], writes=[K('g5rep')])
        P.dma('sp', lambda e: e.dma_start(out=rw[:], in_=T['rw'][:, :, :]), writes=[K('rw')])
        P.dma('sp', lambda e: e.dma_start(out=rbias[:], in_=T['rbias'][:, :]), writes=[K('rbias')])
        ident = C['ident']
        wcount = [0]

        def load_expert(e_idx):
            ws = wset[e_idx % 2]
            for j, wn in enumerate(('w1', 'w3', 'w2')):
                src = T[wn][e_idx, :, :].rearrange("(c p) n -> p c n", p=128)
                for q in range(4):
                    si = wcount[0] % 2
                    wcount[0] += 1
                    sg = stage[si]
                    P.dma('sp', lambda e, sg=sg, src=src, q=q: e.dma_start(out=sg[:], in_=src[:, 2 * q:2 * q + 2, :]),
                          writes=[K(f'stg{si}')])
                    P.op('pool', lambda e, sg=sg, ws=ws, j=j, q=q: e.tensor_copy(out=ws[j][:, 2 * q:2 * q + 2, :], in_=sg[:]),
                         reads=[K(f'stg{si}')], writes=[K(f'w{j}_{e_idx % 2}')])

        for grp in groups:
            ng = len(grp)
            for tl, t in enumerate(grp):
                cls = cls_fn(t)
                P.dma('sp', lambda e, t=t: e.dma_start(out=xm[:], in_=T['x_in'][t * 128:(t + 1) * 128, :]),
                      reads=['x_in_' + tag], writes=[K('xm')])
                P.op('act', lambda e: e.activation(out=junk[:], in_=xm[:], func=AF.Square, accum_out=sm[:, 0:1]),
                     reads=[K('xm')], writes=[K('junk'), K('sm0')])
                P.op('act', lambda e: e.activation(out=sm[:, 1:2], in_=sm[:, 0:1], func=AF.Ln, scale=1.0 / 1024, bias=RMS_EPS),
                     reads=[K('sm0')], writes=[K('sm1')])
                P.op('act', lambda e: e.activation(out=sm[:, 1:2], in_=sm[:, 1:2], func=AF.Exp, scale=-0.5),
                     reads=[K('sm1')], writes=[K('sm1')])
                P.op('dve', lambda e: e.tensor_scalar(out=xn32[:], in0=xm[:], scalar1=sm[:, 1:2], scalar2=None, op0=ALU.mult),
                     reads=[K('xm'), K('sm1')], writes=[K('xn32')])

                def tr(e):
                    for c in range(8):
                        ins = e.transpose(out=pT[:, c, :], in_=xn32[:, c * 128:(c + 1) * 128], identity=ident[:])
                    return ins
                P.op('pe', tr, reads=[K('xn32'), 'ident'], writes=[K('pT32')])
                for c in range(8):
                    if c % 2 == 0:
                        P.op('act', lambda e, c=c, cls=cls: e.activation(
                            out=h32[:, c, :], in_=pT[:, c, :], func=AF.Identity,
                            scale=M['Gf'][:, c, cls:cls + 1], bias=M['Sf'][:, c, cls:cls + 1]),
                            reads=[K('pT32'), 'Gf', 'Sf'], writes=[K('h32')])
                    else:
                        P.op('dve', lambda e, c=c, cls=cls: e.tensor_scalar(
                            out=h32[:, c, :], in0=pT[:, c, :], scalar1=M['Gf'][:, c, cls:cls + 1],
                            scalar2=M['Sf'][:, c, cls:cls + 1], op0=ALU.mult, op1=ALU.add),
                            reads=[K('pT32'), 'Gf', 'Sf'], writes=[K('h32')])
                P.op('pool', lambda e, tl=tl: e.tensor_copy(out=h2T[:, :, tl * 128:(tl + 1) * 128], in_=h32[:]),
                     reads=[K('h32')], writes=[K('h2T')])
                pR = pY[0]

                def rm(e):
                    for k in range(8):
                        ins = e.matmul(pR[:, 0:16], lhsT=h32[:, k, :], rhs=rw[:, k, :], start=(k == 0), stop=(k == 7))
                    return ins
                P.op('pe', rm, reads=[K('h32'), K('rw')], writes=[K('pY0')])
                lg, ex, probs, sel, sel2 = r16
                top1, top2, gs, gsel = r4
                kk = [K(f'r16_{i}') for i in range(5)]
                k4 = [K(f'r4_{i}') for i in range(4)]
                P.op('dve', lambda e: e.tensor_copy(out=lg[:], in_=pR[:, 0:16]), reads=[K('pY0')], writes=[kk[0]])
                P.op('dve', lambda e: e.tensor_reduce(out=sm[:, 2:3], in_=lg[:], axis=AX.X, op=ALU.max, negate=True),
                     reads=[kk[0]], writes=[K('sm2')])
                P.op('act', lambda e: e.activation(out=ex[:], in_=lg[:], func=AF.Exp, bias=sm[:, 2:3], accum_out=sm[:, 3:4]),
                     reads=[kk[0], K('sm2')], writes=[kk[1], K('sm3')])
                P.op('dve', lambda e: e.reciprocal(out=sm[:, 4:5], in_=sm[:, 3:4]), reads=[K('sm3')], writes=[K('sm4')])
                P.op('dve', lambda e: e.tensor_scalar(out=probs[:], in0=ex[:], scalar1=sm[:, 4:5], scalar2=None, op0=ALU.mult),
                     reads=[kk[1], K('sm4')], writes=[kk[2]])
                P.op('dve', lambda e: e.tensor_tensor(out=sel[:], in0=probs[:], in1=rbias[:], op=ALU.add),
                     reads=[kk[2], K('rbias')], writes=[kk[3]])
                s3 = lambda ap: ap.rearrange("p (g j) -> p g j", g=4)
                b3 = lambda ap: ap.unsqueeze(2).broadcast_to([128, 4, 4])
                P.op('dve', lambda e: e.tensor_reduce(out=top1[:], in_=s3(sel[:]), axis=AX.X, op=ALU.max),
                     reads=[kk[3]], writes=[k4[0]])
                P.op('dve', lambda e: e.tensor_tensor(out=s3(sel2[:]), in0=s3(sel[:]), in1=b3(top1[:]), op=ALU.is_equal),
                     reads=[kk[3], k4[0]], writes=[kk[4]])
                P.op('dve', lambda e: e.scalar_tensor_tensor(out=sel2[:], in0=sel2[:], scalar=-1e30, in1=sel[:],
                                                             op0=ALU.mult, op1=ALU.add),
                     reads=[kk[4], kk[3]], writes=[kk[4]])
                P.op('dve', lambda e: e.tensor_reduce(out=top2[:], in_=s3(sel2[:]), axis=AX.X, op=ALU.max),
                     reads=[kk[4]], writes=[k4[1]])
                P.op('dve', lambda e: e.tensor_tensor(out=gs[:], in0=top1[:], in1=top2[:], op=ALU.add),
                     reads=[k4[0], k4[1]], writes=[k4[2]])
                P.op('dve', lambda e: e.tensor_reduce(out=sm[:, 5:6], in_=gs[:], axis=AX.X, op=ALU.max),
                     reads=[k4[2]], writes=[K('sm5')])
                P.op('dve', lambda e: e.tensor_scalar(out=gsel[:], in0=gs[:], scalar1=sm[:, 5:6], scalar2=None, op0=ALU.is_equal),
                     reads=[k4[2], K('sm5')], writes=[k4[3]])
                P.op('dve', lambda e: e.tensor_tensor(out=s3(sel2[:]), in0=s3(sel[:]), in1=b3(top2[:]), op=ALU.is_ge),
                     reads=[kk[3], k4[1]], writes=[kk[4]])
                P.op('dve', lambda e: e.tensor_tensor(out=s3(sel2[:]), in0=s3(sel2[:]), in1=b3(gsel[:]), op=ALU.mult),
                     reads=[kk[4], k4[3]], writes=[kk[4]])
                P.op('dve', lambda e: e.tensor_tensor(out=ex[:], in0=probs[:], in1=sel2[:], op=ALU.mult),
                     reads=[kk[2], kk[4]], writes=[kk[1]])
                P.op('dve', lambda e: e.tensor_reduce(out=sm[:, 6:7], in_=ex[:], axis=AX.X, op=ALU.add),
                     reads=[kk[1]], writes=[K('sm6')])
                P.op('dve', lambda e: e.reciprocal(out=sm[:, 7:8], in_=sm[:, 6:7]), reads=[K('sm6')], writes=[K('sm7')])
                P.op('dve', lambda e, tl=tl: e.tensor_scalar(out=gate[:, tl, :], in0=ex[:], scalar1=sm[:, 7:8], scalar2=None,
                                                             op0=ALU.mult), reads=[kk[1], K('sm7')], writes=[K('gate')])
            P.op('pool', lambda e: e.memset(acc[:], 0.0), writes=[K('acc')])
            subs = [list(range(i, min(i + 4, ng))) for i in range(0, ng, 4)]
            pc = [0]
            load_expert(0)
            for ex_i in range(16):
                if ex_i + 1 < 16:
                    load_expert(ex_i + 1)
                ws = wset[ex_i % 2]
                wk = [K(f'w{j}_{ex_i % 2}') for j in range(3)]
                for sub in subs:
                    c0 = sub[0] * 128
                    ncol = len(sub) * 128
                    for fc in range(8):
                        pi = pc[0] % 2
                        pc[0] += 1
                        pa, pb = pA[pi], pB[pi]

                        def m1(e, pa=pa, fc=fc, w=ws[0], ncol=ncol, c0=c0):
                            for k in range(8):
                                ins = e.matmul(pa[:, 0:ncol], lhsT=w[:, k, fc * 128:(fc + 1) * 128], rhs=h2T[:, k, c0:c0 + ncol],
                                               start=(k == 0), stop=(k == 7))
                            return ins

                        def m3(e, pb=pb, fc=fc, w=ws[1], ncol=ncol, c0=c0):
                            for k in range(8):
                                ins = e.matmul(pb[:, 0:ncol], lhsT=w[:, k, fc * 128:(fc + 1) * 128], rhs=h2T[:, k, c0:c0 + ncol],
                                               start=(k == 0), stop=(k == 7))
                            return ins
                        P.op('pe', m1, reads=[wk[0], K('h2T')], writes=[K(f'pA{pi}')])
                        P.op('pe', m3, reads=[wk[1], K('h2T')], writes=[K(f'pB{pi}')])
                        P.op('act', lambda e, pa=pa, ncol=ncol: e.activation(out=sgt[:, 0:ncol], in_=pa[:, 0:ncol], func=AF.Silu),
                             reads=[K(f'pA{pi}')], writes=[K('sgt')])
                        P.op('dve', lambda e, pb=pb, fc=fc, ncol=ncol: e.tensor_tensor(out=gT[:, fc, 0:ncol], in0=sgt[:, 0:ncol],
                                                                            in1=pb[:, 0:ncol], op=ALU.mult),
                             reads=[K('sgt'), K(f'pB{pi}')], writes=[K('gT')])
                    for tl2, tl in enumerate(sub):
                        for half in range(2):
                            yi = pc[0] % 2
                            pc[0] += 1
                            py = pY[yi]

                            def m2(e, py=py, tl2=tl2, half=half, w=ws[2]):
                                for fc in range(8):
                                    ins = e.matmul(py[:], lhsT=gT[:, fc, tl2 * 128:(tl2 + 1) * 128],
                                                   rhs=w[:, fc, half * 512:(half + 1) * 512], start=(fc == 0), stop=(fc == 7))
                                return ins
                            P.op('pe', m2, reads=[K('gT'), wk[2]], writes=[K(f'pY{yi}')])
                            P.op('dve', lambda e, py=py, tl=tl, half=half, ex_i=ex_i: e.scalar_tensor_tensor(
                                out=acc[:, tl, half * 512:(half + 1) * 512], in0=py[:], scalar=gate[:, tl, ex_i:ex_i + 1],
                                in1=acc[:, tl, half * 512:(half + 1) * 512], op0=ALU.mult, op1=ALU.add),
                                reads=[K(f'pY{yi}'), K('gate'), K('acc')], writes=[K('acc')])
            for tl, t in enumerate(grp):
                cls = cls_fn(t)
                P.dma('sp', lambda e, t=t: e.dma_start(out=xm[:], in_=T['x_in'][t * 128:(t + 1) * 128, :]),
                      reads=['x_in_' + tag], writes=[K('xm')])
                P.op('dve', lambda e, tl=tl, cls=cls: e.tensor_tensor(out=xn32[:], in0=acc[:, tl, :], in1=g5rep[:, cls, :],
                                                                      op=ALU.mult),
                     reads=[K('acc'), K('g5rep')], writes=[K('xn32')])
                P.op('dve', lambda e: e.tensor_tensor(out=xn32[:], in0=xn32[:], in1=xm[:], op=ALU.add),
                     reads=[K('xn32'), K('xm')], writes=[K('xn32')])
                if final_norm:
                    P.op('act', lambda e: e.activation(out=junk[:], in_=xn32[:], func=AF.Square, accum_out=sm[:, 8:9]),
                         reads=[K('xn32')], writes=[K('junk'), K('sm8')])
                    P.op('act', lambda e: e.activation(out=sm[:, 9:10], in_=sm[:, 8:9], func=AF.Ln, scale=1.0 / 1024, bias=RMS_EPS),
                         reads=[K('sm8')], writes=[K('sm9')])
                    P.op('act', lambda e: e.activation(out=sm[:, 9:10], in_=sm[:, 9:10], func=AF.Exp, scale=-0.5),
                         reads=[K('sm9')], writes=[K('sm9')])
                    P.op('dve', lambda e: e.scalar_tensor_tensor(out=xn32[:], in0=xn32[:], scalar=sm[:, 9:10], in1=fng[:],
                                                                 op0=ALU.mult, op1=ALU.mult),
                         reads=[K('xn32'), K('sm9'), K('fng')], writes=[K('xn32')])
                P.dma('sp', lambda e, t=t: e.dma_start(out=T['x_out'][t * 128:(t + 1) * 128, :], in_=xn32[:]),
                      reads=[K('xn32')], writes=['x_out_' + tag])
        P.barrier()


def build_A(cfg, debug_out=True, phases=('attn', 'moe'), n_exp=16):
    nc = bass.Bass("TRN2", target_bir_lowering=False)
    def din(name, shape):
        return nc.dram_tensor(name, shape, F32, kind="ExternalInput").ap()
    T = {}
    T['xo'] = din("xo", [cfg.TO, 1024])
    T['xr'] = din("xr", [cfg.TR, 1024])
    T['cosO'] = din("cosO", [128, cfg.TO])
    T['sinO'] = din("sinO", [128, cfg.TO])
    T['cosR'] = din("cosR", [128, cfg.TR])
    T['sinR'] = din("sinR", [128, cfg.TR])
    T['masks'] = din("masks", [4, 128, 128])
    T['gqk'] = din("gqk", [128, 4])
    T['sink'] = din("sink", [1, 8])
    T['pm'] = din("pm", [128, 128])
    T['wkv'] = din("wkv", [1024, 512])
    T['wq'] = din("wq", [1024, 1024])
    T['wout'] = din("wout", [128, 8, 1024])
    cT = din("cT", [128, 8, 2])
    ada_w = din("ada_w", [1024, 6144])
    ada_bT = din("ada_bT", [128, 48])
    adab_rep = din("adab_rep", [128, 2, 1024])
    gmixT = din("gmixT", [128, 8])
    gffnT = din("gffnT", [128, 8])
    if 'moe' in phases:
        T['rw'] = din("rw", [128, 8, 16])
        T['rbias'] = din("rbias", [128, 16])
        T['w1'] = din("w1", [n_exp, 1024, 1024])
        T['w3'] = din("w3", [n_exp, 1024, 1024])
        T['w2'] = din("w2", [n_exp, 1024, 1024])
        T['x2'] = nc.dram_tensor("x2", [cfg.TQ, 1024], F32, kind="ExternalOutput").ap()
    T['x1'] = nc.dram_tensor("x1", [cfg.TQ, 1024], F32, kind="ExternalOutput").ap()
    T['gdram'] = nc.dram_tensor("gdram", [2, 128, 2, 1024], F32, kind="ExternalOutput").ap()
    P = Prog(nc)
    P.psum_keys = PSUM_KEYS
    with ExitStack() as st:
        sb = _sb(nc, st)
        C = make_consts(P, nc, sb)
        M = mod_setup(P, nc, st, C, cT, ada_w, ada_bT, adab_rep, gmixT, gffnT, T['gdram'])
        if 'attn' in phases:
            try:
                attn_phase(P, nc, cfg, C, M, T)
            except StopBuild:
                P.barrier()
        if 'moe' in phases:
            T['x_in'] = T['x1']
            T['x_out'] = T['x2']
            moe_phase(P, nc, C, M, T, cfg.NTQ, lambda t: 1 if t < 2 else 0, "m")
        P.final_wait('sp')
        P.emit(st)
    print("A ops:", P.nops, {e: len(P.streams[e]) for e in P.ENG})
    return nc


def rope_tab(pos):
    p = np.arange(128)
    d = p % 64
    axis = d // 32
    half = (d % 32) // 16
    f = d % 16
    inv = (10000.0 ** (-(f.astype(np.float32)) / 16.0)).astype(np.float32)
    pos = np.asarray(pos)
    row = (pos // 64).astype(np.float32)
    col = (pos % 64).astype(np.float32)
    pa = np.where(axis[:, None] == 0, row[None, :], col[None, :]).astype(np.float32)
    ang = (pa * inv[:, None]).astype(np.float32)
    cos = np.cos(ang).astype(np.float32)
    sin = np.sin(ang).astype(np.float32) * np.where(half == 0, -1.0, 1.0)[:, None].astype(np.float32)
    nr = pos < 0
    cos[:, nr] = 1.0
    sin[:, nr] = 0.0
    return cos, sin.astype(np.float32)


def partner():
    p = np.arange(128)
    d = p % 64
    pd = np.where((d % 32) < 16, d + 16, d - 16)
    return (p // 64) * 64 + pd


def host_A(inp, cfg, core, layer=0, with_moe=True):
    b, qd = core // 4, core % 4
    S, OWN = cfg.S, cfg.OWN
    x = inp['x'][b, :S]
    ctx = inp['ctx'][b]
    o0 = qd * OWN
    z = np.zeros((128, 1024), np.float32)
    hl = x[o0 - 128:o0] if qd > 0 else z
    hr = x[o0 + OWN:o0 + OWN + 128] if qd < 3 else z
    xo = np.concatenate([ctx, hl, x[o0:o0 + OWN], hr], 0)
    posO = np.concatenate([-np.ones(256, np.int64),
                           np.arange(o0 - 128, o0) if qd > 0 else -np.ones(128, np.int64),
                           np.arange(o0, o0 + OWN),
                           np.arange(o0 + OWN, o0 + OWN + 128) if qd < 3 else -np.ones(128, np.int64)])
    xr = np.concatenate([x[:o0], x[o0 + OWN:]], 0)
    posR = np.concatenate([np.arange(0, o0), np.arange(o0 + OWN, S)])
    cosO, sinO = rope_tab(posO)
    cosR, sinR = rope_tab(posR)
    k = np.arange(128)[:, None]
    q = np.arange(128)[None, :]
    mL = (k >= q).astype(np.float32)
    mR = (k <= q).astype(np.float32)
    masks = np.stack([mL if qd > 0 else 0 * mL, mL, mR, mR if qd < 3 else 0 * mR])
    pt = partner()
    d = np.arange(128) % 64
    gq = inp['attn_q_norm_g'][0]
    gk = inp['attn_k_norm_g'][0]
    gqk = np.stack([gq[d], gq[pt % 64], gk[d], gk[pt % 64]], 1).astype(np.float32)
    pm = np.zeros((128, 128), np.float32)
    pm[pt, np.arange(128)] = 1.0
    w_in = inp['attn_w_in'][0]
    wkv = np.concatenate([w_in[:, 512:640], w_in[:, 1280:1408], w_in[:, 640:768], w_in[:, 1408:1536]], 1)
    cols = []
    for c in range(4):
        cols += [w_in[:, c * 64:(c + 1) * 64], w_in[:, (4 + c) * 64:(5 + c) * 64]]
    for j in range(4):
        cols += [w_in[:, 768 + j * 64:768 + (j + 1) * 64], w_in[:, 768 + (4 + j) * 64:768 + (5 + j) * 64]]
    wq = np.concatenate(cols, 1)
    w_out = inp['attn_w_out'][0]
    wout = np.zeros((128, 8, 1024), np.float32)
    for hh in range(8):
        wout[0:64, hh] = w_out[hh * 64:(hh + 1) * 64]
        wout[64:128, hh] = w_out[512 + hh * 64:512 + (hh + 1) * 64]
    m = {}
    m.update(xo=xo, xr=xr, cosO=cosO, sinO=sinO, cosR=cosR, sinR=sinR, masks=masks, gqk=gqk,
             sink=inp['attn_sink'][0][None, :], pm=pm, wkv=wkv, wq=wq, wout=wout)
    m.update(host_mod(inp, b, layer))
    if with_moe:
        m.update(host_moe(inp, layer))
    return {k_: np.ascontiguousarray(v, dtype=np.float32) for k_, v in m.items()}


def host_mod(inp, b, layer):
    cT = np.stack([inp['c'][b].reshape(8, 128).T, inp['c_ctx'].reshape(8, 128).T], 2)
    ab = inp['ada_b'][layer]
    return dict(cT=cT, ada_w=inp['ada_w'][layer], ada_bT=ab.reshape(48, 128).T,
                adab_rep=np.broadcast_to(np.stack([ab[2048:3072], ab[5120:6144]])[None], (128, 2, 1024)),
                gmixT=inp['norm_mix_g'][layer].reshape(8, 128).T, gffnT=inp['norm_ffn_g'][layer].reshape(8, 128).T)


def host_moe(inp, layer):
    return dict(rw=inp['router_w'].reshape(8, 128, 16).transpose(1, 0, 2),
                rbias=np.broadcast_to(inp['router_bias'][None], (128, 16)),
                w1=inp['moe_w1'][layer], w3=inp['moe_w3'][layer], w2=inp['moe_w2'][layer])


def build_A2(NT, n_exp=16):
    nc = bass.Bass("TRN2", target_bir_lowering=False)
    def din(name, shape):
        return nc.dram_tensor(name, shape, F32, kind="ExternalInput").ap()
    T = {}
    T['x_in'] = din("xin", [NT * 128, 1024])
    cT = din("cT", [128, 8, 2])
    ada_w = din("ada_w", [1024, 6144])
    ada_bT = din("ada_bT", [128, 48])
    adab_rep = din("adab_rep", [128, 2, 1024])
    gmixT = din("gmixT", [128, 8])
    gffnT = din("gffnT", [128, 8])
    T['rw'] = din("rw", [128, 8, 16])
    T['rbias'] = din("rbias", [128, 16])
    T['w1'] = din("w1", [n_exp, 1024, 1024])
    T['w3'] = din("w3", [n_exp, 1024, 1024])
    T['w2'] = din("w2", [n_exp, 1024, 1024])
    T['x_out'] = nc.dram_tensor("x2", [NT * 128, 1024], F32, kind="ExternalOutput").ap()
    T['gdram'] = nc.dram_tensor("gdram", [2, 128, 2, 1024], F32, kind="ExternalOutput").ap()
    P = Prog(nc)
    P.psum_keys = PSUM_KEYS
    with ExitStack() as st:
        sb = _sb(nc, st)
        C = make_consts(P, nc, sb)
        M = mod_setup(P, nc, st, C, cT, ada_w, ada_bT, adab_rep, gmixT, gffnT, T['gdram'])
        moe_phase(P, nc, C, M, T, NT, lambda t: 1 if t < 2 else 0, "m")
        P.final_wait('sp')
        P.emit(st)
    print("A2 ops:", P.nops, {e: len(P.streams[e]) for e in P.ENG})
    return nc


def host_A2(inp, xin, b):
    m = {'xin': xin}
    m.update(host_mod(inp, b, 0))
    m.update(host_moe(inp, 0))
    return {k_: np.ascontiguousarray(v, dtype=np.float32) for k_, v in m.items()}


DEC_C = -0.6065306597126334
INV_DT = BF16


def rwkv_phase(P, nc, C, M, T, NL):
    with ExitStack() as st:
        sb = _sb(nc, st)
        ps = _ps(nc, st)
        wrkv = sb("wrkv", [128, 8, 768], BF16)
        wl = sb("wl", [128, 8, 416], BF16)
        w2c = sb("w2c", [128, 256], BF16)
        a2c = sb("a2c", [128, 256], BF16)
        g2a = sb("g2a", [128, 256], BF16)
        g2b = sb("g2b", [32, 256], BF16)
        xmix = sb("xmix", [128, 8, 6], F32)
        colv = sb("colv", [128, 2, 6], F32)
        rkw = sb("rkw", [128, 2, 2], BF16)
        lnrep = sb("lnrep", [128, 2, 256], F32)
        mk = sb("mk", [128, 10, 128], F32)
        rmask = sb("rmask", [128, 512], F32)
        stg = [sb(f"bstg{i}", [128, 1024], F32) for i in range(2)]
        sk = ['bstg0', 'bstg1']
        cnt = [0]

        def ld(dst, src, shape_cols, dkey, nparts=128):
            i = cnt[0] % 2
            cnt[0] += 1
            P.dma('sp', lambda e: e.dma_start(out=stg[i][0:nparts, 0:shape_cols], in_=src), writes=[sk[i]])
            P.op('pool', lambda e: e.tensor_copy(out=dst, in_=stg[i][0:nparts, 0:shape_cols]), reads=[sk[i]], writes=[dkey])
        wsrc = T['wrkv'].rearrange("(c p) n -> p c n", p=128)
        for k in range(8):
            ld(wrkv[:, k, :], wsrc[:, k, :], 768, 'wrkv')
        for (nm, c0, n) in (('w1cat', 0, 128), ('a1cat', 128, 128), ('g1', 256, 160)):
            src = T[nm].rearrange("(c p) n -> p c n", p=128)
            for k in range(8):
                ld(wl[:, k, c0:c0 + n], src[:, k, :], n, 'wl')
        ld(w2c[:], T['w2cat'][:, :], 256, 'w2c')
        ld(a2c[:], T['a2cat'][:, :], 256, 'a2c')
        ld(g2a[:], T['g2a'][:, :], 256, 'g2a')
        ld(g2b[:], T['g2b'][:, :], 256, 'g2b', nparts=32)
        ld(rkw[:].rearrange("p a b -> p (a b)"), T['rkw'].rearrange("p a b -> p (a b)"), 4, 'rkw')
        P.dma('sp', lambda e: e.dma_start(out=xmix[:], in_=T['xmixT'][:, :, :]), writes=['xmix'])
        P.dma('sp', lambda e: e.dma_start(out=colv[:], in_=T['colvec'][:, :, :]), writes=['colv'])
        P.dma('sp', lambda e: e.dma_start(out=lnrep[:], in_=T['lnrep'][:, :, :]), writes=['lnrep'])
        for i in range(10):
            P.dma('sp', lambda e, i=i: e.dma_start(out=mk[:, i, :], in_=T['mk'][i, :, :]), writes=['mk'])
        P.op('pool', lambda e: e.memset(rmask[:], 1.0), writes=['rmask'])
        P.op('pool', lambda e: e.memset(rmask[:].rearrange("p (c t) -> p c t", t=64)[:, :, 0:1], 0.0),
             reads=['rmask'], writes=['rmask'])
        P.barrier()

        xt = [sb(f"bxt{i}", [128, 1024], F32) for i in range(2)]
        xh = sb("bxh", [2, 1024], F32)
        hTx = sb("hTx", [128, 8, 514], BF16)
        xx = sb("xx", [128, 8, 512], BF16)
        mixb = [sb(f"mix{i}", [128, 8, 512], BF16) for i in range(2)]
        nrm = NormT(P, nc, sb, ps, C, "bn")
        F = lambda name: sb(name, [128, 2, 512], F32)
        rT, kT, lw, icl, kkn, kd, cin, tmpA, tmpB, e2 = [F(n) for n in
                                                         ("rT", "kT", "lw", "icl", "kkn", "kd", "cin", "tmpA", "tmpB", "e2")]
        arT = sb("arT", [128, 2, 2, 512], BF16)
        bhT = sb("bhT", [128, 2, 512], BF16)
        khT = sb("khT", [128, 2, 512], BF16)
        bgT = sb("bgT", [128, 2, 512], BF16)
        kgT = sb("kgT", [128, 2, 512], BF16)
        rkb = sb("rkb", [128, 2, 512], BF16)
        sqb = sb("sqb", [128, 512], BF16)
        th = sb("th", [128, 512], BF16)
        ua = sb("ua", [128, 512], BF16)
        sg = sb("sg", [128, 512], BF16)
        sg2 = sb("sg2", [32, 512], BF16)
        vtok = sb("vtok", [128, 4, 256], BF16)
        gtok = sb("gtok", [128, 4, 256], F32)
        bon = sb("bon", [128, 4, 4], F32)
        gC = sb("gC", [128, 2, 8], F32)
        tot = sb("tot", [128, 2, 8], F32)
        tk = sb("tk", [128, 3, 256], BF16)
        U1 = sb("U1", [128, 4, 2, 128], BF16)
        U2 = sb("U2", [128, 4, 2, 128], BF16)
        AA = [sb(f"AA{i}", [128, 4, 2, 128], F32) for i in range(2)]
        AB = [sb(f"AB{i}", [128, 4, 2, 128], F32) for i in range(2)]
        UoffT = sb("UoffT", [128, 4, 3, 128], F32)
        T32 = sb("T32", [128, 4, 2, 128], F32)
        Zs = sb("Zs", [128, 4, 2, 128], F32)
        Tfin = sb("Tfin", [128, 4, 128], BF16)
        Mtmp = sb("Mtmp", [128, 2, 64], F32)
        XA = sb("XA", [128, 4, 2, 64], BF16)
        WA = sb("WA", [128, 2, 4, 64], BF16)
        RtT = sb("RtT", [128, 2, 128], F32)
        Msb = sb("Msb", [128, 2, 2, 128], F32)
        Sbd = sb("Sbd", [128, 2, 128], F32)
        ysb = sb("ysb", [128, 256], F32)
        y0t = sb("y0t", [128, 256], F32)
        b0t = sb("b0t", [128, 4], F32)
        st8 = sb("st8", [128, 16], F32)
        osb = sb("osb", [128, 256], F32)
        pA = ps("bpA", [128, 512], F32)
        pG = [ps(f"bpG{i}", [128, 2, 2, 128], F32) for i in range(2)]
        pXf = ps("bpX", [128, 512], F32)
        pX = pXf[:].rearrange("p (h a k) -> p h a k", h=4, a=2)
        pB = pXf
        pYf = ps("bpY", [128, 512], F32)
        pY = pYf[:, 0:256].rearrange("p (h v) -> p h v", h=4)
        pSf = ps("bpS", [128, 512], F32)
        pS = pSf[:, 0:128].rearrange("p (c v) -> p c v", c=2)
        pTt_full = ps("bpTt", [128, 4, 256], BF16)
        pTt = pTt_full[:, 0:3, :]
        ident = C['ident']
        identb = C['identb']
        xc_ = [0]

        def proj_fm(w, c0, n, mix, out_ps, wkey):
            def f(e):
                for k in range(8):
                    ins = e.matmul(out_ps, lhsT=w[:, k, c0:c0 + n], rhs=mix[:, k, :], start=(k == 0), stop=(k == 7))
                return ins
            return f

        def do_segment(tok0, n, is_ctx, left_ok, right_ok, d, final):
            nt = n // 128
            nsc = n // 64
            cls = 1 if is_ctx else 0
            for tt in range(nt):
                xi = xc_[0] % 2
                xc_[0] += 1
                P.dma('sp', lambda e, xi=xi, tt=tt: e.dma_start(out=xt[xi][:], in_=T['xs'][tok0 + tt * 128:tok0 + (tt + 1) * 128, :]),
                      writes=[f'bxt{xi}'])
                nrm.run(xt[xi][:], f'bxt{xi}', M['Gm'], M['Sm'], cls,
                        lambda c, tt=tt: hTx[:, c, 1 + tt * 128:1 + (tt + 1) * 128], 'hTx', ['Gm', 'Sm'])
            P.op('dve', lambda e: e.memset(hTx[:, :, 0:1], 0.0), writes=['hTx'])
            P.op('dve', lambda e: e.memset(hTx[:, :, n + 1:n + 2], 0.0), writes=['hTx'])
            if left_ok or right_ok:
                P.op('dve', lambda e: e.memset(xh[:], 1.0), writes=['bxh'])
                if left_ok:
                    P.dma('sp', lambda e: e.dma_start(out=xh[0:1, :], in_=T['xs'][tok0 - 1:tok0, :]), reads=['bxh'], writes=['bxh'])
                if right_ok:
                    P.dma('sp', lambda e: e.dma_start(out=xh[1:2, :], in_=T['xs'][tok0 + n:tok0 + n + 1, :]), reads=['bxh'], writes=['bxh'])
                nrm.run(xh[:], 'bxh', M['Gm'], M['Sm'], cls, None, 'hTx', ['Gm', 'Sm'], npart=2,
                        halo=(hTx, n, left_ok, right_ok))
            P.op('dve', lambda e: e.tensor_tensor(out=xx[:, :, 0:n], in0=hTx[:, :, 0:n], in1=hTx[:, :, 2:n + 2], op=ALU.add),
                 reads=['hTx'], writes=['xx'])
            P.op('dve', lambda e: e.scalar_tensor_tensor(out=xx[:, :, 0:n], in0=xx[:, :, 0:n], scalar=0.5, in1=hTx[:, :, 1:n + 1],
                                                         op0=ALU.mult, op1=ALU.subtract), reads=['xx', 'hTx'], writes=['xx'])
            mc = [0]

            def make_mix(j):
                mi = mc[0] % 2
                mc[0] += 1
                mb = mixb[mi]
                for c in range(8):
                    P.op('dve', lambda e, c=c, mb=mb: e.scalar_tensor_tensor(
                        out=mb[:, c, 0:n], in0=xx[:, c, 0:n], scalar=xmix[:, c, j:j + 1], in1=hTx[:, c, 1:n + 1],
                        op0=ALU.mult, op1=ALU.add), reads=['xx', 'hTx', 'xmix'], writes=[f'mix{mi}'])
                return mb, f'mix{mi}'
            mb, mkey = make_mix(0)
            for cc in range(2):
                P.op('pe', proj_fm(wrkv, cc * 128, 128, mb[:, :, 0:n], pA[:, 0:n], 'wrkv'), reads=['wrkv', mkey], writes=['bpA'])
                P.op('act', lambda e, cc=cc: e.activation(out=rT[:, cc, 0:n], in_=pA[:, 0:n], func=AF.Copy), reads=['bpA'], writes=['rT'])
            mb, mkey = make_mix(2)
            for cc in range(2):
                P.op('pe', proj_fm(wrkv, 256 + cc * 128, 128, mb[:, :, 0:n], pA[:, 0:n], 'wrkv'), reads=['wrkv', mkey], writes=['bpA'])
                P.op('act', lambda e, cc=cc: e.activation(out=kT[:, cc, 0:n], in_=pA[:, 0:n], func=AF.Copy), reads=['bpA'], writes=['kT'])
            mb, mkey = make_mix(3)
            for tt in range(nt):
                def vm(e, tt=tt, mb=mb):
                    for k in range(8):
                        ins = e.matmul(pA[:, 0:256], lhsT=mb[:, k, tt * 128:(tt + 1) * 128], rhs=wrkv[:, k, 512:768],
                                       start=(k == 0), stop=(k == 7))
                    return ins
                P.op('pe', vm, reads=['wrkv', mkey], writes=['bpA'])
                P.op('act', lambda e, tt=tt: e.activation(out=vtok[:, tt, :], in_=pA[:, 0:256], func=AF.Copy), reads=['bpA'], writes=['vtok'])
            mb, mkey = make_mix(1)
            dp = d * 64
            P.op('pe', proj_fm(wl, d * 64, 64, mb[:, :, 0:n], pB[dp:dp + 64, 0:n], 'wl'), reads=['wl', mkey], writes=['bpX'])
            P.op('act', lambda e: e.activation(out=th[dp:dp + 64, 0:n], in_=pB[dp:dp + 64, 0:n], func=AF.Tanh), reads=['bpX'], writes=['th'])
            for cc in range(2):
                P.op('pe', lambda e, cc=cc: e.matmul(pA[:, 0:n], lhsT=w2c[dp:dp + 64, cc * 128:(cc + 1) * 128], rhs=th[dp:dp + 64, 0:n],
                                                     start=True, stop=True), reads=['w2c', 'th'], writes=['bpA'])
                P.op('act', lambda e, cc=cc: e.activation(out=lw[:, cc, 0:n], in_=pA[:, 0:n], func=AF.Sigmoid, bias=colv[:, cc, d:d + 1]),
                     reads=['bpA', 'colv'], writes=['lw'])
            P.op('dve', lambda e: e.tensor_scalar(out=lw[:, :, 0:n], in0=lw[:, :, 0:n], scalar1=DEC_C, scalar2=None, op0=ALU.mult),
                 reads=['lw'], writes=['lw'])
            mb, mkey = make_mix(4)
            P.op('pe', proj_fm(wl, 128 + d * 64, 64, mb[:, :, 0:n], pB[dp:dp + 64, 0:n], 'wl'), reads=['wl', mkey], writes=['bpX'])
            P.op('act', lambda e: e.activation(out=ua[dp:dp + 64, 0:n], in_=pB[dp:dp + 64, 0:n], func=AF.Copy), reads=['bpX'], writes=['ua'])
            for cc in range(2):
                P.op('pe', lambda e, cc=cc: e.matmul(pA[:, 0:n], lhsT=a2c[dp:dp + 64, cc * 128:(cc + 1) * 128], rhs=ua[dp:dp + 64, 0:n],
                                                     start=True, stop=True), reads=['a2c', 'ua'], writes=['bpA'])
                P.op('act', lambda e, cc=cc: e.activation(out=icl[:, cc, 0:n], in_=pA[:, 0:n], func=AF.Sigmoid, bias=colv[:, cc, 2 + d:3 + d]),
                     reads=['bpA', 'colv'], writes=['icl'])
            if final and not is_ctx:
                mb, mkey = make_mix(5)
                P.op('pe', proj_fm(wl, 256, 128, mb[:, :, 0:n], pA[:, 0:n], 'wl'), reads=['wl', mkey], writes=['bpA'])
                P.op('act', lambda e: e.activation(out=sg[:, 0:n], in_=pA[:, 0:n], func=AF.Sigmoid), reads=['bpA'], writes=['sg'])
                P.op('pe', proj_fm(wl, 384, 32, mb[:, :, 0:n], pB[0:32, 0:n], 'wl'), reads=['wl', mkey], writes=['bpX'])
                P.op('act', lambda e: e.activation(out=sg2[:, 0:n], in_=pB[0:32, 0:n], func=AF.Sigmoid), reads=['bpX'], writes=['sg2'])
                for tt in range(nt):
                    def gm(e, tt=tt):
                        e.matmul(pA[:, 0:256], lhsT=sg[:, tt * 128:(tt + 1) * 128], rhs=g2a[:, :], start=True, stop=False)
                        return e.matmul(pA[:, 0:256], lhsT=sg2[:, tt * 128:(tt + 1) * 128], rhs=g2b[:, :], start=False, stop=True)
                    P.op('pe', gm, reads=['sg', 'sg2', 'g2a', 'g2b'], writes=['bpA'])
                    P.op('act', lambda e, tt=tt: e.activation(out=gtok[:, tt, :], in_=pA[:, 0:256], func=AF.Copy), reads=['bpA'], writes=['gtok'])
            V3 = lambda t_: t_[:, :, 0:n]
            for cc in range(2):
                P.op('dve', lambda e, cc=cc: e.tensor_scalar(out=kkn[:, cc, 0:n], in0=kT[:, cc, 0:n], scalar1=colv[:, cc, 4:5], scalar2=None,
                                                             op0=ALU.mult), reads=['kT', 'colv'], writes=['kkn'])
                P.op('act', lambda e, cc=cc: e.activation(out=sqb[:, 0:n], in_=kkn[:, cc, 0:n], func=AF.Square), reads=['kkn'], writes=['sqb'])
                P.op('pe', lambda e: e.matmul(pB[:, 0:n], lhsT=C['bones'][:], rhs=sqb[:, 0:n], start=True, stop=True),
                     reads=['sqb', 'bones'], writes=['bpX'])
                P.op('dve', lambda e, cc=cc: e.tensor_scalar(out=tmpA[:, cc, 0:n], in0=pB[:, 0:n], scalar1=1e-18, scalar2=None, op0=ALU.max),
                     reads=['bpX'], writes=['tmpA'])
            P.op('act', lambda e: e.activation(out=V3(tmpA), in_=V3(tmpA), func=AF.Ln), reads=['tmpA'], writes=['tmpA'])
            P.op('act', lambda e: e.activation(out=V3(tmpA), in_=V3(tmpA), func=AF.Exp, scale=-0.5), reads=['tmpA'], writes=['tmpA'])
            P.op('dve', lambda e: e.tensor_tensor(out=V3(kkn), in0=V3(kkn), in1=V3(tmpA), op=ALU.mult), reads=['kkn', 'tmpA'], writes=['kkn'])
            for cc in range(2):
                P.op('dve', lambda e, cc=cc: e.tensor_scalar(out=kd[:, cc, 0:n], in0=icl[:, cc, 0:n], scalar1=-1.0, scalar2=colv[:, cc, 5:6],
                                                             op0=ALU.add, op1=ALU.mult), reads=['icl', 'colv'], writes=['kd'])
            P.op('dve', lambda e: e.scalar_tensor_tensor(out=V3(kd), in0=V3(kd), scalar=1.0, in1=V3(kT), op0=ALU.add, op1=ALU.mult),
                 reads=['kd', 'kT'], writes=['kd'])
            P.op('dve', lambda e: e.tensor_tensor(out=V3(rkb), in0=V3(rT), in1=V3(kd), op=ALU.mult), reads=['rT', 'kd'], writes=['rkb'])
            for tt in range(nt):
                def bm(e, tt=tt):
                    for cc in range(2):
                        ins = e.matmul(pB[:, cc * 2:cc * 2 + 2], lhsT=rkb[:, cc, tt * 128:(tt + 1) * 128], rhs=rkw[:, cc, :], start=True, stop=True)
                    return ins
                P.op('pe', bm, reads=['rkb', 'rkw'], writes=['bpX'])
                P.op('dve', lambda e, tt=tt: e.tensor_copy(out=bon[:, tt, :], in_=pB[:, 0:4]), reads=['bpX'], writes=['bon'])
            for cc in range(2):
                P.op('dve', lambda e, cc=cc: e.tensor_tensor_scan(out=cin[:, cc, 0:n], data0=rmask[:, 0:n], data1=lw[:, cc, 0:n], initial=0.0,
                                                                  op0=ALU.mult, op1=ALU.add), reads=['rmask', 'lw'], writes=['cin'])
            c4 = lambda t_: t_[:, :, 0:n].rearrange("p c (k t) -> p c k t", t=64)
            P.op('dve', lambda e: e.tensor_copy(out=tot[:, :, 0:nsc], in_=c4(cin)[:, :, :, 63]), reads=['cin'], writes=['tot'])
            totb = lambda: tot[:, :, 0:nsc].unsqueeze(3).broadcast_to([128, 2, nsc, 64])
            if d == 1:
                P.op('dve', lambda e: e.tensor_tensor(out=V3(cin), in0=V3(lw), in1=V3(cin), op=ALU.subtract), reads=['lw', 'cin'], writes=['cin'])
                P.op('dve', lambda e: e.tensor_tensor(out=c4(cin), in0=c4(cin), in1=totb(), op=ALU.add), reads=['cin', 'tot'], writes=['cin'])
            P.op('act', lambda e: e.activation(out=gC[:, :, 0:nsc], in_=tot[:, :, 0:nsc], func=AF.Exp), reads=['tot'], writes=['gC'])
            P.op('dve', lambda e: e.tensor_tensor(out=c4(e2), in0=totb(), in1=c4(cin), op=ALU.subtract), reads=['cin', 'tot'], writes=['e2'])
            P.op('act', lambda e: e.activation(out=V3(e2), in_=V3(e2), func=AF.Exp), reads=['e2'], writes=['e2'])
            P.op('act', lambda e: e.activation(out=V3(tmpA), in_=V3(cin), func=AF.Exp), reads=['cin'], writes=['tmpA'])
            P.op('dve', lambda e: e.tensor_tensor(out=arT[:, :, 1, 0:n], in0=V3(rT), in1=V3(tmpA), op=ALU.mult), reads=['rT', 'tmpA'], writes=['arT'])
            P.op('dve', lambda e: e.tensor_tensor(out=V3(tmpB), in0=V3(cin), in1=V3(lw), op=ALU.subtract), reads=['cin', 'lw'], writes=['tmpB'])
            P.op('act', lambda e: e.activation(out=V3(tmpB), in_=V3(tmpB), func=AF.Exp), reads=['tmpB'], writes=['tmpB'])
            P.op('dve', lambda e: e.scalar_tensor_tensor(out=arT[:, :, 0, 0:n], in0=V3(kkn), scalar=-1.0, in1=V3(tmpB), op0=ALU.mult, op1=ALU.mult),
                 reads=['kkn', 'tmpB'], writes=['arT'])
            P.op('act', lambda e: e.activation(out=V3(tmpA), in_=V3(cin), func=AF.Exp, scale=-1.0), reads=['cin', 'arT'], writes=['tmpA'])
            P.op('dve', lambda e: e.tensor_tensor(out=V3(tmpB), in0=V3(kkn), in1=V3(icl), op=ALU.mult), reads=['kkn', 'icl', 'arT'], writes=['tmpB'])
            P.op('dve', lambda e: e.tensor_tensor(out=V3(bhT), in0=V3(tmpB), in1=V3(tmpA), op=ALU.mult), reads=['tmpB', 'tmpA'], writes=['bhT'])
            P.op('dve', lambda e: e.tensor_tensor(out=V3(khT), in0=V3(kd), in1=V3(tmpA), op=ALU.mult), reads=['kd', 'tmpA'], writes=['khT'])
            P.op('dve', lambda e: e.tensor_tensor(out=V3(bgT), in0=V3(tmpB), in1=V3(e2), op=ALU.mult), reads=['tmpB', 'e2'], writes=['bgT'])
            P.op('dve', lambda e: e.tensor_tensor(out=V3(kgT), in0=V3(kd), in1=V3(e2), op=ALU.mult), reads=['kd', 'e2'], writes=['kgT'])
            if d == 0:
                m_s, mA, mAt, mO1, mO1T, mO2T = 0, 4, 5, 6, 7, 9
            else:
                m_s, mA, mAt, mO1, mO1T, mO2T = 2, 5, 4, 7, 6, 8
            order = list(range(nt)) if d == 0 else list(range(nt - 1, -1, -1))
            corder = [0, 1] if d == 0 else [1, 0]
            for tt in order:
                cs = slice(tt * 128, (tt + 1) * 128)
                def trs(e, cs=cs):
                    for qi, src in enumerate((None, bgT, kgT)):
                        for cc in range(2):
                            s_ap = arT[:, cc, 0, cs] if qi == 0 else src[:, cc, cs]
                            ins = e.transpose(out=pTt[:, qi, cc * 128:(cc + 1) * 128], in_=s_ap, identity=identb[:])
                    return ins
                P.op('pe', trs, reads=['arT', 'bgT', 'kgT', 'identb'], writes=['bpTt'])
                P.op('act', lambda e: e.activation(out=tk[:], in_=pTt, func=AF.Copy), reads=['bpTt'], writes=['tk'])
                P.op('dve', lambda e: e.tensor_copy(out=XA[:, :, 1, :], in_=tk[:, 0, :].rearrange("p (h k) -> p h k", h=4)),
                     reads=['tk'], writes=['XA'])
                for hh in range(2):
                    hp = hh * 64
                    hs = slice(hh, 4, 2)

                    def gr(e, hp=hp, cs=cs, src=bhT, gp=pG[0]):
                        for cc in range(2):
                            ins = e.matmul(gp[:, cc, :, :], lhsT=src[hp:hp + 64, cc, cs], rhs=arT[hp:hp + 64, cc, :, cs], start=True, stop=True)
                        return ins
                    P.op('pe', gr, reads=['bhT', 'arT'], writes=['bpG0'])
                    P.op('dve', lambda e, hs=hs: e.tensor_tensor(
                        out=U1[:, hs, :, :], in0=pG[0][:],
                        in1=mk[:, m_s:m_s + 2, :].unsqueeze(1).broadcast_to([128, 2, 2, 128]), op=ALU.mult),
                        reads=['bpG0', 'mk'], writes=['U1'])
                    P.op('dve', lambda e, hs=hs: e.tensor_tensor(
                        out=AA[0][:, hs, 0, :], in0=pG[0][:, :, 0, :],
                        in1=mk[:, mA, :].unsqueeze(1).broadcast_to([128, 2, 128]), op=ALU.mult),
                        reads=['bpG0', 'mk'], writes=['AA0'])
                    P.op('dve', lambda e, hs=hs: e.tensor_tensor(
                        out=UoffT[:, hs, 1, :], in0=pG[0][:, :, 0, :],
                        in1=mk[:, mO1, :].unsqueeze(1).broadcast_to([128, 2, 128]), op=ALU.mult),
                        reads=['bpG0', 'mk'], writes=['UoffT'])
                    P.op('pe', lambda e, hp=hp, cs=cs: gr(e, hp, cs, khT, pG[1]), reads=['khT', 'arT'], writes=['bpG1'])
                    P.op('dve', lambda e, hs=hs: e.tensor_tensor(
                        out=U2[:, hs, :, :], in0=pG[1][:],
                        in1=mk[:, m_s:m_s + 2, :].unsqueeze(1).broadcast_to([128, 2, 2, 128]), op=ALU.mult),
                        reads=['bpG1', 'mk'], writes=['U2'])

                    def gt(e, hp=hp, cs=cs):
                        for cc in range(2):
                            ins = e.matmul(pG[0][:, cc, 0, :], lhsT=arT[hp:hp + 64, cc, 0, cs], rhs=bhT[hp:hp + 64, cc, cs], start=True, stop=True)
                        return ins
                    P.op('pe', gt, reads=['bhT', 'arT'], writes=['bpG0'])
                    P.op('dve', lambda e, hs=hs: e.tensor_tensor(
                        out=AB[0][:, hs, 0, :], in0=pG[0][:, :, 0, :],
                        in1=mk[:, mAt, :].unsqueeze(1).broadcast_to([128, 2, 128]), op=ALU.mult),
                        reads=['bpG0', 'mk'], writes=['AB0'])
                    P.op('dve', lambda e, hs=hs: e.tensor_tensor(
                        out=UoffT[:, hs, 0, :], in0=pG[0][:, :, 0, :],
                        in1=mk[:, mO1T, :].unsqueeze(1).broadcast_to([128, 2, 128]), op=ALU.mult),
                        reads=['bpG0', 'mk'], writes=['UoffT'])
                    P.op('dve', lambda e, hs=hs: e.tensor_tensor(
                        out=UoffT[:, hs, 2, :], in0=pG[0][:, :, 0, :],
                        in1=mk[:, mO2T, :].unsqueeze(1).broadcast_to([128, 2, 128]), op=ALU.mult),
                        reads=['bpG0', 'mk'], writes=['UoffT'])
                P.op('dve', lambda e: e.tensor_copy(out=AA[0][:, :, 1, :], in_=ident[:].unsqueeze(1).broadcast_to([128, 4, 128])),
                     reads=['ident'], writes=['AA0'])
                P.op('dve', lambda e: e.tensor_copy(out=AB[0][:, :, 1, :], in_=ident[:].unsqueeze(1).broadcast_to([128, 4, 128])),
                     reads=['ident'], writes=['AB0'])
                cur = 0
                NIT = 4
                for it in range(NIT):
                    nxt = 1 - cur
                    last = (it == NIT - 1)
                    for cc in range(2):
                        gp = pG[cc]
                        hs = slice(cc * 2, cc * 2 + 2)

                        def im(e, cc=cc, cur=cur, gp=gp, last=last):
                            for hh in range(2):
                                h = cc * 2 + hh
                                if last:
                                    ins = e.matmul(gp[:, hh, 1, :], lhsT=AB[cur][:, h, 0, :], rhs=AA[cur][:, h, 1, :], start=True, stop=True)
                                else:
                                    ins = e.matmul(gp[:, hh, :, :], lhsT=AB[cur][:, h, 0, :], rhs=AA[cur][:, h, :, :], start=True, stop=True)
                            return ins
                        P.op('pe', im, reads=[f'AB{cur}', f'AA{cur}'], writes=[f'bpG{cc}'])
                        P.op('dve', lambda e, hs=hs, cur=cur, nxt=nxt, gp=gp: e.tensor_tensor(
                            out=AA[nxt][:, hs, 1, :], in0=gp[:, :, 1, :], in1=AA[cur][:, hs, 1, :], op=ALU.add),
                            reads=[f'bpG{cc}', f'AA{cur}'], writes=[f'AA{nxt}'])
                        if not last:
                            P.op('act', lambda e, hs=hs, nxt=nxt, gp=gp: e.activation(
                                out=AA[nxt][:, hs, 0, :], in_=gp[:, :, 0, :], func=AF.Copy),
                                reads=[f'bpG{cc}'], writes=[f'AA{nxt}'])

                        def im2(e, cc=cc, cur=cur, gp=gp, last=last):
                            for hh in range(2):
                                h = cc * 2 + hh
                                if last:
                                    ins = e.matmul(gp[:, hh, 1, :], lhsT=AA[cur][:, h, 0, :], rhs=AB[cur][:, h, 1, :], start=True, stop=True)
                                else:
                                    ins = e.matmul(gp[:, hh, :, :], lhsT=AA[cur][:, h, 0, :], rhs=AB[cur][:, h, :, :], start=True, stop=True)
                            return ins
                        P.op('pe', im2, reads=[f'AB{cur}', f'AA{cur}'], writes=[f'bpG{cc}'])
                        P.op('dve', lambda e, hs=hs, cur=cur, nxt=nxt, gp=gp: e.tensor_tensor(
                            out=AB[nxt][:, hs, 1, :], in0=gp[:, :, 1, :], in1=AB[cur][:, hs, 1, :], op=ALU.add),
                            reads=[f'bpG{cc}', f'AB{cur}'], writes=[f'AB{nxt}'])
                        if not last:
                            P.op('act', lambda e, hs=hs, nxt=nxt, gp=gp: e.activation(
                                out=AB[nxt][:, hs, 0, :], in_=gp[:, :, 0, :], func=AF.Copy),
                                reads=[f'bpG{cc}'], writes=[f'AB{nxt}'])
                    cur = nxt
                for cc in range(2):
                    gp = pG[cc]
                    hs = slice(cc * 2, cc * 2 + 2)

                    def z1(e, cc=cc, cur=cur, gp=gp):
                        for hh in range(2):
                            h = cc * 2 + hh
                            e.matmul(gp[:, hh, 0, :], lhsT=UoffT[:, h, 0, :], rhs=AA[cur][:, h, 1, :], start=True, stop=True)
                            ins = e.matmul(gp[:, hh, 1, :], lhsT=UoffT[:, h, 1, :], rhs=AB[cur][:, h, 1, :], start=True, stop=True)
                        return ins
                    P.op('pe', z1, reads=['UoffT', f'AA{cur}', f'AB{cur}'], writes=[f'bpG{cc}'])
                    P.op('act', lambda e, hs=hs, gp=gp: e.activation(out=Zs[:, hs, :, :], in_=gp[:], func=AF.Copy),
                         reads=[f'bpG{cc}'], writes=['Zs'])

                    def t1(e, cc=cc, cur=cur, gp=gp):
                        for hh in range(2):
                            h = cc * 2 + hh
                            e.matmul(gp[:, hh, 0, :], lhsT=AB[cur][:, h, 1, :], rhs=Zs[:, h, 0, :], start=True, stop=True)
                            ins = e.matmul(gp[:, hh, 1, :], lhsT=AA[cur][:, h, 1, :], rhs=Zs[:, h, 1, :], start=True, stop=True)
                        return ins
                    P.op('pe', t1, reads=['Zs', f'AA{cur}', f'AB{cur}'], writes=[f'bpG{cc}'])
                    P.op('dve', lambda e, hs=hs, cur=cur, gp=gp: e.tensor_tensor(
                        out=T32[:, hs, 0, :], in0=gp[:, :, 0, :], in1=AA[cur][:, hs, 1, :], op=ALU.add),
                        reads=[f'bpG{cc}', f'AA{cur}'], writes=['T32'])
                    P.op('dve', lambda e, hs=hs, cur=cur, gp=gp: e.tensor_tensor(
                        out=T32[:, hs, 1, :], in0=gp[:, :, 1, :], in1=AB[cur][:, hs, 1, :], op=ALU.add),
                        reads=[f'bpG{cc}', f'AB{cur}'], writes=['T32'])

                    def z2(e, cc=cc, gp=gp):
                        for hh in range(2):
                            h = cc * 2 + hh
                            ins = e.matmul(gp[:, hh, 0, :], lhsT=UoffT[:, h, 2, :], rhs=T32[:, h, 0, :], start=True, stop=True)
                        return ins
                    P.op('pe', z2, reads=['UoffT', 'T32'], writes=[f'bpG{cc}'])
                    P.op('act', lambda e, hs=hs, gp=gp: e.activation(out=Zs[:, hs, 0, :], in_=gp[:, :, 0, :], func=AF.Copy),
                         reads=[f'bpG{cc}'], writes=['Zs'])

                    def t2(e, cc=cc, gp=gp):
                        for hh in range(2):
                            h = cc * 2 + hh
                            ins = e.matmul(gp[:, hh, 1, :], lhsT=T32[:, h, 1, :], rhs=Zs[:, h, 0, :], start=True, stop=True)
                        return ins
                    P.op('pe', t2, reads=['Zs', 'T32'], writes=[f'bpG{cc}'])
                    P.op('dve', lambda e, hs=hs, gp=gp: e.tensor_tensor(
                        out=Tfin[:, hs, :], in0=gp[:, :, 1, :], in1=T32[:, hs, 0, :], op=ALU.add),
                        reads=[f'bpG{cc}', 'T32'], writes=['Tfin'])
                def xm(e, tt=tt):
                    for h in range(4):
                        ins = e.matmul(pX[:, h, 0, :], lhsT=U2[:, h, 0, :], rhs=vtok[:, tt, h * 64:(h + 1) * 64], start=True, stop=True)
                    return ins
                P.op('pe', xm, reads=['U2', 'vtok'], writes=['bpX'])
                P.op('act', lambda e: e.activation(out=XA[:, :, 0, :], in_=pX[:, :, 0, :], func=AF.Copy), reads=['bpX'], writes=['XA'])

                def wm(e):
                    for h in range(4):
                        ins = e.matmul(pX[:, h, :, :], lhsT=Tfin[:, h, :], rhs=XA[:, h, :, :], start=True, stop=True)
                    return ins
                P.op('pe', wm, reads=['Tfin', 'XA'], writes=['bpX'])
                P.op('act', lambda e: e.activation(out=WA[:].rearrange("p a h k -> p h a k"), in_=pX, func=AF.Copy), reads=['bpX'], writes=['WA'])

                def rm(e):
                    for cc in range(2):
                        for hh in range(2):
                            h = cc * 2 + hh
                            hp = hh * 64
                            ins = e.matmul(pG[0][hp:hp + 64, cc, 0, :], lhsT=WA[:, 1, h, :], rhs=U1[:, h, 1, :], start=True, stop=True)
                    return ins
                P.op('pe', rm, reads=['WA', 'U1'], writes=['bpG0'])
                P.op('dve', lambda e, cs=cs: e.tensor_tensor(out=RtT[:], in0=pG[0][:, :, 0, :], in1=arT[:, :, 1, cs], op=ALU.add),
                     reads=['bpG0', 'arT'], writes=['RtT'])
                for c in range(2):
                    pc = slice(c * 64, (c + 1) * 64)
                    pMc = pG[1 - c][:].rearrange("p a b t -> p (a b) t")

                    def mmm(e, c=c, pc=pc, pMc=pMc):
                        for cc in range(2):
                            ins = e.matmul(pMc[:, cc, :], lhsT=WA[pc, 1, cc * 2:cc * 2 + 2, :].rearrange("p h k -> p (h k)"),
                                           rhs=tk[pc, 1, cc * 128:(cc + 1) * 128], start=True, stop=True)
                        return ins
                    P.op('pe', mmm, reads=['WA', 'tk'], writes=[f'bpG{1 - c}'])
                    for cc in range(2):
                        P.op('dve', lambda e, c=c, cc=cc, tt=tt, pMc=pMc: e.scalar_tensor_tensor(
                            out=Msb[:, cc, c, :], in0=ident[:], scalar=gC[:, cc, tt * 2 + c:tt * 2 + c + 1], in1=pMc[:, cc, :],
                            op0=ALU.mult, op1=ALU.add), reads=[f'bpG{1 - c}', 'gC', 'ident'], writes=['Msb'])
                if not is_ctx:
                    def y0m(e, tt=tt):
                        for h in range(4):
                            e.matmul(pY[:, h, :], lhsT=U1[:, h, 1, :], rhs=WA[:, 0, h, :], start=(h == 0), stop=False, skip_group_check=True)
                            ins = e.matmul(pY[:, h, :], lhsT=U2[:, h, 1, :], rhs=vtok[:, tt, h * 64:(h + 1) * 64], start=False, stop=False,
                                           skip_group_check=True)
                        return ins
                    P.op('pe', y0m, reads=['U1', 'U2', 'WA', 'vtok'], writes=['bpY'])
                for ci, c in enumerate(corder):
                    pc = slice(c * 64, (c + 1) * 64)
                    if not is_ctx:
                        def ycm(e, pc=pc, ci=ci):
                            for cc in range(2):
                                ins = e.matmul(pYf[pc, cc * 128:(cc + 1) * 128], lhsT=RtT[:, cc, pc], rhs=Sbd[:, cc, :], start=False,
                                               stop=(ci == 1), skip_group_check=True)
                            return ins
                        P.op('pe', ycm, reads=['RtT', 'Sbd'], writes=['bpY'])

                    def scm(e, pc=pc, c=c, tt=tt):
                        for cc in range(2):
                            o_ = pSf[:, cc * 128:(cc + 1) * 128]
                            e.matmul(o_, lhsT=tk[pc, 1, cc * 128:(cc + 1) * 128], rhs=WA[pc, 0, cc * 2:cc * 2 + 2, :].rearrange("p h k -> p (h k)"),
                                     start=(cc == 0), stop=False, skip_group_check=True)
                            e.matmul(o_, lhsT=tk[pc, 2, cc * 128:(cc + 1) * 128], rhs=vtok[pc, tt, cc * 128:(cc + 1) * 128],
                                     start=False, stop=False, skip_group_check=True)
                            ins = e.matmul(o_, lhsT=Msb[:, cc, c, :], rhs=Sbd[:, cc, :], start=False, stop=True, skip_group_check=True)
                        return ins
                    P.op('pe', scm, reads=['tk', 'WA', 'vtok', 'Msb', 'Sbd'], writes=['bpS'])
                    pS3 = pSf[:, 0:256].rearrange("p (c x) -> p c x", c=2)
                    P.op('act', lambda e, pS3=pS3: e.activation(out=Sbd[0:64, :, 0:64], in_=pS3[0:64, :, 0:64], func=AF.Copy),
                         reads=['bpS'], writes=['Sbd'])
                    P.op('dve', lambda e, pS3=pS3: e.tensor_copy(out=Sbd[64:128, :, 64:128], in_=pS3[64:128, :, 64:128]),
                         reads=['bpS'], writes=['Sbd'])
                if is_ctx:
                    continue
                row0 = tok0 - 256 + tt * 128
                if not final:
                    P.op('dve', lambda e: e.tensor_copy(out=ysb[:], in_=pYf[:, 0:256]), reads=['bpY'], writes=['ysb'])
                    P.dma('sp', lambda e, row0=row0: e.dma_start(out=T['y0'][row0:row0 + 128, :], in_=ysb[:]), reads=['ysb'], writes=['y0'])
                    P.dma('sp', lambda e, row0=row0, tt=tt: e.dma_start(out=T['bon0'][row0:row0 + 128, :], in_=bon[:, tt, :]), reads=['bon'], writes=['bon0'])
                else:
                    P.dma('sp', lambda e, row0=row0: e.dma_start(out=y0t[:], in_=T['y0'][row0:row0 + 128, :]), reads=['y0'], writes=['y0t'])
                    P.dma('sp', lambda e, row0=row0: e.dma_start(out=b0t[:], in_=T['bon0'][row0:row0 + 128, :]), reads=['bon0'], writes=['b0t'])
                    P.op('dve', lambda e: e.tensor_tensor(out=ysb[:], in0=pYf[:, 0:256], in1=y0t[:], op=ALU.add),
                         reads=['bpY', 'y0t'], writes=['ysb'])
                    P.op('dve', lambda e, tt=tt: e.tensor_tensor(out=b0t[:], in0=b0t[:], in1=bon[:, tt, :], op=ALU.add), reads=['b0t', 'bon'], writes=['b0t'])
                    y3 = ysb[:].rearrange("p (h v) -> p h v", h=4)
                    P.op('dve', lambda e: e.tensor_reduce(out=st8[:, 0:4], in_=y3, axis=AX.X, op=ALU.add), reads=['ysb'], writes=['st8'])
                    P.op('dve', lambda e: e.tensor_scalar(out=st8[:, 0:4], in0=st8[:, 0:4], scalar1=1.0 / 64, scalar2=None, op0=ALU.mult),
                         reads=['st8'], writes=['st8'])
                    P.op('dve', lambda e: e.tensor_tensor(out=y3, in0=y3, in1=st8[:, 0:4].unsqueeze(2).broadcast_to([128, 4, 64]), op=ALU.subtract),
                         reads=['ysb', 'st8'], writes=['ysb'])
                    P.op('dve', lambda e: e.tensor_tensor(out=osb[:], in0=ysb[:], in1=ysb[:], op=ALU.mult), reads=['ysb'], writes=['osb'])
                    P.op('dve', lambda e: e.tensor_reduce(out=st8[:, 4:8], in_=osb[:].rearrange("p (h v) -> p h v", h=4), axis=AX.X, op=ALU.add),
                         reads=['osb'], writes=['st8'])
                    P.op('act', lambda e: e.activation(out=st8[:, 4:8], in_=st8[:, 4:8], func=AF.Ln, scale=1.0 / 64, bias=64e-5), reads=['st8'], writes=['st8'])
                    P.op('act', lambda e: e.activation(out=st8[:, 4:8], in_=st8[:, 4:8], func=AF.Exp, scale=-0.5), reads=['st8'], writes=['st8'])
                    P.op('dve', lambda e: e.tensor_tensor(out=y3, in0=y3, in1=st8[:, 4:8].unsqueeze(2).broadcast_to([128, 4, 64]), op=ALU.mult),
                         reads=['ysb', 'st8'], writes=['ysb'])
                    P.op('dve', lambda e: e.tensor_tensor(out=ysb[:], in0=ysb[:], in1=lnrep[:, 0, :], op=ALU.mult), reads=['ysb', 'lnrep'], writes=['ysb'])
                    P.op('dve', lambda e: e.tensor_tensor(out=ysb[:], in0=ysb[:], in1=lnrep[:, 1, :], op=ALU.add), reads=['ysb', 'lnrep'], writes=['ysb'])
                    P.op('dve', lambda e, tt=tt: e.tensor_tensor(
                        out=osb[:].rearrange("p (h v) -> p h v", h=4), in0=vtok[:, tt, :].rearrange("p (h v) -> p h v", h=4),
                        in1=b0t[:, 0:4].unsqueeze(2).broadcast_to([128, 4, 64]), op=ALU.mult), reads=['vtok', 'b0t'], writes=['osb'])
                    P.op('dve', lambda e: e.tensor_tensor(out=osb[:], in0=osb[:], in1=ysb[:], op=ALU.add), reads=['osb', 'ysb'], writes=['osb'])
                    P.op('dve', lambda e, tt=tt: e.tensor_tensor(out=osb[:], in0=osb[:], in1=gtok[:, tt, :], op=ALU.mult), reads=['osb', 'gtok'], writes=['osb'])
                    P.dma('sp', lambda e, row0=row0: e.dma_start(out=T['og'][row0:row0 + 128, :], in_=osb[:]), reads=['osb'], writes=['og'])

        for d in range(2):
            P.op('dve', lambda e: e.memset(Sbd[:], 0.0), writes=['Sbd'])
            do_segment(0, 256, True, False, False, d, d == 1)
            segs = list(range(NL)) if d == 0 else list(range(NL - 1, -1, -1))
            for si in segs:
                do_segment(256 + si * 512, 512, False, si > 0, si < NL - 1, d, d == 1)
        P.barrier()


def build_B(NL):
    nc = bass.Bass("TRN2", target_bir_lowering=False)
    def din(name, shape):
        return nc.dram_tensor(name, shape, F32, kind="ExternalInput").ap()
    T = {}
    NTOK = 256 + NL * 512
    T['xs'] = din("xs", [NTOK, 1024])
    T['xmixT'] = din("xmixT", [128, 8, 6])
    T['wrkv'] = din("wrkv", [1024, 768])
    T['w1cat'] = din("w1cat", [1024, 128])
    T['a1cat'] = din("a1cat", [1024, 128])
    T['g1'] = din("g1", [1024, 160])
    T['w2cat'] = din("w2cat", [128, 256])
    T['a2cat'] = din("a2cat", [128, 256])
    T['g2a'] = din("g2a", [128, 256])
    T['g2b'] = din("g2b", [32, 256])
    T['colvec'] = din("colvec", [128, 2, 6])
    T['rkw'] = din("rkw", [128, 2, 2])
    T['lnrep'] = din("lnrep", [128, 2, 256])
    T['mk'] = din("mk", [10, 128, 128])
    cT = din("cT", [128, 8, 2])
    ada_w = din("ada_w", [1024, 6144])
    ada_bT = din("ada_bT", [128, 48])
    adab_rep = din("adab_rep", [128, 2, 1024])
    gmixT = din("gmixT", [128, 8])
    gffnT = din("gffnT", [128, 8])
    dout = lambda name, shape: nc.dram_tensor(name, shape, F32, kind="ExternalOutput").ap()
    T['y0'] = dout("y0", [NL * 512, 256])
    T['bon0'] = dout("bon0", [NL * 512, 4])
    T['og'] = dout("og", [NL * 512, 256])
    T['gdram'] = dout("gdram", [2, 128, 2, 1024])
    P = Prog(nc)
    P.psum_keys = PSUM_KEYS
    with ExitStack() as st:
        sb = _sb(nc, st)
        C = make_consts(P, nc, sb)
        M = mod_setup(P, nc, st, C, cT, ada_w, ada_bT, adab_rep, gmixT, gffnT, T['gdram'])
        rwkv_phase(P, nc, C, M, T, NL)
        P.final_wait('sp')
        P.emit(st)
    print("B ops:", P.nops, {e: len(P.streams[e]) for e in P.ENG})
    return nc


def host_B(inp, xs_b, core):
    b, hg = core // 4, core % 4
    cols = slice(hg * 256, (hg + 1) * 256)
    m = {}
    m['xs'] = xs_b
    m['xmixT'] = inp['rwkv_x_mix'][0].reshape(6, 8, 128).transpose(2, 1, 0)
    m['wrkv'] = np.concatenate([inp['rwkv_w_r'][0][:, cols], inp['rwkv_w_k'][0][:, cols], inp['rwkv_w_v'][0][:, cols]], 1)
    m['w1cat'] = np.concatenate([inp['rwkv_decay_w1'][0, 0], inp['rwkv_decay_w1'][0, 1]], 1)
    m['a1cat'] = np.concatenate([inp['rwkv_iclr_a1'][0, 0], inp['rwkv_iclr_a1'][0, 1]], 1)
    m['g1'] = inp['rwkv_gate_g1'][0]
    m['w2cat'] = np.concatenate([inp['rwkv_decay_w2'][0, 0][:, cols], inp['rwkv_decay_w2'][0, 1][:, cols]], 0)
    m['a2cat'] = np.concatenate([inp['rwkv_iclr_a2'][0, 0][:, cols], inp['rwkv_iclr_a2'][0, 1][:, cols]], 0)
    g2 = inp['rwkv_gate_g2'][0][:, cols]
    m['g2a'] = g2[0:128]
    m['g2b'] = g2[128:160]
    vecs = [inp['rwkv_decay_w0'][0, 0], inp['rwkv_decay_w0'][0, 1], inp['rwkv_iclr_a0'][0, 0], inp['rwkv_iclr_a0'][0, 1],
            inp['rwkv_k_k'][0], inp['rwkv_k_a'][0]]
    cv = np.stack([v[cols].reshape(2, 128) for v in vecs], 2)
    m['colvec'] = cv.transpose(1, 0, 2)
    rkw = np.zeros((128, 2, 2), np.float32)
    for cc in range(2):
        for hh in range(2):
            rkw[hh * 64:(hh + 1) * 64, cc, hh] = inp['rwkv_r_k'][0][hg * 4 + cc * 2 + hh]
    m['rkw'] = rkw
    m['lnrep'] = np.broadcast_to(np.stack([inp['rwkv_ln_g'][0][cols], inp['rwkv_ln_b'][0][cols]])[None], (128, 2, 256))
    r = np.arange(128)[:, None]
    c = np.arange(128)[None, :]
    b64 = (r // 64) == (c // 64)
    b32 = (r // 32) == (c // 32)
    b16 = (r // 16) == (c // 16)
    S64 = (r < c) & b64
    I64 = (r <= c) & b64
    S16 = (r < c) & b16
    O1 = (r < c) & b32 & ~b16
    O2 = (r < c) & b64 & ~b32
    m['mk'] = np.stack([S64, I64, S64.T, I64.T, S16, S16.T, O1, O1.T, O2, O2.T]).astype(np.float32)
    m.update(host_mod(inp, b, 1))
    return {k_: np.ascontiguousarray(v, dtype=np.float32) for k_, v in m.items()}


def wo_phase(P, nc, C, T, NT):
    with ExitStack() as st:
        sb = _sb(nc, st)
        ps = _ps(nc, st)
        wo = sb("wo", [128, 8, 1024], BF16)
        stg = [sb(f"cstg{i}", [128, 1024], F32) for i in range(2)]
        g2rep = sb("cg2rep", [128, 1024], F32)
        ot32 = [sb(f"ot32_{i}", [128, 8, 128], F32) for i in range(2)]
        otb = [sb(f"otb_{i}", [128, 8, 128], BF16) for i in range(2)]
        xt = [sb(f"cxt{i}", [128, 1024], F32) for i in range(2)]
        ty = [sb(f"cty{i}", [128, 1024], F32) for i in range(2)]
        pA = [ps(f"cpA{i}", [128, 512], F32) for i in range(2)]
        P.dma('sp', lambda e: e.dma_start(out=g2rep[:], in_=T['gdram'][0, :, 0, :]), reads=['g_dram'], writes=['cg2rep'])
        wsrc = T['wo'].rearrange("(c p) n -> p c n", p=128)
        for k in range(8):
            P.dma('sp', lambda e, k=k: e.dma_start(out=stg[k % 2][:], in_=wsrc[:, k, :]), writes=[f'cstg{k % 2}'])
            P.op('pool', lambda e, k=k: e.tensor_copy(out=wo[:, k, :], in_=stg[k % 2][:]), reads=[f'cstg{k % 2}'], writes=['wo'])
        osrc = T['oT'].rearrange("(c p) n -> p c n", p=128)
        for t in range(NT):
            i = t % 2
            P.dma('sp', lambda e, t=t, i=i: e.dma_start(out=ot32[i][:], in_=osrc[:, :, t * 128:(t + 1) * 128]), writes=[f'ot32_{i}'])
            P.dma('sp', lambda e, t=t, i=i: e.dma_start(out=xt[i][:], in_=T['xin'][t * 128:(t + 1) * 128, :]), writes=[f'cxt{i}'])
            P.op('pool', lambda e, i=i: e.tensor_copy(out=otb[i][:], in_=ot32[i][:]), reads=[f'ot32_{i}'], writes=[f'otb_{i}'])
            for half in range(2):
                pa = pA[half]

                def om(e, i=i, half=half, pa=pa):
                    for k in range(8):
                        ins = e.matmul(pa[:], lhsT=otb[i][:, k, :], rhs=wo[:, k, half * 512:(half + 1) * 512], start=(k == 0), stop=(k == 7))
                    return ins
                P.op('pe', om, reads=[f'otb_{i}', 'wo'], writes=[f'cpA{half}'])
                P.op('dve', lambda e, i=i, half=half, pa=pa: e.tensor_tensor(
                    out=ty[i][:, half * 512:(half + 1) * 512], in0=pa[:], in1=g2rep[:, half * 512:(half + 1) * 512], op=ALU.mult),
                    reads=[f'cpA{half}', 'cg2rep'], writes=[f'cty{i}'])
            P.op('dve', lambda e, i=i: e.tensor_tensor(out=ty[i][:], in0=ty[i][:], in1=xt[i][:], op=ALU.add),
                 reads=[f'cty{i}', f'cxt{i}'], writes=[f'cty{i}'])
            P.dma('sp', lambda e, t=t, i=i: e.dma_start(out=T['x3'][t * 128:(t + 1) * 128, :], in_=ty[i][:]), reads=[f'cty{i}'], writes=['x3'])
        P.barrier()


def build_C(NT, n_exp=16):
    nc = bass.Bass("TRN2", target_bir_lowering=False)
    def din(name, shape):
        return nc.dram_tensor(name, shape, F32, kind="ExternalInput").ap()
    dout = lambda name, shape: nc.dram_tensor(name, shape, F32, kind="ExternalOutput").ap()
    T = {}
    T['oT'] = din("oT", [1024, NT * 128])
    T['wo'] = din("wo", [1024, 1024])
    T['xin'] = din("xin", [NT * 128, 1024])
    cT = din("cT", [128, 8, 2])
    ada_w = din("ada_w", [1024, 6144])
    ada_bT = din("ada_bT", [128, 48])
    adab_rep = din("adab_rep", [128, 2, 1024])
    gmixT = din("gmixT", [128, 8])
    gffnT = din("gffnT", [128, 8])
    T['rw'] = din("rw", [128, 8, 16])
    T['rbias'] = din("rbias", [128, 16])
    T['w1'] = din("w1", [n_exp, 1024, 1024])
    T['w3'] = din("w3", [n_exp, 1024, 1024])
    T['w2'] = din("w2", [n_exp, 1024, 1024])
    T['fng'] = din("fng", [128, 1024])
    T['x3'] = dout("x3", [NT * 128, 1024])
    T['out'] = dout("out", [NT * 128, 1024])
    T['gdram'] = dout("gdram", [2, 128, 2, 1024])
    P = Prog(nc)
    P.psum_keys = PSUM_KEYS
    with ExitStack() as st:
        sb = _sb(nc, st)
        C = make_consts(P, nc, sb)
        M = mod_setup(P, nc, st, C, cT, ada_w, ada_bT, adab_rep, gmixT, gffnT, T['gdram'])
        wo_phase(P, nc, C, T, NT)
        T['x_in'] = T['x3']
        T['x_out'] = T['out']
        moe_phase(P, nc, C, M, T, NT, lambda t: 0, "m", final_norm=True)
        P.final_wait('sp')
        P.emit(st)
    print("C ops:", P.nops, {e: len(P.streams[e]) for e in P.ENG})
    return nc


def host_C(inp, o_own, xl2_own, b):
    m = {}
    m['oT'] = o_own.T
    m['wo'] = inp['rwkv_w_o'][0]
    m['xin'] = xl2_own
    m['fng'] = np.broadcast_to(inp['final_norm_g'][None], (128, 1024))
    m.update(host_mod(inp, b, 1))
    m.update(host_moe(inp, 1))
    return {k_: np.ascontiguousarray(v, dtype=np.float32) for k_, v in m.items()}


def kernel(**inp):
    inp = {k: np.asarray(v) for k, v in inp.items()}
    S = 16384
    cfg = Cfg(S)
    OWN = cfg.OWN
    cores = list(range(8))
    ncA = build_A(cfg)
    mapsA = [host_A(inp, cfg, c) for c in cores]
    resA = run_bass_kernel_spmd(ncA, mapsA, core_ids=cores).results
    x2 = [np.asarray(resA[c]['x2']) for c in cores]
    del mapsA, resA
    xs = [np.concatenate([x2[b * 4][:256]] + [x2[b * 4 + q][256:] for q in range(4)], 0) for b in range(2)]
    ncB = build_B(S // 512)
    mapsB = [host_B(inp, xs[c // 4], c) for c in cores]
    resB = run_bass_kernel_spmd(ncB, mapsB, core_ids=cores).results
    o = [np.concatenate([np.asarray(resB[b * 4 + hg]['og']) for hg in range(4)], 1) for b in range(2)]
    del mapsB, resB
    ncC = build_C(OWN // 128)
    mapsC = [host_C(inp, o[c // 4][(c % 4) * OWN:(c % 4 + 1) * OWN], x2[c][256:], c // 4) for c in cores]
    resC = run_bass_kernel_spmd(ncC, mapsC, core_ids=cores).results
    out = np.stack([np.concatenate([np.asarray(resC[b * 4 + q]['out']) for q in range(4)], 0) for b in range(2)])
    return np.ascontiguousarray(out, dtype=np.float32)
```

```python
import numpy as np
from contextlib import ExitStack
import concourse.bass as bass
import concourse.mybir as mybir
from concourse.bass_utils import run_bass_kernel_spmd


F32 = mybir.dt.float32
BF16 = mybir.dt.bfloat16
I32 = mybir.dt.int32
U32 = mybir.dt.uint32
AF = mybir.ActivationFunctionType
ALU = mybir.AluOpType
AX = mybir.AxisListType

EPOCH = 20000
NDMA = 6


class Prog:
    ENG = ('pe', 'act', 'dve', 'pool', 'sp')

    def __init__(self, nc):
        self.nc = nc
        self.streams = {e: [] for e in self.ENG}
        self.cnt = {e: 0 for e in self.ENG}
        self.last_w = {}
        self.readers = {}
        self.known = {e: {} for e in self.ENG}
        self.dma_rr = {e: 0 for e in self.ENG}
        self.dma_cnt = {}
        self.extra = {e: [] for e in self.ENG}
        self.nops = 0
        self.psum_keys = set()

    def _deps(self, reads, writes):
        deps = []
        for k in reads:
            w = self.last_w.get(k)
            if w is not None:
                deps.append(w)
        for k in writes:
            w = self.last_w.get(k)
            if w is not None:
                deps.append(w)
            deps.extend(self.readers.get(k, ()))
        return deps

    def _resolve(self, eng, deps):
        need = {}
        for t in deps:
            if t[0] == 'c':
                if t[1] == 'pe' and eng == 'pe':
                    continue
                key = ('c', t[1])
                val = t[2]
            else:
                key = ('d', t[1], t[2])
                val = t[3]
            if val > need.get(key, -1):
                need[key] = val
        out = []
        kn = self.known[eng]
        for key, val in need.items():
            if kn.get(key, -1) >= val:
                continue
            kn[key] = val
            out.append((key, val))
        return out

    def _record(self, tok, reads, writes):
        for k in reads:
            self.readers.setdefault(k, []).append(tok)
        for k in writes:
            self.last_w[k] = tok
            self.readers[k] = []

    def op(self, eng, fn, reads=(), writes=()):
        pr = [k for k in reads if k in self.psum_keys]
        if pr:
            writes = list(writes) + pr
        deps = self._deps(reads, writes) + self.extra[eng]
        self.extra[eng] = []
        waits = self._resolve(eng, deps)
        idx = self.cnt[eng]
        self.cnt[eng] += 1
        tok = ('c', eng, idx)
        self.streams[eng].append((waits, fn, tok))
        self._record(tok, reads, writes)
        self.nops += 1
        return tok

    def dma(self, q, fn, reads=(), writes=()):
        k = self.dma_rr[q]
        self.dma_rr[q] = (k + 1) % NDMA
        m = self.dma_cnt.get((q, k), 0)
        deps = self._deps(reads, writes) + self.extra[q]
        self.extra[q] = []
        if m > 0:
            deps.append(('d', q, k, m - 1))
        waits = self._resolve(q, deps)
        self.dma_cnt[(q, k)] = m + 1
        tok = ('d', q, k, m)
        self.streams[q].append((waits, fn, tok))
        self._record(tok, reads, writes)
        self.nops += 1
        return tok

    def barrier(self):
        snap = []
        for e in self.ENG:
            if self.cnt[e] > 0:
                snap.append(('c', e, self.cnt[e] - 1))
        for (q, k), m in self.dma_cnt.items():
            snap.append(('d', q, k, m - 1))
        for e in self.ENG:
            self.extra[e] = self.extra[e] + snap

    def final_wait(self, eng='sp'):
        self.barrier()
        waits = self._resolve(eng, self.extra[eng])
        self.extra[eng] = []
        self.streams[eng].append((waits, None, None))

    def emit(self, stack):
        nc = self.nc
        csem = {}
        for e in self.ENG:
            ne = (self.cnt[e] + EPOCH - 1) // EPOCH
            for j in range(ne):
                csem[(e, j)] = stack.enter_context(nc.semaphore(f"c_{e}_{j}"))
        dsem = {}
        for (q, k) in self.dma_cnt:
            dsem[(q, k)] = stack.enter_context(nc.semaphore(f"d_{q}_{k}"))
        block = stack.enter_context(nc.Block())

        def run(ename, engine):
            for waits, fn, tok in self.streams[ename]:
                for key, val in waits:
                    if key[0] == 'c':
                        engine.wait_ge(csem[(key[1], val // EPOCH)], val % EPOCH + 1)
                    else:
                        engine.wait_ge(dsem[(key[1], key[2])], 16 * (val + 1))
                if fn is None:
                    continue
                ins = fn(engine)
                if tok[0] == 'c':
                    ins.then_inc(csem[(tok[1], tok[2] // EPOCH)], 1)
                else:
                    ins.then_inc(dsem[(tok[1], tok[2])], 16)

        @block.sync
        def _(e):
            run('sp', e)

        @block.tensor
        def _(e):
            run('pe', e)

        @block.scalar
        def _(e):
            run('act', e)

        @block.vector
        def _(e):
            run('dve', e)

        @block.gpsimd
        def _(e):
            run('pool', e)


D = 1024
RMS_EPS = 1e-6


class StopBuild(Exception):
    pass

STOP = [10 ** 9]

def CHK(n):
    if n >= STOP[0]:
        raise StopBuild()


class Scope:
    def __enter__(self):
        self.st = ExitStack()
        self.st.__enter__()
        self.stopped = False
        return self

    def __exit__(self, et, ev, tb):
        if et is StopBuild:
            self.st.__exit__(None, None, None)
            self.stopped = True
            return True
        return self.st.__exit__(et, ev, tb)


class Cfg:
    def __init__(self, S):
        self.S = S
        self.OWN = S // 4
        self.TO = 256 + 128 + self.OWN + 128
        self.TR = S - self.OWN
        assert self.TO % 512 == 0 and self.TR % 512 == 0
        self.NSO = self.TO // 512
        self.NSR = self.TR // 512
        self.NTO = self.TO // 128
        self.NKT = (self.TO + self.TR) // 128
        self.TQ = 256 + self.OWN
        self.NTQ = self.TQ // 128


def _sb(nc, st):
    return lambda name, shape, dt: st.enter_context(nc.sbuf_tensor("s_" + name, shape, dt))


PSUM_KEYS = set()


def _ps(nc, st):
    def f(name, shape, dt):
        PSUM_KEYS.add(name)
        return st.enter_context(nc.psum_tensor("p_" + name, shape, dt))
    return f


def make_consts(P, nc, sb):
    C = {}
    C['ident'] = sb("ident", [128, 128], F32)
    C['identb'] = sb("identb", [128, 128], BF16)
    C['bones'] = sb("bones", [128, 128], BF16)
    C['ones'] = sb("ones", [128, 128], F32)
    i32 = C['ident']
    P.op('pool', lambda e: e.memset(i32[:], 0.0), writes=['ident'])
    P.op('pool', lambda e: e.affine_select(out=i32[:], in_=i32[:], pattern=[[-1, 128]], base=0,
                                           channel_multiplier=1, compare_op=ALU.not_equal, fill=1.0),
         reads=['ident'], writes=['ident'])
    P.op('pool', lambda e: e.tensor_copy(out=C['identb'][:], in_=i32[:]), reads=['ident'], writes=['identb'])
    P.op('pool', lambda e: e.memset(C['ones'][:], 1.0), writes=['ones'])
    bo = C['bones']
    P.op('pool', lambda e: e.memset(bo[:], 0.0), writes=['bones'])
    P.op('pool', lambda e: e.memset(bo[0:64, 0:64], 1.0), reads=['bones'], writes=['bones'])
    P.op('pool', lambda e: e.memset(bo[64:128, 64:128], 1.0), reads=['bones'], writes=['bones'])
    return C


def mod_setup(P, nc, st_outer, C, cT, ada_w, ada_bT, adab_rep, gmixT, gffnT, g5_dram, tagp=""):
    sbo = _sb(nc, st_outer)
    M = {}
    for n in ('Gm', 'Sm', 'Gf', 'Sf'):
        M[n] = sbo(tagp + n, [128, 8, 2], F32)
    with ExitStack() as st:
        sb = _sb(nc, st)
        ps = _ps(nc, st)
        scT = sb("scT", [128, 8, 2], F32)
        screp = sb("screp", [128, 8, 2, 128], F32)
        modT = sb("modT", [128, 48, 2], F32)
        abT = sb("abT", [128, 48], F32)
        gmix = sb("gmix", [128, 8], F32)
        gffn = sb("gffn", [128, 8], F32)
        abrep = sb("abrep", [128, 2, 1024], F32)
        g5t = sb("g5t", [128, 2, 1024], F32)
        g2t = sb("g2t", [128, 2, 1024], F32)
        wblk = [sb(f"wblk{i}", [128, 8, 512], F32) for i in range(2)]
        pmod_full = ps("pmod", [128, 512], F32)
        pmod = pmod_full[:, 0:96].rearrange("p (a b) -> p a b", b=2)
        pg = [ps(f"pg{i}", [128, 512], F32) for i in range(2)]
        P.dma('sp', lambda e: e.dma_start(out=scT[:], in_=cT[:, :, :]), writes=['scT'])
        P.dma('sp', lambda e: e.dma_start(out=abT[:], in_=ada_bT[:, :]), writes=['abT'])
        P.dma('sp', lambda e: e.dma_start(out=gmix[:], in_=gmixT[:, :]), writes=['gmix'])
        P.dma('sp', lambda e: e.dma_start(out=gffn[:], in_=gffnT[:, :]), writes=['gffn'])
        P.dma('sp', lambda e: e.dma_start(out=abrep[:], in_=adab_rep[:, :, :]), writes=['abrep'])
        P.op('act', lambda e: e.activation(out=scT[:], in_=scT[:], func=AF.Silu), reads=['scT'], writes=['scT'])
        for cl in range(2):
            P.op('dve', lambda e, cl=cl: e.tensor_copy(
                out=screp[:, :, cl, :], in_=scT[:, :, cl:cl + 1].broadcast_to([128, 8, 128])),
                reads=['scT'], writes=['screp'])
        for blk in range(12):
            wb = wblk[blk % 2]
            wk = f"wblk{blk % 2}"
            P.dma('sp', lambda e, wb=wb, blk=blk: e.dma_start(
                out=wb[:], in_=ada_w[:, blk * 512:(blk + 1) * 512].rearrange("(c p) n -> p c n", p=128)),
                writes=[wk])
            j = blk // 2
            half = blk % 2

            def fm(e, wb=wb, j=j, half=half):
                for fc in range(4):
                    for kc in range(8):
                        ins = e.matmul(pmod[:, j * 8 + half * 4 + fc, :], lhsT=wb[:, kc, fc * 128:(fc + 1) * 128],
                                       rhs=scT[:, kc, :], start=(kc == 0), stop=(kc == 7))
                return ins
            P.op('pe', fm, reads=[wk, 'scT'], writes=['pmod'])
            if j in (2, 5):
                gi = 0 if j == 2 else 1
                dst = g2t if j == 2 else g5t
                for cl in range(2):
                    pgt = pg[cl]

                    def rm(e, wb=wb, cl=cl, pgt=pgt):
                        for kc in range(8):
                            ins = e.matmul(pgt[:], lhsT=screp[:, kc, cl, :], rhs=wb[:, kc, :],
                                           start=(kc == 0), stop=(kc == 7))
                        return ins
                    P.op('pe', rm, reads=[wk, 'screp'], writes=[f'pg{cl}'])
                    P.op('dve', lambda e, pgt=pgt, dst=dst, cl=cl, gi=gi, half=half: e.tensor_tensor(
                        out=dst[:, cl, half * 512:(half + 1) * 512], in0=pgt[:],
                        in1=abrep[:, gi, half * 512:(half + 1) * 512], op=ALU.add),
                        reads=[f'pg{cl}', 'abrep'], writes=['grep'])
        P.dma('sp', lambda e: e.dma_start(out=g5_dram[1, :, :, :], in_=g5t[:]), reads=['grep'], writes=['g_dram'])
        P.dma('sp', lambda e: e.dma_start(out=g5_dram[0, :, :, :], in_=g2t[:]), reads=['grep'], writes=['g_dram'])
        P.op('dve', lambda e: e.tensor_tensor(out=modT[:], in0=pmod[:],
                                              in1=abT[:, :].unsqueeze(2).broadcast_to([128, 48, 2]), op=ALU.add),
             reads=['pmod', 'abT'], writes=['modT'])
        for (Gn, Sn, gg, jsh, jsc) in (('Gm', 'Sm', gmix, 0, 1), ('Gf', 'Sf', gffn, 3, 4)):
            Gt = M[Gn]
            St = M[Sn]
            P.op('dve', lambda e, Gt=Gt, jsc=jsc: e.tensor_scalar(
                out=Gt[:], in0=modT[:, jsc * 8:(jsc + 1) * 8, :], scalar1=1.0, scalar2=None, op0=ALU.add),
                reads=['modT'], writes=[tagp + Gn])
            P.op('dve', lambda e, Gt=Gt, gg=gg: e.tensor_tensor(
                out=Gt[:], in0=Gt[:], in1=gg[:, :].unsqueeze(2).broadcast_to([128, 8, 2]), op=ALU.mult),
                reads=[tagp + Gn, 'gmix', 'gffn'], writes=[tagp + Gn])
            P.op('dve', lambda e, St=St, jsh=jsh: e.tensor_copy(out=St[:], in_=modT[:, jsh * 8:(jsh + 1) * 8, :]),
                 reads=['modT'], writes=[tagp + Sn])
        P.barrier()
    return M


class NormT:
    def __init__(self, P, nc, sb, ps, C, tag="n"):
        self.P = P
        self.C = C
        self.tag = tag
        self.junk = sb(tag + "junk", [128, 1024], BF16)
        self.ss = [sb(tag + f"ss{i}", [128, 1], F32) for i in range(2)]
        self.rstd = [sb(tag + f"rstd{i}", [128, 1], F32) for i in range(2)]
        self.xn = [sb(tag + f"xn{i}", [128, 1024], BF16) for i in range(2)]
        self.pT = ps(tag + "pT", [128, 8, 128], BF16)
        self.i = 0

    def run(self, xt, xkey, G, S, cls, hT_out, hkey, mkeys, npart=128, halo=None):
        P = self.P
        t = self.tag
        i = self.i % 2
        self.i += 1
        npq = npart
        ss, rstd, xn = self.ss[i], self.rstd[i], self.xn[i]
        junk = self.junk
        P.op('act', lambda e: e.activation(out=junk[0:npq, :], in_=xt, func=AF.Square, accum_out=ss[0:npq, :]),
             reads=[xkey], writes=[t + 'junk', t + f'ss{i}'])
        P.op('act', lambda e: e.activation(out=rstd[0:npq, :], in_=ss[0:npq, :], func=AF.Ln, scale=1.0 / 1024, bias=RMS_EPS),
             reads=[t + f'ss{i}'], writes=[t + f'rstd{i}'])
        P.op('act', lambda e: e.activation(out=rstd[0:npq, :], in_=rstd[0:npq, :], func=AF.Exp, scale=-0.5),
             reads=[t + f'rstd{i}'], writes=[t + f'rstd{i}'])
        P.op('dve', lambda e: e.tensor_scalar(out=xn[0:npq, :], in0=xt, scalar1=rstd[0:npq, :], scalar2=None, op0=ALU.mult),
             reads=[xkey, t + f'rstd{i}'], writes=[t + f'xn{i}'])
        pT = self.pT
        identb = self.C['identb']

        def tr(e):
            for c in range(8):
                ins = e.transpose(out=pT[:, c, 0:npq], in_=xn[0:npq, c * 128:(c + 1) * 128], identity=identb[0:npq, 0:npq])
            return ins
        P.op('pe', tr, reads=[t + f'xn{i}', 'identb'], writes=[t + 'pT'])
        if halo is not None:
            hTx, n, left_ok, right_ok = halo
            for (ok, src, dst) in ((left_ok, 0, 0), (right_ok, 1, n + 1)):
                if not ok:
                    continue
                P.op('dve', lambda e, src=src, dst=dst: e.tensor_tensor(
                    out=hTx[:, :, dst:dst + 1], in0=pT[:, :, src:src + 1], in1=G[:, :, cls:cls + 1], op=ALU.mult),
                    reads=[t + 'pT'] + mkeys, writes=[hkey])
                P.op('dve', lambda e, dst=dst: e.tensor_tensor(
                    out=hTx[:, :, dst:dst + 1], in0=hTx[:, :, dst:dst + 1], in1=S[:, :, cls:cls + 1], op=ALU.add),
                    reads=mkeys, writes=[hkey])
            return
        for c in range(8):
            if c % 2 == 0:
                P.op('act', lambda e, c=c: e.activation(out=hT_out(c), in_=pT[:, c, :], func=AF.Identity,
                                                        scale=G[:, c, cls:cls + 1], bias=S[:, c, cls:cls + 1]),
                     reads=[t + 'pT'] + mkeys, writes=[hkey])
            else:
                P.op('dve', lambda e, c=c: e.tensor_scalar(out=hT_out(c), in0=pT[:, c, :],
                                                           scalar1=G[:, c, cls:cls + 1], scalar2=S[:, c, cls:cls + 1],
                                                           op0=ALU.mult, op1=ALU.add),
                     reads=[t + 'pT'] + mkeys, writes=[hkey])


def load_cast(P, nc, stage, skeys, dst_ap_fn, src_ap_fn, nparts, dkey, engs=('pool', 'act')):
    for i in range(nparts):
        sbuf = stage[i % len(stage)]
        sk = skeys[i % len(stage)]
        d = dst_ap_fn(i)
        s = src_ap_fn(i)
        shp = d.shape
        sv = sbuf
        P.dma('sp', lambda e, sv=sv, s=s: e.dma_start(out=sv, in_=s), writes=[sk])
        eng = engs[i % len(engs)]
        if eng == 'act':
            P.op('act', lambda e, d=d, sv=sv: e.activation(out=d, in_=sv, func=AF.Copy), reads=[sk], writes=[dkey])
        else:
            P.op(eng, lambda e, d=d, sv=sv: e.tensor_copy(out=d, in_=sv), reads=[sk], writes=[dkey])


def attn_phase(P, nc, cfg, C, M, T):
    with Scope() as sc0:
        st = sc0.st
        sb = _sb(nc, st)
        ps = _ps(nc, st)
        NKT, NTO = cfg.NKT, cfg.NTO
        kbT = sb("kbT", [128, NKT * 128], BF16)
        vb = sb("vb", [128, NKT, 2, 65], BF16)
        kaT = sb("kaT", [128, NTO * 128], BF16)
        va = sb("va", [128, NTO, 2, 65], BF16)
        gqk = sb("gqk", [128, 4], F32)
        g2rep = sb("g2rep", [128, 2, 1024], F32)
        P.dma('sp', lambda e: e.dma_start(out=g2rep[:], in_=T['gdram'][0, :, :, :]), reads=['g_dram'], writes=['grep'])
        masks = sb("masks", [128, 4, 128], BF16)
        sinkrow = sb("sinkrow", [65, 8], F32)
        pm = sb("pm", [128, 128], BF16)
        xt2 = [sb(f"xt2_{i}", [128, 1024], F32) for i in range(2)]
        hT = sb("hT", [128, 8, 512], BF16)
        cos_t = sb("cos", [128, 512], F32)
        sin_t = sb("sin", [128, 512], F32)
        t1 = sb("t1", [128, 512], F32)
        t2 = sb("t2", [128, 512], F32)
        sq = sb("sq", [128, 512], BF16)
        qraw = sb("qraw", [128, 512], BF16)
        rs = sb("rs", [128, 512], F32)
        pA = ps("pA", [128, 512], F32)
        pB = ps("pB", [128, 512], F32)
        pC = ps("pC", [128, 512], F32)
        nrm = NormT(P, nc, sb, ps, C, "n")
        xcount = [0]

        def rope_chunk(mm, normed, gi, dst, dkey, wkeys):
            P.op('pe', lambda e: mm(e, pA), reads=wkeys + ['hT'], writes=['pA'])
            P.op('act', lambda e: e.activation(out=qraw[:], in_=pA[:], func=AF.Copy), reads=['pA'], writes=['qraw'])
            P.op('pe', lambda e: e.matmul(pB[:], lhsT=pm[:], rhs=qraw[:], start=True, stop=True),
                 reads=['qraw', 'pm'], writes=['pB'])
            if not normed:
                P.op('dve', lambda e: e.tensor_tensor(out=t1[:], in0=pA[:], in1=cos_t[:], op=ALU.mult),
                     reads=['pA', 'cos'], writes=['t1'])
                P.op('dve', lambda e: e.tensor_tensor(out=t2[:], in0=pB[:], in1=sin_t[:], op=ALU.mult),
                     reads=['pB', 'sin'], writes=['t2'])
                P.op('dve', lambda e: e.tensor_tensor(out=dst, in0=t1[:], in1=t2[:], op=ALU.add),
                     reads=['t1', 't2'], writes=[dkey])
            else:
                P.op('act', lambda e: e.activation(out=sq[:], in_=pA[:], func=AF.Square), reads=['pA'], writes=['sq'])
                P.op('pe', lambda e: e.matmul(pC[:], lhsT=C['bones'][:], rhs=sq[:], start=True, stop=True),
                     reads=['sq', 'bones'], writes=['pC'])
                P.op('act', lambda e: e.activation(out=rs[:], in_=pC[:], func=AF.Ln, scale=1.0 / 64, bias=RMS_EPS),
                     reads=['pC'], writes=['rs'])
                P.op('act', lambda e: e.activation(out=rs[:], in_=rs[:], func=AF.Exp, scale=-0.5),
                     reads=['rs'], writes=['rs'])
                P.op('dve', lambda e: e.scalar_tensor_tensor(out=t1[:], in0=pA[:], scalar=gqk[:, gi:gi + 1], in1=cos_t[:],
                                                             op0=ALU.mult, op1=ALU.mult),
                     reads=['pA', 'cos', 'gqk'], writes=['t1'])
                P.op('dve', lambda e: e.scalar_tensor_tensor(out=t2[:], in0=pB[:], scalar=gqk[:, gi + 1:gi + 2],
                                                             in1=sin_t[:], op0=ALU.mult, op1=ALU.mult),
                     reads=['pB', 'sin', 'gqk'], writes=['t2'])
                P.op('dve', lambda e: e.tensor_tensor(out=t1[:], in0=t1[:], in1=t2[:], op=ALU.add),
                     reads=['t1', 't2'], writes=['t1'])
                P.op('dve', lambda e: e.tensor_tensor(out=dst, in0=t1[:], in1=rs[:], op=ALU.mult),
                     reads=['t1', 'rs'], writes=[dkey])

        def mm8(wt, c0, n=128):
            def f(e, pt):
                for k in range(8):
                    ins = e.matmul(pt[:], lhsT=wt[:, k, c0:c0 + n], rhs=hT[:, k, :], start=(k == 0), stop=(k == 7))
                return ins
            return f

        def load_norm_supertile(xsrc, s, clsfn, cosd, sind):
            P.dma('sp', lambda e: e.dma_start(out=cos_t[:], in_=cosd[:, s * 512:(s + 1) * 512]), writes=['cos'])
            P.dma('sp', lambda e: e.dma_start(out=sin_t[:], in_=sind[:, s * 512:(s + 1) * 512]), writes=['sin'])
            for tt in range(4):
                ti = s * 4 + tt
                xi = xcount[0] % 2
                xcount[0] += 1
                xt = xt2[xi]
                P.dma('sp', lambda e, xt=xt, ti=ti: e.dma_start(out=xt[:], in_=xsrc[ti * 128:(ti + 1) * 128, :]),
                      writes=[f'xt2_{xi}'])
                cls = clsfn(ti)
                nrm.run(xt[:], f'xt2_{xi}', M['Gm'], M['Sm'], cls,
                        lambda c, tt=tt: hT[:, c, tt * 128:(tt + 1) * 128], 'hT', ['Gm', 'Sm'])

        with Scope() as sc1:
            st1 = sc1.st
            sb1 = _sb(nc, st1)
            stage = [sb1(f"stage{i}", [128, 2048], F32) for i in range(2)]
            skeys = ['stage0', 'stage1']
            wkv = sb1("wkv", [128, 8, 512], BF16)
            P.dma('sp', lambda e: e.dma_start(out=gqk[:], in_=T['gqk'][:, :]), writes=['gqk'])
            P.op('pool', lambda e: e.memset(vb[:], 1.0), writes=['vb'])
            P.op('pool', lambda e: e.memset(va[:], 1.0), writes=['va'])
            for mi in range(4):
                P.dma('sp', lambda e, mi=mi: e.dma_start(out=stage[mi % 2][:, 0:128], in_=T['masks'][mi, :, :]),
                      writes=[skeys[mi % 2]])
                P.op('dve', lambda e, mi=mi: e.tensor_copy(out=masks[:, mi, :], in_=stage[mi % 2][:, 0:128]),
                     reads=[skeys[mi % 2]], writes=['masks'])
            P.dma('sp', lambda e: e.dma_start(out=stage[0][:, 0:128], in_=T['pm'][:, :]), writes=['stage0'])
            P.op('dve', lambda e: e.tensor_copy(out=pm[:], in_=stage[0][:, 0:128]), reads=['stage0'], writes=['pm'])
            P.op('pool', lambda e: e.memset(sinkrow[:], 0.0), writes=['sinkrow'])
            P.dma('sp', lambda e: e.dma_start(out=sinkrow[64:65, :], in_=T['sink'][:, :]),
                  reads=['sinkrow'], writes=['sinkrow'])
            P.op('act', lambda e: e.activation(out=sinkrow[64:65, :], in_=sinkrow[64:65, :], func=AF.Exp),
                 reads=['sinkrow'], writes=['sinkrow'])
            wsrc = T['wkv'].rearrange("(c p) n -> p c n", p=128)
            for k in range(8):
                P.dma('sp', lambda e, k=k: e.dma_start(out=stage[k % 2][:, 0:512], in_=wsrc[:, k, :]),
                      writes=[skeys[k % 2]])
                P.op('pool', lambda e, k=k: e.tensor_copy(out=wkv[:, k, :], in_=stage[k % 2][:, 0:512]),
                     reads=[skeys[k % 2]], writes=['wkv'])
            CHK(1)
            for stream in range(2):
                ns = cfg.NSO if stream == 0 else cfg.NSR
                xsrc = T['xo'] if stream == 0 else T['xr']
                cosd = T['cosO'] if stream == 0 else T['cosR']
                sind = T['sinO'] if stream == 0 else T['sinR']
                tbase = 0 if stream == 0 else NTO
                for s in range(ns):
                    clsfn = (lambda ti: 1 if ti < 2 else 0) if stream == 0 else (lambda ti: 0)
                    load_norm_supertile(xsrc, s, clsfn, cosd, sind)
                    CHK(2)
                    tok0 = (tbase + s * 4) * 128
                    if stream == 0:
                        rope_chunk(mm8(wkv, 0), False, 0, kaT[:, s * 512:(s + 1) * 512], 'kaT', ['wkv'])
                        CHK(3)
                    rope_chunk(mm8(wkv, 128), True, 2, kbT[:, tok0:tok0 + 512], 'kbT', ['wkv'])
                    CHK(4)
                    for tt in range(4):
                        c0, n = (256, 256) if stream == 0 else (384, 128)

                        def vm(e, tt=tt, c0=c0, n=n):
                            for k in range(8):
                                ins = e.matmul(pA[:, 0:n], lhsT=hT[:, k, tt * 128:(tt + 1) * 128], rhs=wkv[:, k, c0:c0 + n],
                                               start=(k == 0), stop=(k == 7))
                            return ins
                        P.op('pe', vm, reads=['hT', 'wkv'], writes=['pA'])
                        if stream == 0:
                            P.op('act', lambda e, s=s, tt=tt: e.activation(
                                out=va[:, s * 4 + tt, :, 0:64], in_=pA[:, 0:128].rearrange("p (g d) -> p g d", g=2),
                                func=AF.Copy), reads=['pA'], writes=['va'])
                            P.op('dve', lambda e, s=s, tt=tt: e.tensor_copy(
                                out=vb[:, s * 4 + tt, :, 0:64], in_=pA[:, 128:256].rearrange("p (g d) -> p g d", g=2)),
                                reads=['pA'], writes=['vb'])
                        else:
                            P.op('act', lambda e, s=s, tt=tt, tbase=tbase: e.activation(
                                out=vb[:, tbase + s * 4 + tt, :, 0:64],
                                in_=pA[:, 0:128].rearrange("p (g d) -> p g d", g=2),
                                func=AF.Copy), reads=['pA'], writes=['vb'])
                    CHK(5)
            P.barrier()
            CHK(6)

        if sc1.stopped:
            raise StopBuild()
        with Scope() as sc2:
            st2 = sc2.st
            sb2 = _sb(nc, st2)
            ps2 = _ps(nc, st2)
            wq = sb2("wq", [128, 8, 1024], BF16)
            wout = sb2("wout", [128, 8, 1024], BF16)
            with ExitStack() as st2a:
                sb2a = _sb(nc, st2a)
                stageB = [sb2a(f"stageb{i}", [128, 2048], F32) for i in range(2)]
                skeysB = ['stageb0', 'stageb1']
                wsrcq = T['wq'].rearrange("(c p) n -> p c n", p=128)
                for k in range(8):
                    P.dma('sp', lambda e, k=k: e.dma_start(out=stageB[k % 2][:, 0:1024], in_=wsrcq[:, k, :]),
                          writes=[skeysB[k % 2]])
                    P.op('pool', lambda e, k=k: e.tensor_copy(out=wq[:, k, :], in_=stageB[k % 2][:, 0:1024]),
                         reads=[skeysB[k % 2]], writes=['wq'])
                for k in range(8):
                    P.dma('sp', lambda e, k=k: e.dma_start(out=stageB[k % 2][:, 0:1024], in_=T['wout'][:, k, :]),
                          writes=[skeysB[k % 2]])
                    P.op('pool', lambda e, k=k: e.tensor_copy(out=wout[:, k, :], in_=stageB[k % 2][:, 0:1024]),
                         reads=[skeysB[k % 2]], writes=['wout'])
                P.barrier()
            CHK(7)
            qT = sb2("qT", [128, 8, 512], BF16)
            o_all = sb2("o_all", [128, 8, 512], BF16)
            Pt = [sb2(f"Pt{i}", [128, 512], BF16) for i in range(3)]
            rden = sb2("rden", [65, 512], F32)
            bcs = sb2("bcs", [64, 512], F32)
            tmpy = sb2("tmpy", [128, 1024], F32)
            xres = sb2("xres", [128, 1024], F32)
            pS = [ps2(f"pS{i}", [128, 512], F32) for i in range(2)]
            pO = ps2("pO", [128, 512], F32)
            pBC = ps2("pBC", [128, 512], F32)
            P.op('pool', lambda e: e.memset(o_all[:], 0.0), writes=['o_all'])
            P.op('pool', lambda e: e.memset(rden[:], 1.0), writes=['rden'])
            pcount = [0]
            ones = C['ones']

            def v3(ap, three):
                return ap.rearrange("p (h q) -> p h q", h=4) if three else ap

            def attend(ktiles, kT, vt, kkey, vkey, g, rhs_fn, ncols, mask_fn, sink_g, out_fn, three):
                nk = len(ktiles)
                for ii, kt in enumerate(ktiles):
                    pi = pcount[0] % 2
                    bi = pcount[0] % 3
                    pcount[0] += 1
                    pSt = pS[pi]
                    Pb = Pt[bi]
                    P.op('pe', lambda e, kt=kt, pSt=pSt: e.matmul(
                        v3(pSt[:, 0:ncols], three), lhsT=kT[g * 64:(g + 1) * 64, kt * 128:(kt + 1) * 128], rhs=rhs_fn(),
                        start=True, stop=True), reads=[kkey, 'qT'], writes=[f'pS{pi}'])
                    P.op('act', lambda e, pSt=pSt, Pb=Pb: e.activation(out=Pb[:, 0:ncols], in_=pSt[:, 0:ncols],
                                                                        func=AF.Exp, scale=0.125),
                         reads=[f'pS{pi}'], writes=[f'Pt{bi}'])
                    mk = mask_fn(kt) if mask_fn is not None else None
                    if mk is not None:
                        P.op('dve', lambda e, Pb=Pb, mk=mk: e.tensor_tensor(
                            out=v3(Pb[:, 0:ncols], True), in0=v3(Pb[:, 0:ncols], True),
                            in1=masks[:, mk, :].unsqueeze(1).broadcast_to([128, 4, 128]),
                            op=ALU.mult), reads=[f'Pt{bi}', 'masks'], writes=[f'Pt{bi}'])
                    P.op('pe', lambda e, kt=kt, Pb=Pb, ii=ii: e.matmul(
                        pO[0:65, 0:ncols], lhsT=vt[:, kt, g, 0:65], rhs=Pb[:, 0:ncols], start=(ii == 0), stop=(ii == nk - 1)),
                        reads=[vkey, f'Pt{bi}'], writes=['pO'])
                if sink_g is not None:
                    P.op('dve', lambda e: e.tensor_tensor(
                        out=v3(rden[64:65, 0:ncols], True), in0=v3(pO[64:65, 0:ncols], True),
                        in1=sinkrow[64:65, sink_g * 4:(sink_g + 1) * 4].unsqueeze(2).broadcast_to([1, 4, 128]),
                        op=ALU.add), reads=['pO', 'sinkrow'], writes=['rden'])
                    P.op('dve', lambda e: e.reciprocal(out=rden[64:65, 0:ncols], in_=rden[64:65, 0:ncols]),
                         reads=['rden'], writes=['rden'])
                else:
                    P.op('dve', lambda e: e.reciprocal(out=rden[64:65, 0:ncols], in_=pO[64:65, 0:ncols]),
                         reads=['pO'], writes=['rden'])
                P.op('pe', lambda e: e.matmul(pBC[0:64, 0:ncols], lhsT=ones[64:65, 0:64], rhs=rden[64:65, 0:ncols],
                                              start=True, stop=True), reads=['rden', 'ones'], writes=['pBC'])
                P.op('act', lambda e: e.activation(out=bcs[:, 0:ncols], in_=pBC[0:64, 0:ncols], func=AF.Copy),
                     reads=['pBC'], writes=['bcs'])
                P.op('dve', lambda e: e.tensor_tensor(out=out_fn(), in0=v3(pO[0:64, 0:ncols], three),
                                                      in1=v3(bcs[:, 0:ncols], three),
                                                      op=ALU.mult), reads=['pO', 'bcs'], writes=['o_all'])

            last_own = NTO - 2
            allk = [kt for kt in range(NKT) if kt != 2 and kt != NTO - 1]
            for s in range(cfg.NSO):
                load_norm_supertile(T['xo'], s, (lambda ti: 1 if ti < 2 else 0), T['cosO'], T['sinO'])
                for c in range(8):
                    rope_chunk(mm8(wq, c * 128), c >= 4, 0, qT[:, c, :], 'qT', ['wq'])
                CHK(8)
                tiles = [s * 4 + tt for tt in range(4)]
                for tt, ti in enumerate(tiles):
                    if ti == 2 or ti == NTO - 1:
                        continue
                    if ti < 2:
                        kts = [0, 1]
                        mfn = None
                    else:
                        kts = [0, 1, ti - 1, ti, ti + 1]

                        def mfn(kt, ti=ti):
                            if kt == ti - 1:
                                return 0 if ti == 3 else 1
                            if kt == ti + 1:
                                return 3 if ti == last_own else 2
                            return None
                    for g in range(2):
                        attend(kts, kaT, va, 'kaT', 'va', g,
                               lambda g=g, tt=tt: qT[g * 64:(g + 1) * 64, 0:4, tt * 128:(tt + 1) * 128],
                               512, mfn, g,
                               lambda g=g, tt=tt: o_all[0:64, g * 4:(g + 1) * 4, tt * 128:(tt + 1) * 128], True)
                CHK(9)
                groups = []
                run0 = None
                for tt, ti in enumerate(tiles):
                    if ti < 2:
                        kind = 'c'
                    elif ti == 2 or ti == NTO - 1:
                        kind = None
                    else:
                        kind = 'o'
                    if run0 is not None and run0[0] == kind:
                        run0[2] += 128
                    else:
                        if run0 is not None and run0[0] is not None:
                            groups.append(tuple(run0))
                        run0 = [kind, tt * 128, 128]
                if run0 is not None and run0[0] is not None:
                    groups.append(tuple(run0))
                for (kind, c0, n) in groups:
                    kts = [0, 1] if kind == 'c' else allk
                    for hb in range(8):
                        g = hb // 4
                        j = hb % 4
                        attend(kts, kbT, vb, 'kbT', 'vb', g,
                               lambda g=g, j=j, c0=c0, n=n: qT[g * 64:(g + 1) * 64, 4 + j, c0:c0 + n],
                               n, None, None,
                               lambda hb=hb, c0=c0, n=n: o_all[64:128, hb, c0:c0 + n], False)
                CHK(10)
                for tt, ti in enumerate(tiles):
                    if ti == 2 or ti == NTO - 1:
                        continue
                    cls = 1 if ti < 2 else 0
                    row0 = ti * 128 if ti < 2 else 256 + (ti - 3) * 128
                    P.dma('sp', lambda e, ti=ti: e.dma_start(out=xres[:], in_=T['xo'][ti * 128:(ti + 1) * 128, :]),
                          writes=['xres'])
                    for half in range(2):
                        def om(e, tt=tt, half=half):
                            for hh in range(8):
                                ins = e.matmul(pA[:], lhsT=o_all[:, hh, tt * 128:(tt + 1) * 128],
                                               rhs=wout[:, hh, half * 512:(half + 1) * 512],
                                               start=(hh == 0), stop=(hh == 7))
                            return ins
                        P.op('pe', om, reads=['o_all', 'wout'], writes=['pA'])
                        P.op('dve', lambda e, half=half, cls=cls: e.tensor_tensor(
                            out=tmpy[:, half * 512:(half + 1) * 512], in0=pA[:],
                            in1=g2rep[:, cls, half * 512:(half + 1) * 512], op=ALU.mult),
                            reads=['pA', 'grep'], writes=['tmpy'])
                    P.op('dve', lambda e: e.tensor_tensor(out=tmpy[:], in0=tmpy[:], in1=xres[:], op=ALU.add),
                         reads=['tmpy', 'xres'], writes=['tmpy'])
                    P.dma('sp', lambda e, row0=row0: e.dma_start(out=T['x1'][row0:row0 + 128, :], in_=tmpy[:]),
                          reads=['tmpy'], writes=['x1'])
            P.barrier()
        if sc2.stopped:
            raise StopBuild()
    if sc0.stopped:
        raise StopBuild()


def moe_phase(P, nc, C, M, T, NT, cls_fn, tag="m", final_norm=False):
    GT = 11
    ngr = (NT + GT - 1) // GT
    base, rem = NT // ngr, NT % ngr
    groups = []
    i0 = 0
    for gi in range(ngr):
        sz = base + (1 if gi < rem else 0)
        groups.append(list(range(i0, i0 + sz)))
        i0 += sz
    with ExitStack() as st:
        sb = _sb(nc, st)
        ps = _ps(nc, st)
        wset = [[sb(f"{tag}w{j}_{i}", [128, 8, 1024], BF16) for j in range(3)] for i in range(2)]
        stage = [sb(f"{tag}stg{i}", [128, 1, 1024], F32) for i in range(2)]
        h2T = sb(tag + "h2T", [128, 8, GT * 128], BF16)
        acc = sb(tag + "acc", [128, GT, 1024], F32)
        gT = sb(tag + "gT", [128, 8, 512], BF16)
        sgt = sb(tag + "sgt", [128, 512], F32)
        xm = sb(tag + "xm", [128, 1024], F32)
        xn32 = sb(tag + "xn32", [128, 1024], F32)
        h32 = sb(tag + "h32", [128, 8, 128], F32)
        g5rep = sb(tag + "g5rep", [128, 2, 1024], F32)
        gate = sb(tag + "gate", [128, GT, 16], F32)
        rw = sb(tag + "rw", [128, 8, 16], F32)
        rbias = sb(tag + "rbias", [128, 16], F32)
        if final_norm:
            fng = sb(tag + "fng", [128, 1024], F32)
            P.dma('sp', lambda e: e.dma_start(out=fng[:], in_=T['fng'][:, :]), writes=[tag + 'fng'])
        sm = sb(tag + "sm", [128, 16], F32)
        r16 = [sb(tag + f"r16_{i}", [128, 16], F32) for i in range(5)]
        r4 = [sb(tag + f"r4_{i}", [128, 4], F32) for i in range(4)]
        pA = [ps(f"{tag}pA{i}", [128, 512], F32) for i in range(2)]
        pB = [ps(f"{tag}pB{i}", [128, 512], F32) for i in range(2)]
        pY = [ps(f"{tag}pY{i}", [128, 512], F32) for i in range(2)]
        pT = ps(tag + "pT32", [128, 8, 128], F32)
        K = lambda n: tag + n
        P.dma('sp', lambda e: e.dma_start(out=g5rep[:], in_=T['gdram'][1, :, :, :]), reads=['g_dram'], writes=[K('g5rep')])
        P.dma('sp', lambda e: e.dma_start(out=rw[:], in_=T['rw'][:, :, :]), writes=[K('rw')])
        P.dma('sp', lambda e: e.dma_start(out=rbias[:], in_=T['rbias'][:, :]), writes=[K('rbias')])
        ident = C['ident']
        wcount = [0]

        def load_expert(e_idx):
            ws = wset[e_idx % 2]
            for j, wn in enumerate(('w1', 'w3', 'w2')):
                src = T[wn][e_idx, :, :].rearrange("(c p) n -> p c n", p=128)
                for q in range(8):
                    si = wcount[0] % 2
                    wcount[0] += 1
                    sg = stage[si]
                    P.dma('sp', lambda e, sg=sg, src=src, q=q: e.dma_start(out=sg[:], in_=src[:, q:q + 1, :]),
                          writes=[K(f'stg{si}')])
                    P.op('pool', lambda e, sg=sg, ws=ws, j=j, q=q: e.tensor_copy(out=ws[j][:, q:q + 1, :], in_=sg[:]),
                         reads=[K(f'stg{si}')], writes=[K(f'w{j}_{e_idx % 2}')])

        for grp in groups:
            ng = len(grp)
            for tl, t in enumerate(grp):
                cls = cls_fn(t)
                P.dma('sp', lambda e, t=t: e.dma_start(out=xm[:], in_=T['x_in'][t * 128:(t + 1) * 128, :]),
                      reads=['x_in_' + tag], writes=[K('xm')])
                P.op('act', lambda e: e.activation(out=h32[:].rearrange("p c t -> p (c t)"), in_=xm[:], func=AF.Square, accum_out=sm[:, 0:1]),
                     reads=[K('xm')], writes=[K('h32'), K('sm0')])
                P.op('act', lambda e: e.activation(out=sm[:, 1:2], in_=sm[:, 0:1], func=AF.Ln, scale=1.0 / 1024, bias=RMS_EPS),
                     reads=[K('sm0')], writes=[K('sm1')])
                P.op('act', lambda e: e.activation(out=sm[:, 1:2], in_=sm[:, 1:2], func=AF.Exp, scale=-0.5),
                     reads=[K('sm1')], writes=[K('sm1')])
                P.op('dve', lambda e: e.tensor_scalar(out=xn32[:], in0=xm[:], scalar1=sm[:, 1:2], scalar2=None, op0=ALU.mult),
                     reads=[K('xm'), K('sm1')], writes=[K('xn32')])

                def tr(e):
                    for c in range(8):
                        ins = e.transpose(out=pT[:, c, :], in_=xn32[:, c * 128:(c + 1) * 128], identity=ident[:])
                    return ins
                P.op('pe', tr, reads=[K('xn32'), 'ident'], writes=[K('pT32')])
                for c in range(8):
                    if c % 2 == 0:
                        P.op('act', lambda e, c=c, cls=cls: e.activation(
                            out=h32[:, c, :], in_=pT[:, c, :], func=AF.Identity,
                            scale=M['Gf'][:, c, cls:cls + 1], bias=M['Sf'][:, c, cls:cls + 1]),
                            reads=[K('pT32'), 'Gf', 'Sf'], writes=[K('h32')])
                    else:
                        P.op('dve', lambda e, c=c, cls=cls: e.tensor_scalar(
                            out=h32[:, c, :], in0=pT[:, c, :], scalar1=M['Gf'][:, c, cls:cls + 1],
                            scalar2=M['Sf'][:, c, cls:cls + 1], op0=ALU.mult, op1=ALU.add),
                            reads=[K('pT32'), 'Gf', 'Sf'], writes=[K('h32')])
                P.op('pool', lambda e, tl=tl: e.tensor_copy(out=h2T[:, :, tl * 128:(tl + 1) * 128], in_=h32[:]),
                     reads=[K('h32')], writes=[K('h2T')])
                pR = pY[0]

                def rm(e):
                    for k in range(8):
                        ins = e.matmul(pR[:, 0:16], lhsT=h32[:, k, :], rhs=rw[:, k, :], start=(k == 0), stop=(k == 7))
                    return ins
                P.op('pe', rm, reads=[K('h32'), K('rw')], writes=[K('pY0')])
                lg, ex, probs, sel, sel2 = r16
                top1, top2, gs, gsel = r4
                kk = [K(f'r16_{i}') for i in range(5)]
                k4 = [K(f'r4_{i}') for i in range(4)]
                P.op('dve', lambda e: e.tensor_copy(out=lg[:], in_=pR[:, 0:16]), reads=[K('pY0')], writes=[kk[0]])
                P.op('dve', lambda e: e.tensor_reduce(out=sm[:, 2:3], in_=lg[:], axis=AX.X, op=ALU.max, negate=True),
                     reads=[kk[0]], writes=[K('sm2')])
                P.op('act', lambda e: e.activation(out=ex[:], in_=lg[:], func=AF.Exp, bias=sm[:, 2:3], accum_out=sm[:, 3:4]),
                     reads=[kk[0], K('sm2')], writes=[kk[1], K('sm3')])
                P.op('dve', lambda e: e.reciprocal(out=sm[:, 4:5], in_=sm[:, 3:4]), reads=[K('sm3')], writes=[K('sm4')])
                P.op('dve', lambda e: e.tensor_scalar(out=probs[:], in0=ex[:], scalar1=sm[:, 4:5], scalar2=None, op0=ALU.mult),
                     reads=[kk[1], K('sm4')], writes=[kk[2]])
                P.op('dve', lambda e: e.tensor_tensor(out=sel[:], in0=probs[:], in1=rbias[:], op=ALU.add),
                     reads=[kk[2], K('rbias')], writes=[kk[3]])
                s3 = lambda ap: ap.rearrange("p (g j) -> p g j", g=4)
                b3 = lambda ap: ap.unsqueeze(2).broadcast_to([128, 4, 4])
                P.op('dve', lambda e: e.tensor_reduce(out=top1[:], in_=s3(sel[:]), axis=AX.X, op=ALU.max),
                     reads=[kk[3]], writes=[k4[0]])
                P.op('dve', lambda e: e.tensor_tensor(out=s3(sel2[:]), in0=s3(sel[:]), in1=b3(top1[:]), op=ALU.is_equal),
                     reads=[kk[3], k4[0]], writes=[kk[4]])
                P.op('dve', lambda e: e.scalar_tensor_tensor(out=sel2[:], in0=sel2[:], scalar=-1e30, in1=sel[:],
                                                             op0=ALU.mult, op1=ALU.add),
                     reads=[kk[4], kk[3]], writes=[kk[4]])
                P.op('dve', lambda e: e.tensor_reduce(out=top2[:], in_=s3(sel2[:]), axis=AX.X, op=ALU.max),
                     reads=[kk[4]], writes=[k4[1]])
                P.op('dve', lambda e: e.tensor_tensor(out=gs[:], in0=top1[:], in1=top2[:], op=ALU.add),
                     reads=[k4[0], k4[1]], writes=[k4[2]])
                P.op('dve', lambda e: e.tensor_reduce(out=sm[:, 5:6], in_=gs[:], axis=AX.X, op=ALU.max),
                     reads=[k4[2]], writes=[K('sm5')])
                P.op('dve', lambda e: e.tensor_scalar(out=gsel[:], in0=gs[:], scalar1=sm[:, 5:6], scalar2=None, op0=ALU.is_equal),
                     reads=[k4[2], K('sm5')], writes=[k4[3]])
                P.op('dve', lambda e: e.tensor_tensor(out=s3(sel2[:]), in0=s3(sel[:]), in1=b3(top2[:]), op=ALU.is_ge),
                     reads=[kk[3], k4[1]], writes=[kk[4]])
                P.op('dve', lambda e: e.tensor_tensor(out=s3(sel2[:]), in0=s3(sel2[:]), in1=b3(gsel[:]), op=ALU.mult),
                     reads=[kk[4], k4[3]], writes=[kk[4]])
                P.op('dve', lambda e: e.tensor_tensor(out=ex[:], in0=probs[:], in1=sel2[:], op=ALU.mult),
                     reads=[kk[2], kk[4]], writes=[kk[1]])
                P.op('dve', lambda e: e.tensor_reduce(out=sm[:, 6:7], in_=ex[:], axis=AX.X, op=ALU.add),
                     reads=[kk[1]], writes=[K('sm6')])
                P.op('dve', lambda e: e.reciprocal(out=sm[:, 7:8], in_=sm[:, 6:7]), reads=[K('sm6')], writes=[K('sm7')])
                P.op('dve', lambda e, tl=tl: e.tensor_scalar(out=gate[:, tl, :], in0=ex[:], scalar1=sm[:, 7:8], scalar2=None,
                                                             op0=ALU.mult), reads=[kk[1], K('sm7')], writes=[K('gate')])
            P.op('pool', lambda e: e.memset(acc[:], 0.0), writes=[K('acc')])
            subs = [list(range(i, min(i + 4, ng))) for i in range(0, ng, 4)]
            pc = [0]
            load_expert(0)
            for ex_i in range(16):
                if ex_i + 1 < 16:
                    load_expert(ex_i + 1)
                ws = wset[ex_i % 2]
                wk = [K(f'w{j}_{ex_i % 2}') for j in range(3)]
                for sub in subs:
                    c0 = sub[0] * 128
                    ncol = len(sub) * 128
                    for fc in range(8):
                        pi = pc[0] % 2
                        pc[0] += 1
                        pa, pb = pA[pi], pB[pi]

                        def m1(e, pa=pa, fc=fc, w=ws[0], ncol=ncol, c0=c0):
                            for k in range(8):
                                ins = e.matmul(pa[:, 0:ncol], lhsT=w[:, k, fc * 128:(fc + 1) * 128], rhs=h2T[:, k, c0:c0 + ncol],
                                               start=(k == 0), stop=(k == 7))
                            return ins

                        def m3(e, pb=pb, fc=fc, w=ws[1], ncol=ncol, c0=c0):
                            for k in range(8):
                                ins = e.matmul(pb[:, 0:ncol], lhsT=w[:, k, fc * 128:(fc + 1) * 128], rhs=h2T[:, k, c0:c0 + ncol],
                                               start=(k == 0), stop=(k == 7))
                            return ins
                        P.op('pe', m1, reads=[wk[0], K('h2T')], writes=[K(f'pA{pi}')])
                        P.op('pe', m3, reads=[wk[1], K('h2T')], writes=[K(f'pB{pi}')])
                        P.op('act', lambda e, pa=pa, ncol=ncol: e.activation(out=sgt[:, 0:ncol], in_=pa[:, 0:ncol], func=AF.Silu),
                             reads=[K(f'pA{pi}')], writes=[K('sgt')])
                        P.op('dve', lambda e, pb=pb, fc=fc, ncol=ncol: e.tensor_tensor(out=gT[:, fc, 0:ncol], in0=sgt[:, 0:ncol],
                                                                            in1=pb[:, 0:ncol], op=ALU.mult),
                             reads=[K('sgt'), K(f'pB{pi}')], writes=[K('gT')])
                    for tl2, tl in enumerate(sub):
                        for half in range(2):
                            yi = pc[0] % 2
                            pc[0] += 1
                            py = pY[yi]

                            def m2(e, py=py, tl2=tl2, half=half, w=ws[2]):
                                for fc in range(8):
                                    ins = e.matmul(py[:], lhsT=gT[:, fc, tl2 * 128:(tl2 + 1) * 128],
                                                   rhs=w[:, fc, half * 512:(half + 1) * 512], start=(fc == 0), stop=(fc == 7))
                                return ins
                            P.op('pe', m2, reads=[K('gT'), wk[2]], writes=[K(f'pY{yi}')])
                            P.op('dve', lambda e, py=py, tl=tl, half=half, ex_i=ex_i: e.scalar_tensor_tensor(
                                out=acc[:, tl, half * 512:(half + 1) * 512], in0=py[:], scalar=gate[:, tl, ex_i:ex_i + 1],
                                in1=acc[:, tl, half * 512:(half + 1) * 512], op0=ALU.mult, op1=ALU.add),
                                reads=[K(f'pY{yi}'), K('gate'), K('acc')], writes=[K('acc')])
            for tl, t in enumerate(grp):
                cls = cls_fn(t)
                P.dma('sp', lambda e, t=t: e.dma_start(out=xm[:], in_=T['x_in'][t * 128:(t + 1) * 128, :]),
                      reads=['x_in_' + tag], writes=[K('xm')])
                P.op('dve', lambda e, tl=tl, cls=cls: e.tensor_tensor(out=xn32[:], in0=acc[:, tl, :], in1=g5rep[:, cls, :],
                                                                      op=ALU.mult),
                     reads=[K('acc'), K('g5rep')], writes=[K('xn32')])
                P.op('dve', lambda e: e.tensor_tensor(out=xn32[:], in0=xn32[:], in1=xm[:], op=ALU.add),
                     reads=[K('xn32'), K('xm')], writes=[K('xn32')])
                if final_norm:
                    P.op('act', lambda e: e.activation(out=h32[:].rearrange("p c t -> p (c t)"), in_=xn32[:], func=AF.Square, accum_out=sm[:, 8:9]),
                         reads=[K('xn32')], writes=[K('h32'), K('sm8')])
                    P.op('act', lambda e: e.activation(out=sm[:, 9:10], in_=sm[:, 8:9], func=AF.Ln, scale=1.0 / 1024, bias=RMS_EPS),
                         reads=[K('sm8')], writes=[K('sm9')])
                    P.op('act', lambda e: e.activation(out=sm[:, 9:10], in_=sm[:, 9:10], func=AF.Exp, scale=-0.5),
                         reads=[K('sm9')], writes=[K('sm9')])
                    P.op('dve', lambda e: e.scalar_tensor_tensor(out=xn32[:], in0=xn32[:], scalar=sm[:, 9:10], in1=fng[:],
                                                                 op0=ALU.mult, op1=ALU.mult),
                         reads=[K('xn32'), K('sm9'), K('fng')], writes=[K('xn32')])
                P.dma('sp', lambda e, t=t: e.dma_start(out=T['x_out'][t * 128:(t + 1) * 128, :], in_=xn32[:]),
                      reads=[K('xn32')], writes=['x_out_' + tag])
        P.barrier()


def build_A(cfg, debug_out=True, phases=('attn', 'moe'), n_exp=16):
    nc = bass.Bass("TRN2", target_bir_lowering=False)
    def din(name, shape):
        return nc.dram_tensor(name, shape, F32, kind="ExternalInput").ap()
    T = {}
    T['xo'] = din("xo", [cfg.TO, 1024])
    T['xr'] = din("xr", [cfg.TR, 1024])
    T['cosO'] = din("cosO", [128, cfg.TO])
    T['sinO'] = din("sinO", [128, cfg.TO])
    T['cosR'] = din("cosR", [128, cfg.TR])
    T['sinR'] = din("sinR", [128, cfg.TR])
    T['masks'] = din("masks", [4, 128, 128])
    T['gqk'] = din("gqk", [128, 4])
    T['sink'] = din("sink", [1, 8])
    T['pm'] = din("pm", [128, 128])
    T['wkv'] = din("wkv", [1024, 512])
    T['wq'] = din("wq", [1024, 1024])
    T['wout'] = din("wout", [128, 8, 1024])
    cT = din("cT", [128, 8, 2])
    ada_w = din("ada_w", [1024, 6144])
    ada_bT = din("ada_bT", [128, 48])
    adab_rep = din("adab_rep", [128, 2, 1024])
    gmixT = din("gmixT", [128, 8])
    gffnT = din("gffnT", [128, 8])
    if 'moe' in phases:
        T['rw'] = din("rw", [128, 8, 16])
        T['rbias'] = din("rbias", [128, 16])
        T['w1'] = din("w1", [n_exp, 1024, 1024])
        T['w3'] = din("w3", [n_exp, 1024, 1024])
        T['w2'] = din("w2", [n_exp, 1024, 1024])
        T['x2'] = nc.dram_tensor("x2", [cfg.TQ, 1024], F32, kind="ExternalOutput").ap()
    T['x1'] = nc.dram_tensor("x1", [cfg.TQ, 1024], F32, kind="ExternalOutput").ap()
    T['gdram'] = nc.dram_tensor("gdram", [2, 128, 2, 1024], F32, kind="ExternalOutput").ap()
    P = Prog(nc)
    P.psum_keys = PSUM_KEYS
    with ExitStack() as st:
        sb = _sb(nc, st)
        C = make_consts(P, nc, sb)
        M = mod_setup(P, nc, st, C, cT, ada_w, ada_bT, adab_rep, gmixT, gffnT, T['gdram'])
        if 'attn' in phases:
            try:
                attn_phase(P, nc, cfg, C, M, T)
            except StopBuild:
                P.barrier()
        if 'moe' in phases:
            T['x_in'] = T['x1']
            T['x_out'] = T['x2']
            moe_phase(P, nc, C, M, T, cfg.NTQ, lambda t: 1 if t < 2 else 0, "m")
        P.final_wait('sp')
        P.emit(st)
    print("A ops:", P.nops, {e: len(P.streams[e]) for e in P.ENG})
    return nc


def rope_tab(pos):
    p = np.arange(128)
    d = p % 64
    axis = d // 32
    half = (d % 32) // 16
    f = d % 16
    inv = (10000.0 ** (-(f.astype(np.float32)) / 16.0)).astype(np.float32)
    pos = np.asarray(pos)
    row = (pos // 64).astype(np.float32)
    col = (pos % 64).astype(np.float32)
    pa = np.where(axis[:, None] == 0, row[None, :], col[None, :]).astype(np.float32)
    ang = (pa * inv[:, None]).astype(np.float32)
    cos = np.cos(ang).astype(np.float32)
    sin = np.sin(ang).astype(np.float32) * np.where(half == 0, -1.0, 1.0)[:, None].astype(np.float32)
    nr = pos < 0
    cos[:, nr] = 1.0
    sin[:, nr] = 0.0
    return cos, sin.astype(np.float32)


def partner():
    p = np.arange(128)
    d = p % 64
    pd = np.where((d % 32) < 16, d + 16, d - 16)
    return (p // 64) * 64 + pd


def host_A(inp, cfg, core, layer=0, with_moe=True):
    b, qd = core // 4, core % 4
    S, OWN = cfg.S, cfg.OWN
    x = inp['x'][b, :S]
    ctx = inp['ctx'][b]
    o0 = qd * OWN
    z = np.zeros((128, 1024), np.float32)
    hl = x[o0 - 128:o0] if qd > 0 else z
    hr = x[o0 + OWN:o0 + OWN + 128] if qd < 3 else z
    xo = np.concatenate([ctx, hl, x[o0:o0 + OWN], hr], 0)
    posO = np.concatenate([-np.ones(256, np.int64),
                           np.arange(o0 - 128, o0) if qd > 0 else -np.ones(128, np.int64),
                           np.arange(o0, o0 + OWN),
                           np.arange(o0 + OWN, o0 + OWN + 128) if qd < 3 else -np.ones(128, np.int64)])
    xr = np.concatenate([x[:o0], x[o0 + OWN:]], 0)
    posR = np.concatenate([np.arange(0, o0), np.arange(o0 + OWN, S)])
    cosO, sinO = rope_tab(posO)
    cosR, sinR = rope_tab(posR)
    k = np.arange(128)[:, None]
    q = np.arange(128)[None, :]
    mL = (k >= q).astype(np.float32)
    mR = (k <= q).astype(np.float32)
    masks = np.stack([mL if qd > 0 else 0 * mL, mL, mR, mR if qd < 3 else 0 * mR])
    pt = partner()
    d = np.arange(128) % 64
    gq = inp['attn_q_norm_g'][0]
    gk = inp['attn_k_norm_g'][0]
    gqk = np.stack([gq[d], gq[pt % 64], gk[d], gk[pt % 64]], 1).astype(np.float32)
    pm = np.zeros((128, 128), np.float32)
    pm[pt, np.arange(128)] = 1.0
    w_in = inp['attn_w_in'][0]
    wkv = np.concatenate([w_in[:, 512:640], w_in[:, 1280:1408], w_in[:, 640:768], w_in[:, 1408:1536]], 1)
    cols = []
    for c in range(4):
        cols += [w_in[:, c * 64:(c + 1) * 64], w_in[:, (4 + c) * 64:(5 + c) * 64]]
    for j in range(4):
        cols += [w_in[:, 768 + j * 64:768 + (j + 1) * 64], w_in[:, 768 + (4 + j) * 64:768 + (5 + j) * 64]]
    wq = np.concatenate(cols, 1)
    w_out = inp['attn_w_out'][0]
    wout = np.zeros((128, 8, 1024), np.float32)
    for hh in range(8):
        wout[0:64, hh] = w_out[hh * 64:(hh + 1) * 64]
        wout[64:128, hh] = w_out[512 + hh * 64:512 + (hh + 1) * 64]
    m = {}
    m.update(xo=xo, xr=xr, cosO=cosO, sinO=sinO, cosR=cosR, sinR=sinR, masks=masks, gqk=gqk,
             sink=inp['attn_sink'][0][None, :], pm=pm, wkv=wkv, wq=wq, wout=wout)
    m.update(host_mod(inp, b, layer))
    if with_moe:
        m.update(host_moe(inp, layer))
    return {k_: np.ascontiguousarray(v, dtype=np.float32) for k_, v in m.items()}


def host_mod(inp, b, layer):
    cT = np.stack([inp['c'][b].reshape(8, 128).T, inp['c_ctx'].reshape(8, 128).T], 2)
    ab = inp['ada_b'][layer]
    return dict(cT=cT, ada_w=inp['ada_w'][layer], ada_bT=ab.reshape(48, 128).T,
                adab_rep=np.broadcast_to(np.stack([ab[2048:3072], ab[5120:6144]])[None], (128, 2, 1024)),
                gmixT=inp['norm_mix_g'][layer].reshape(8, 128).T, gffnT=inp['norm_ffn_g'][layer].reshape(8, 128).T)


def host_moe(inp, layer):
    return dict(rw=inp['router_w'].reshape(8, 128, 16).transpose(1, 0, 2),
                rbias=np.broadcast_to(inp['router_bias'][None], (128, 16)),
                w1=inp['moe_w1'][layer], w3=inp['moe_w3'][layer], w2=inp['moe_w2'][layer])


def build_A2(NT, n_exp=16):
    nc = bass.Bass("TRN2", target_bir_lowering=False)
    def din(name, shape):
        return nc.dram_tensor(name, shape, F32, kind="ExternalInput").ap()
    T = {}
    T['x_in'] = din("xin", [NT * 128, 1024])
    cT = din("cT", [128, 8, 2])
    ada_w = din("ada_w", [1024, 6144])
    ada_bT = din("ada_bT", [128, 48])
    adab_rep = din("adab_rep", [128, 2, 1024])
    gmixT = din("gmixT", [128, 8])
    gffnT = din("gffnT", [128, 8])
    T['rw'] = din("rw", [128, 8, 16])
    T['rbias'] = din("rbias", [128, 16])
    T['w1'] = din("w1", [n_exp, 1024, 1024])
    T['w3'] = din("w3", [n_exp, 1024, 1024])
    T['w2'] = din("w2", [n_exp, 1024, 1024])
    T['x_out'] = nc.dram_tensor("x2", [NT * 128, 1024], F32, kind="ExternalOutput").ap()
    T['gdram'] = nc.dram_tensor("gdram", [2, 128, 2, 1024], F32, kind="ExternalOutput").ap()
    P = Prog(nc)
    P.psum_keys = PSUM_KEYS
    with ExitStack() as st:
        sb = _sb(nc, st)
        C = make_consts(P, nc, sb)
        M = mod_setup(P, nc, st, C, cT, ada_w, ada_bT, adab_rep, gmixT, gffnT, T['gdram'])
        moe_phase(P, nc, C, M, T, NT, lambda t: 1 if t < 2 else 0, "m")
        P.final_wait('sp')
        P.emit(st)
    print("A2 ops:", P.nops, {e: len(P.streams[e]) for e in P.ENG})
    return nc


def host_A2(inp, xin, b):
    m = {'xin': xin}
    m.update(host_mod(inp, b, 0))
    m.update(host_moe(inp, 0))
    return {k_: np.ascontiguousarray(v, dtype=np.float32) for k_, v in m.items()}


DEC_C = -0.6065306597126334
INV_DT = BF16


def rwkv_phase(P, nc, C, M, T, NL):
    with ExitStack() as st:
        sb = _sb(nc, st)
        ps = _ps(nc, st)
        wrkv = sb("wrkv", [128, 8, 768], BF16)
        wl = sb("wl", [128, 8, 416], BF16)
        w2c = sb("w2c", [128, 256], BF16)
        a2c = sb("a2c", [128, 256], BF16)
        g2a = sb("g2a", [128, 256], BF16)
        g2b = sb("g2b", [32, 256], BF16)
        xmix = sb("xmix", [128, 8, 6], F32)
        colv = sb("colv", [128, 2, 6], F32)
        rkw = sb("rkw", [128, 2, 2], BF16)
        lnrep = sb("lnrep", [128, 2, 256], F32)
        mk = sb("mk", [128, 10, 128], F32)
        rmask = sb("rmask", [128, 512], F32)
        stg = [sb(f"bstg{i}", [128, 1024], F32) for i in range(2)]
        sk = ['bstg0', 'bstg1']
        cnt = [0]

        def ld(dst, src, shape_cols, dkey, nparts=128):
            i = cnt[0] % 2
            cnt[0] += 1
            P.dma('sp', lambda e: e.dma_start(out=stg[i][0:nparts, 0:shape_cols], in_=src), writes=[sk[i]])
            P.op('pool', lambda e: e.tensor_copy(out=dst, in_=stg[i][0:nparts, 0:shape_cols]), reads=[sk[i]], writes=[dkey])
        wsrc = T['wrkv'].rearrange("(c p) n -> p c n", p=128)
        for k in range(8):
            ld(wrkv[:, k, :], wsrc[:, k, :], 768, 'wrkv')
        for (nm, c0, n) in (('w1cat', 0, 128), ('a1cat', 128, 128), ('g1', 256, 160)):
            src = T[nm].rearrange("(c p) n -> p c n", p=128)
            for k in range(8):
                ld(wl[:, k, c0:c0 + n], src[:, k, :], n, 'wl')
        ld(w2c[:], T['w2cat'][:, :], 256, 'w2c')
        ld(a2c[:], T['a2cat'][:, :], 256, 'a2c')
        ld(g2a[:], T['g2a'][:, :], 256, 'g2a')
        ld(g2b[:], T['g2b'][:, :], 256, 'g2b', nparts=32)
        ld(rkw[:].rearrange("p a b -> p (a b)"), T['rkw'].rearrange("p a b -> p (a b)"), 4, 'rkw')
        P.dma('sp', lambda e: e.dma_start(out=xmix[:], in_=T['xmixT'][:, :, :]), writes=['xmix'])
        P.dma('sp', lambda e: e.dma_start(out=colv[:], in_=T['colvec'][:, :, :]), writes=['colv'])
        P.dma('sp', lambda e: e.dma_start(out=lnrep[:], in_=T['lnrep'][:, :, :]), writes=['lnrep'])
        for i in range(10):
            P.dma('sp', lambda e, i=i: e.dma_start(out=mk[:, i, :], in_=T['mk'][i, :, :]), writes=['mk'])
        P.op('pool', lambda e: e.memset(rmask[:], 1.0), writes=['rmask'])
        P.op('pool', lambda e: e.memset(rmask[:].rearrange("p (c t) -> p c t", t=64)[:, :, 0:1], 0.0),
             reads=['rmask'], writes=['rmask'])
        P.barrier()

        xt = [sb(f"bxt{i}", [128, 1024], F32) for i in range(2)]
        xh = sb("bxh", [2, 1024], F32)
        hTx = sb("hTx", [128, 8, 514], BF16)
        xx = sb("xx", [128, 8, 512], BF16)
        mixb = [sb(f"mix{i}", [128, 8, 512], BF16) for i in range(2)]
        nrm = NormT(P, nc, sb, ps, C, "bn")
        F = lambda name: sb(name, [128, 2, 512], F32)
        rT, kT, lw, icl, kkn, kd, cin, tmpA, tmpB, e2 = [F(n) for n in
                                                         ("rT", "kT", "lw", "icl", "kkn", "kd", "cin", "tmpA", "tmpB", "e2")]
        arT = sb("arT", [128, 2, 2, 512], BF16)
        bhT = sb("bhT", [128, 2, 512], BF16)
        khT = sb("khT", [128, 2, 512], BF16)
        bgT = sb("bgT", [128, 2, 512], BF16)
        kgT = sb("kgT", [128, 2, 512], BF16)
        rkb = sb("rkb", [128, 2, 512], BF16)
        sqb = sb("sqb", [128, 512], BF16)
        th = sb("th", [128, 512], BF16)
        ua = sb("ua", [128, 512], BF16)
        sg = sb("sg", [128, 512], BF16)
        sg2 = sb("sg2", [32, 512], BF16)
        vtok = sb("vtok", [128, 4, 256], BF16)
        gtok = sb("gtok", [128, 4, 256], F32)
        bon = sb("bon", [128, 4, 4], F32)
        gC = sb("gC", [128, 2, 8], F32)
        tot = sb("tot", [128, 2, 8], F32)
        tk = sb("tk", [128, 3, 256], BF16)
        U1 = sb("U1", [128, 4, 2, 128], BF16)
        U2 = sb("U2", [128, 4, 2, 128], BF16)
        AA = [sb(f"AA{i}", [128, 4, 2, 128], F32) for i in range(2)]
        AB = [sb(f"AB{i}", [128, 4, 2, 128], F32) for i in range(2)]
        UoffT = sb("UoffT", [128, 4, 3, 128], F32)
        T32 = sb("T32", [128, 4, 2, 128], F32)
        Zs = sb("Zs", [128, 4, 2, 128], F32)
        Tfin = sb("Tfin", [128, 4, 128], BF16)
        Mtmp = sb("Mtmp", [128, 2, 64], F32)
        XA = sb("XA", [128, 4, 2, 64], BF16)
        WA = sb("WA", [128, 2, 4, 64], BF16)
        RtT = sb("RtT", [128, 2, 128], F32)
        Msb = sb("Msb", [128, 2, 2, 128], F32)
        Sbd = sb("Sbd", [128, 2, 128], F32)
        ysb = sb("ysb", [128, 256], F32)
        y0t = sb("y0t", [128, 256], F32)
        b0t = sb("b0t", [128, 4], F32)
        st8 = sb("st8", [128, 16], F32)
        osb = sb("osb", [128, 256], F32)
        pA = ps("bpA", [128, 512], F32)
        pG = [ps(f"bpG{i}", [128, 2, 2, 128], F32) for i in range(2)]
        pXf = ps("bpX", [128, 512], F32)
        pX = pXf[:].rearrange("p (h a k) -> p h a k", h=4, a=2)
        pB = pXf
        pYf = ps("bpY", [128, 512], F32)
        pY = pYf[:, 0:256].rearrange("p (h v) -> p h v", h=4)
        pSf = ps("bpS", [128, 512], F32)
        pS = pSf[:, 0:128].rearrange("p (c v) -> p c v", c=2)
        pTt_full = ps("bpTt", [128, 4, 256], BF16)
        pTt = pTt_full[:, 0:3, :]
        ident = C['ident']
        identb = C['identb']
        xc_ = [0]

        def proj_fm(w, c0, n, mix, out_ps, wkey):
            def f(e):
                for k in range(8):
                    ins = e.matmul(out_ps, lhsT=w[:, k, c0:c0 + n], rhs=mix[:, k, :], start=(k == 0), stop=(k == 7))
                return ins
            return f

        def do_segment(tok0, n, is_ctx, left_ok, right_ok, d, final):
            nt = n // 128
            nsc = n // 64
            cls = 1 if is_ctx else 0
            for tt in range(nt):
                xi = xc_[0] % 2
                xc_[0] += 1
                P.dma('sp', lambda e, xi=xi, tt=tt: e.dma_start(out=xt[xi][:], in_=T['xs'][tok0 + tt * 128:tok0 + (tt + 1) * 128, :]),
                      writes=[f'bxt{xi}'])
                nrm.run(xt[xi][:], f'bxt{xi}', M['Gm'], M['Sm'], cls,
                        lambda c, tt=tt: hTx[:, c, 1 + tt * 128:1 + (tt + 1) * 128], 'hTx', ['Gm', 'Sm'])
            P.op('dve', lambda e: e.memset(hTx[:, :, 0:1], 0.0), writes=['hTx'])
            P.op('dve', lambda e: e.memset(hTx[:, :, n + 1:n + 2], 0.0), writes=['hTx'])
            if left_ok or right_ok:
                P.op('dve', lambda e: e.memset(xh[:], 1.0), writes=['bxh'])
                if left_ok:
                    P.dma('sp', lambda e: e.dma_start(out=xh[0:1, :], in_=T['xs'][tok0 - 1:tok0, :]), reads=['bxh'], writes=['bxh'])
                if right_ok:
                    P.dma('sp', lambda e: e.dma_start(out=xh[1:2, :], in_=T['xs'][tok0 + n:tok0 + n + 1, :]), reads=['bxh'], writes=['bxh'])
                nrm.run(xh[:], 'bxh', M['Gm'], M['Sm'], cls, None, 'hTx', ['Gm', 'Sm'], npart=2,
                        halo=(hTx, n, left_ok, right_ok))
            P.op('dve', lambda e: e.tensor_tensor(out=xx[:, :, 0:n], in0=hTx[:, :, 0:n], in1=hTx[:, :, 2:n + 2], op=ALU.add),
                 reads=['hTx'], writes=['xx'])
            P.op('dve', lambda e: e.scalar_tensor_tensor(out=xx[:, :, 0:n], in0=xx[:, :, 0:n], scalar=0.5, in1=hTx[:, :, 1:n + 1],
                                                         op0=ALU.mult, op1=ALU.subtract), reads=['xx', 'hTx'], writes=['xx'])
            mc = [0]

            def make_mix(j):
                mi = mc[0] % 2
                mc[0] += 1
                mb = mixb[mi]
                for c in range(8):
                    P.op('dve', lambda e, c=c, mb=mb: e.scalar_tensor_tensor(
                        out=mb[:, c, 0:n], in0=xx[:, c, 0:n], scalar=xmix[:, c, j:j + 1], in1=hTx[:, c, 1:n + 1],
                        op0=ALU.mult, op1=ALU.add), reads=['xx', 'hTx', 'xmix'], writes=[f'mix{mi}'])
                return mb, f'mix{mi}'
            mb, mkey = make_mix(0)
            for cc in range(2):
                P.op('pe', proj_fm(wrkv, cc * 128, 128, mb[:, :, 0:n], pA[:, 0:n], 'wrkv'), reads=['wrkv', mkey], writes=['bpA'])
                P.op('act', lambda e, cc=cc: e.activation(out=rT[:, cc, 0:n], in_=pA[:, 0:n], func=AF.Copy), reads=['bpA'], writes=['rT'])
            mb, mkey = make_mix(2)
            for cc in range(2):
                P.op('pe', proj_fm(wrkv, 256 + cc * 128, 128, mb[:, :, 0:n], pA[:, 0:n], 'wrkv'), reads=['wrkv', mkey], writes=['bpA'])
                P.op('act', lambda e, cc=cc: e.activation(out=kT[:, cc, 0:n], in_=pA[:, 0:n], func=AF.Copy), reads=['bpA'], writes=['kT'])
            mb, mkey = make_mix(3)
            for tt in range(nt):
                def vm(e, tt=tt, mb=mb):
                    for k in range(8):
                        ins = e.matmul(pA[:, 0:256], lhsT=mb[:, k, tt * 128:(tt + 1) * 128], rhs=wrkv[:, k, 512:768],
                                       start=(k == 0), stop=(k == 7))
                    return ins
                P.op('pe', vm, reads=['wrkv', mkey], writes=['bpA'])
                P.op('act', lambda e, tt=tt: e.activation(out=vtok[:, tt, :], in_=pA[:, 0:256], func=AF.Copy), reads=['bpA'], writes=['vtok'])
            mb, mkey = make_mix(1)
            dp = d * 64
            P.op('pe', proj_fm(wl, d * 64, 64, mb[:, :, 0:n], pB[dp:dp + 64, 0:n], 'wl'), reads=['wl', mkey], writes=['bpX'])
            P.op('act', lambda e: e.activation(out=th[dp:dp + 64, 0:n], in_=pB[dp:dp + 64, 0:n], func=AF.Tanh), reads=['bpX'], writes=['th'])
            for cc in range(2):
                P.op('pe', lambda e, cc=cc: e.matmul(pA[:, 0:n], lhsT=w2c[dp:dp + 64, cc * 128:(cc + 1) * 128], rhs=th[dp:dp + 64, 0:n],
                                                     start=True, stop=True), reads=['w2c', 'th'], writes=['bpA'])
                P.op('act', lambda e, cc=cc: e.activation(out=lw[:, cc, 0:n], in_=pA[:, 0:n], func=AF.Sigmoid, bias=colv[:, cc, d:d + 1]),
                     reads=['bpA', 'colv'], writes=['lw'])
            P.op('dve', lambda e: e.tensor_scalar(out=lw[:, :, 0:n], in0=lw[:, :, 0:n], scalar1=DEC_C, scalar2=None, op0=ALU.mult),
                 reads=['lw'], writes=['lw'])
            mb, mkey = make_mix(4)
            P.op('pe', proj_fm(wl, 128 + d * 64, 64, mb[:, :, 0:n], pB[dp:dp + 64, 0:n], 'wl'), reads=['wl', mkey], writes=['bpX'])
            P.op('act', lambda e: e.activation(out=ua[dp:dp + 64, 0:n], in_=pB[dp:dp + 64, 0:n], func=AF.Copy), reads=['bpX'], writes=['ua'])
            for cc in range(2):
                P.op('pe', lambda e, cc=cc: e.matmul(pA[:, 0:n], lhsT=a2c[dp:dp + 64, cc * 128:(cc + 1) * 128], rhs=ua[dp:dp + 64, 0:n],
                                                     start=True, stop=True), reads=['a2c', 'ua'], writes=['bpA'])
                P.op('act', lambda e, cc=cc: e.activation(out=icl[:, cc, 0:n], in_=pA[:, 0:n], func=AF.Sigmoid, bias=colv[:, cc, 2 + d:3 + d]),
                     reads=['bpA', 'colv'], writes=['icl'])
            if final and not is_ctx:
                mb, mkey = make_mix(5)
                P.op('pe', proj_fm(wl, 256, 128, mb[:, :, 0:n], pA[:, 0:n], 'wl'), reads=['wl', mkey], writes=['bpA'])
                P.op('act', lambda e: e.activation(out=sg[:, 0:n], in_=pA[:, 0:n], func=AF.Sigmoid), reads=['bpA'], writes=['sg'])
                P.op('pe', proj_fm(wl, 384, 32, mb[:, :, 0:n], pB[0:32, 0:n], 'wl'), reads=['wl', mkey], writes=['bpX'])
                P.op('act', lambda e: e.activation(out=sg2[:, 0:n], in_=pB[0:32, 0:n], func=AF.Sigmoid), reads=['bpX'], writes=['sg2'])
                for tt in range(nt):
                    def gm(e, tt=tt):
                        e.matmul(pA[:, 0:256], lhsT=sg[:, tt * 128:(tt + 1) * 128], rhs=g2a[:, :], start=True, stop=False)
                        return e.matmul(pA[:, 0:256], lhsT=sg2[:, tt * 128:(tt + 1) * 128], rhs=g2b[:, :], start=False, stop=True)
                    P.op('pe', gm, reads=['sg', 'sg2', 'g2a', 'g2b'], writes=['bpA'])
                    P.op('act', lambda e, tt=tt: e.activation(out=gtok[:, tt, :], in_=pA[:, 0:256], func=AF.Copy), reads=['bpA'], writes=['gtok'])
            V3 = lambda t_: t_[:, :, 0:n]
            for cc in range(2):
                P.op('dve', lambda e, cc=cc: e.tensor_scalar(out=kkn[:, cc, 0:n], in0=kT[:, cc, 0:n], scalar1=colv[:, cc, 4:5], scalar2=None,
                                                             op0=ALU.mult), reads=['kT', 'colv'], writes=['kkn'])
                P.op('act', lambda e, cc=cc: e.activation(out=sqb[:, 0:n], in_=kkn[:, cc, 0:n], func=AF.Square), reads=['kkn'], writes=['sqb'])
                P.op('pe', lambda e: e.matmul(pB[:, 0:n], lhsT=C['bones'][:], rhs=sqb[:, 0:n], start=True, stop=True),
                     reads=['sqb', 'bones'], writes=['bpX'])
                P.op('dve', lambda e, cc=cc: e.tensor_scalar(out=tmpA[:, cc, 0:n], in0=pB[:, 0:n], scalar1=1e-18, scalar2=None, op0=ALU.max),
                     reads=['bpX'], writes=['tmpA'])
            P.op('act', lambda e: e.activation(out=V3(tmpA), in_=V3(tmpA), func=AF.Ln), reads=['tmpA'], writes=['tmpA'])
            P.op('act', lambda e: e.activation(out=V3(tmpA), in_=V3(tmpA), func=AF.Exp, scale=-0.5), reads=['tmpA'], writes=['tmpA'])
            P.op('dve', lambda e: e.tensor_tensor(out=V3(kkn), in0=V3(kkn), in1=V3(tmpA), op=ALU.mult), reads=['kkn', 'tmpA'], writes=['kkn'])
            for cc in range(2):
                P.op('dve', lambda e, cc=cc: e.tensor_scalar(out=kd[:, cc, 0:n], in0=icl[:, cc, 0:n], scalar1=-1.0, scalar2=colv[:, cc, 5:6],
                                                             op0=ALU.add, op1=ALU.mult), reads=['icl', 'colv'], writes=['kd'])
            P.op('dve', lambda e: e.scalar_tensor_tensor(out=V3(kd), in0=V3(kd), scalar=1.0, in1=V3(kT), op0=ALU.add, op1=ALU.mult),
                 reads=['kd', 'kT'], writes=['kd'])
            P.op('dve', lambda e: e.tensor_tensor(out=V3(rkb), in0=V3(rT), in1=V3(kd), op=ALU.mult), reads=['rT', 'kd'], writes=['rkb'])
            for tt in range(nt):
                def bm(e, tt=tt):
                    for cc in range(2):
                        ins = e.matmul(pB[:, cc * 2:cc * 2 + 2], lhsT=rkb[:, cc, tt * 128:(tt + 1) * 128], rhs=rkw[:, cc, :], start=True, stop=True)
                    return ins
                P.op('pe', bm, reads=['rkb', 'rkw'], writes=['bpX'])
                P.op('dve', lambda e, tt=tt: e.tensor_copy(out=bon[:, tt, :], in_=pB[:, 0:4]), reads=['bpX'], writes=['bon'])
            for cc in range(2):
                P.op('dve', lambda e, cc=cc: e.tensor_tensor_scan(out=cin[:, cc, 0:n], data0=rmask[:, 0:n], data1=lw[:, cc, 0:n], initial=0.0,
                                                                  op0=ALU.mult, op1=ALU.add), reads=['rmask', 'lw'], writes=['cin'])
            c4 = lambda t_: t_[:, :, 0:n].rearrange("p c (k t) -> p c k t", t=64)
            P.op('dve', lambda e: e.tensor_copy(out=tot[:, :, 0:nsc], in_=c4(cin)[:, :, :, 63]), reads=['cin'], writes=['tot'])
            totb = lambda: tot[:, :, 0:nsc].unsqueeze(3).broadcast_to([128, 2, nsc, 64])
            if d == 1:
                P.op('dve', lambda e: e.tensor_tensor(out=V3(cin), in0=V3(lw), in1=V3(cin), op=ALU.subtract), reads=['lw', 'cin'], writes=['cin'])
                P.op('dve', lambda e: e.tensor_tensor(out=c4(cin), in0=c4(cin), in1=totb(), op=ALU.add), reads=['cin', 'tot'], writes=['cin'])
            P.op('act', lambda e: e.activation(out=gC[:, :, 0:nsc], in_=tot[:, :, 0:nsc], func=AF.Exp), reads=['tot'], writes=['gC'])
            P.op('dve', lambda e: e.tensor_tensor(out=c4(e2), in0=totb(), in1=c4(cin), op=ALU.subtract), reads=['cin', 'tot'], writes=['e2'])
            P.op('act', lambda e: e.activation(out=V3(e2), in_=V3(e2), func=AF.Exp), reads=['e2'], writes=['e2'])
            P.op('act', lambda e: e.activation(out=V3(tmpA), in_=V3(cin), func=AF.Exp), reads=['cin'], writes=['tmpA'])
            P.op('dve', lambda e: e.tensor_tensor(out=arT[:, :, 1, 0:n], in0=V3(rT), in1=V3(tmpA), op=ALU.mult), reads=['rT', 'tmpA'], writes=['arT'])
            P.op('dve', lambda e: e.tensor_tensor(out=V3(tmpB), in0=V3(cin), in1=V3(lw), op=ALU.subtract), reads=['cin', 'lw'], writes=['tmpB'])
            P.op('act', lambda e: e.activation(out=V3(tmpB), in_=V3(tmpB), func=AF.Exp), reads=['tmpB'], writes=['tmpB'])
            P.op('dve', lambda e: e.scalar_tensor_tensor(out=arT[:, :, 0, 0:n], in0=V3(kkn), scalar=-1.0, in1=V3(tmpB), op0=ALU.mult, op1=ALU.mult),
                 reads=['kkn', 'tmpB'], writes=['arT'])
            P.op('act', lambda e: e.activation(out=V3(tmpA), in_=V3(cin), func=AF.Exp, scale=-1.0), reads=['cin', 'arT'], writes=['tmpA'])
            P.op('dve', lambda e: e.tensor_tensor(out=V3(tmpB), in0=V3(kkn), in1=V3(icl), op=ALU.mult), reads=['kkn', 'icl', 'arT'], writes=['tmpB'])
            P.op('dve', lambda e: e.tensor_tensor(out=V3(bhT), in0=V3(tmpB), in1=V3(tmpA), op=ALU.mult), reads=['tmpB', 'tmpA'], writes=['bhT'])
            P.op('dve', lambda e: e.tensor_tensor(out=V3(khT), in0=V3(kd), in1=V3(tmpA), op=ALU.mult), reads=['kd', 'tmpA'], writes=['khT'])
            P.op('dve', lambda e: e.tensor_tensor(out=V3(bgT), in0=V3(tmpB), in1=V3(e2), op=ALU.mult), reads=['tmpB', 'e2'], writes=['bgT'])
            P.op('dve', lambda e: e.tensor_tensor(out=V3(kgT), in0=V3(kd), in1=V3(e2), op=ALU.mult), reads=['kd', 'e2'], writes=['kgT'])
            if d == 0:
                m_s, mA, mAt, mO1, mO1T, mO2T = 0, 4, 5, 6, 7, 9
            else:
                m_s, mA, mAt, mO1, mO1T, mO2T = 2, 5, 4, 7, 6, 8
            order = list(range(nt)) if d == 0 else list(range(nt - 1, -1, -1))
            corder = [0, 1] if d == 0 else [1, 0]
            for tt in order:
                cs = slice(tt * 128, (tt + 1) * 128)
                def trs(e, cs=cs):
                    for qi, src in enumerate((None, bgT, kgT)):
                        for cc in range(2):
                            s_ap = arT[:, cc, 0, cs] if qi == 0 else src[:, cc, cs]
                            ins = e.transpose(out=pTt[:, qi, cc * 128:(cc + 1) * 128], in_=s_ap, identity=identb[:])
                    return ins
                P.op('pe', trs, reads=['arT', 'bgT', 'kgT', 'identb'], writes=['bpTt'])
                P.op('act', lambda e: e.activation(out=tk[:], in_=pTt, func=AF.Copy), reads=['bpTt'], writes=['tk'])
                P.op('dve', lambda e: e.tensor_copy(out=XA[:, :, 1, :], in_=tk[:, 0, :].rearrange("p (h k) -> p h k", h=4)),
                     reads=['tk'], writes=['XA'])
                for hh in range(2):
                    hp = hh * 64
                    hs = slice(hh, 4, 2)

                    def gr(e, hp=hp, cs=cs, src=bhT, gp=pG[0]):
                        for cc in range(2):
                            ins = e.matmul(gp[:, cc, :, :], lhsT=src[hp:hp + 64, cc, cs], rhs=arT[hp:hp + 64, cc, :, cs], start=True, stop=True)
                        return ins
                    P.op('pe', gr, reads=['bhT', 'arT'], writes=['bpG0'])
                    P.op('dve', lambda e, hs=hs: e.tensor_tensor(
                        out=U1[:, hs, :, :], in0=pG[0][:],
                        in1=mk[:, m_s:m_s + 2, :].unsqueeze(1).broadcast_to([128, 2, 2, 128]), op=ALU.mult),
                        reads=['bpG0', 'mk'], writes=['U1'])
                    P.op('dve', lambda e, hs=hs: e.tensor_tensor(
                        out=AA[0][:, hs, 0, :], in0=pG[0][:, :, 0, :],
                        in1=mk[:, mA, :].unsqueeze(1).broadcast_to([128, 2, 128]), op=ALU.mult),
                        reads=['bpG0', 'mk'], writes=['AA0'])
                    P.op('dve', lambda e, hs=hs: e.tensor_tensor(
                        out=UoffT[:, hs, 1, :], in0=pG[0][:, :, 0, :],
                        in1=mk[:, mO1, :].unsqueeze(1).broadcast_to([128, 2, 128]), op=ALU.mult),
                        reads=['bpG0', 'mk'], writes=['UoffT'])
                    P.op('pe', lambda e, hp=hp, cs=cs: gr(e, hp, cs, khT, pG[1]), reads=['khT', 'arT'], writes=['bpG1'])
                    P.op('dve', lambda e, hs=hs: e.tensor_tensor(
                        out=U2[:, hs, :, :], in0=pG[1][:],
                        in1=mk[:, m_s:m_s + 2, :].unsqueeze(1).broadcast_to([128, 2, 2, 128]), op=ALU.mult),
                        reads=['bpG1', 'mk'], writes=['U2'])

                    def gt(e, hp=hp, cs=cs):
                        for cc in range(2):
                            ins = e.matmul(pG[0][:, cc, 0, :], lhsT=arT[hp:hp + 64, cc, 0, cs], rhs=bhT[hp:hp + 64, cc, cs], start=True, stop=True)
                        return ins
                    P.op('pe', gt, reads=['bhT', 'arT'], writes=['bpG0'])
                    P.op('dve', lambda e, hs=hs: e.tensor_tensor(
                        out=AB[0][:, hs, 0, :], in0=pG[0][:, :, 0, :],
                        in1=mk[:, mAt, :].unsqueeze(1).broadcast_to([128, 2, 128]), op=ALU.mult),
                        reads=['bpG0', 'mk'], writes=['AB0'])
                    P.op('dve', lambda e, hs=hs: e.tensor_tensor(
                        out=UoffT[:, hs, 0, :], in0=pG[0][:, :, 0, :],
                        in1=mk[:, mO1T, :].unsqueeze(1).broadcast_to([128, 2, 128]), op=ALU.mult),
                        reads=['bpG0', 'mk'], writes=['UoffT'])
                    P.op('dve', lambda e, hs=hs: e.tensor_tensor(
                        out=UoffT[:, hs, 2, :], in0=pG[0][:, :, 0, :],
                        in1=mk[:, mO2T, :].unsqueeze(1).broadcast_to([128, 2, 128]), op=ALU.mult),
                        reads=['bpG0', 'mk'], writes=['UoffT'])
                P.op('dve', lambda e: e.tensor_copy(out=AA[0][:, :, 1, :], in_=ident[:].unsqueeze(1).broadcast_to([128, 4, 128])),
                     reads=['ident'], writes=['AA0'])
                P.op('dve', lambda e: e.tensor_copy(out=AB[0][:, :, 1, :], in_=ident[:].unsqueeze(1).broadcast_to([128, 4, 128])),
                     reads=['ident'], writes=['AB0'])
                cur = 0
                NIT = 4
                for it in range(NIT):
                    nxt = 1 - cur
                    last = (it == NIT - 1)
                    for cc in range(2):
                        gp = pG[cc]
                        hs = slice(cc * 2, cc * 2 + 2)

                        def im(e, cc=cc, cur=cur, gp=gp, last=last):
                            for hh in range(2):
                                h = cc * 2 + hh
                                if last:
                                    ins = e.matmul(gp[:, hh, 1, :], lhsT=AB[cur][:, h, 0, :], rhs=AA[cur][:, h, 1, :], start=True, stop=True)
                                else:
                                    ins = e.matmul(gp[:, hh, :, :], lhsT=AB[cur][:, h, 0, :], rhs=AA[cur][:, h, :, :], start=True, stop=True)
                            return ins
                        P.op('pe', im, reads=[f'AB{cur}', f'AA{cur}'], writes=[f'bpG{cc}'])
                        P.op('dve', lambda e, hs=hs, cur=cur, nxt=nxt, gp=gp: e.tensor_tensor(
                            out=AA[nxt][:, hs, 1, :], in0=gp[:, :, 1, :], in1=AA[cur][:, hs, 1, :], op=ALU.add),
                            reads=[f'bpG{cc}', f'AA{cur}'], writes=[f'AA{nxt}'])
                        if not last:
                            P.op('act', lambda e, hs=hs, nxt=nxt, gp=gp: e.activation(
                                out=AA[nxt][:, hs, 0, :], in_=gp[:, :, 0, :], func=AF.Copy),
                                reads=[f'bpG{cc}'], writes=[f'AA{nxt}'])

                        def im2(e, cc=cc, cur=cur, gp=gp, last=last):
                            for hh in range(2):
                                h = cc * 2 + hh
                                if last:
                                    ins = e.matmul(gp[:, hh, 1, :], lhsT=AA[cur][:, h, 0, :], rhs=AB[cur][:, h, 1, :], start=True, stop=True)
                                else:
                                    ins = e.matmul(gp[:, hh, :, :], lhsT=AA[cur][:, h, 0, :], rhs=AB[cur][:, h, :, :], start=True, stop=True)
                            return ins
                        P.op('pe', im2, reads=[f'AB{cur}', f'AA{cur}'], writes=[f'bpG{cc}'])
                        P.op('dve', lambda e, hs=hs, cur=cur, nxt=nxt, gp=gp: e.tensor_tensor(
                            out=AB[nxt][:, hs, 1, :], in0=gp[:, :, 1, :], in1=AB[cur][:, hs, 1, :], op=ALU.add),
                            reads=[f'bpG{cc}', f'AB{cur}'], writes=[f'AB{nxt}'])
                        if not last:
                            P.op('act', lambda e, hs=hs, nxt=nxt, gp=gp: e.activation(
                                out=AB[nxt][:, hs, 0, :], in_=gp[:, :, 0, :], func=AF.Copy),
                                reads=[f'bpG{cc}'], writes=[f'AB{nxt}'])
                    cur = nxt
                for cc in range(2):
                    gp = pG[cc]
                    hs = slice(cc * 2, cc * 2 + 2)

                    def z1(e, cc=cc, cur=cur, gp=gp):
                        for hh in range(2):
                            h = cc * 2 + hh
                            e.matmul(gp[:, hh, 0, :], lhsT=UoffT[:, h, 0, :], rhs=AA[cur][:, h, 1, :], start=True, stop=True)
                            ins = e.matmul(gp[:, hh, 1, :], lhsT=UoffT[:, h, 1, :], rhs=AB[cur][:, h, 1, :], start=True, stop=True)
                        return ins
                    P.op('pe', z1, reads=['UoffT', f'AA{cur}', f'AB{cur}'], writes=[f'bpG{cc}'])
                    P.op('act', lambda e, hs=hs, gp=gp: e.activation(out=Zs[:, hs, :, :], in_=gp[:], func=AF.Copy),
                         reads=[f'bpG{cc}'], writes=['Zs'])

                    def t1(e, cc=cc, cur=cur, gp=gp):
                        for hh in range(2):
                            h = cc * 2 + hh
                            e.matmul(gp[:, hh, 0, :], lhsT=AB[cur][:, h, 1, :], rhs=Zs[:, h, 0, :], start=True, stop=True)
                            ins = e.matmul(gp[:, hh, 1, :], lhsT=AA[cur][:, h, 1, :], rhs=Zs[:, h, 1, :], start=True, stop=True)
                        return ins
                    P.op('pe', t1, reads=['Zs', f'AA{cur}', f'AB{cur}'], writes=[f'bpG{cc}'])
                    P.op('dve', lambda e, hs=hs, cur=cur, gp=gp: e.tensor_tensor(
                        out=T32[:, hs, 0, :], in0=gp[:, :, 0, :], in1=AA[cur][:, hs, 1, :], op=ALU.add),
                        reads=[f'bpG{cc}', f'AA{cur}'], writes=['T32'])
                    P.op('dve', lambda e, hs=hs, cur=cur, gp=gp: e.tensor_tensor(
                        out=T32[:, hs, 1, :], in0=gp[:, :, 1, :], in1=AB[cur][:, hs, 1, :], op=ALU.add),
                        reads=[f'bpG{cc}', f'AB{cur}'], writes=['T32'])

                    def z2(e, cc=cc, gp=gp):
                        for hh in range(2):
                            h = cc * 2 + hh
                            ins = e.matmul(gp[:, hh, 0, :], lhsT=UoffT[:, h, 2, :], rhs=T32[:, h, 0, :], start=True, stop=True)
                        return ins
                    P.op('pe', z2, reads=['UoffT', 'T32'], writes=[f'bpG{cc}'])
                    P.op('act', lambda e, hs=hs, gp=gp: e.activation(out=Zs[:, hs, 0, :], in_=gp[:, :, 0, :], func=AF.Copy),
                         reads=[f'bpG{cc}'], writes=['Zs'])

                    def t2(e, cc=cc, gp=gp):
                        for hh in range(2):
                            h = cc * 2 + hh
                            ins = e.matmul(gp[:, hh, 1, :], lhsT=T32[:, h, 1, :], rhs=Zs[:, h, 0, :], start=True, stop=True)
                        return ins
                    P.op('pe', t2, reads=['Zs', 'T32'], writes=[f'bpG{cc}'])
                    P.op('dve', lambda e, hs=hs, gp=gp: e.tensor_tensor(
                        out=Tfin[:, hs, :], in0=gp[:, :, 1, :], in1=T32[:, hs, 0, :], op=ALU.add),
                        reads=[f'bpG{cc}', 'T32'], writes=['Tfin'])
                def xm(e, tt=tt):
                    for h in range(4):
                        ins = e.matmul(pX[:, h, 0, :], lhsT=U2[:, h, 0, :], rhs=vtok[:, tt, h * 64:(h + 1) * 64], start=True, stop=True)
                    return ins
                P.op('pe', xm, reads=['U2', 'vtok'], writes=['bpX'])
                P.op('act', lambda e: e.activation(out=XA[:, :, 0, :], in_=pX[:, :, 0, :], func=AF.Copy), reads=['bpX'], writes=['XA'])

                def wm(e):
                    for h in range(4):
                        ins = e.matmul(pX[:, h, :, :], lhsT=Tfin[:, h, :], rhs=XA[:, h, :, :], start=True, stop=True)
                    return ins
                P.op('pe', wm, reads=['Tfin', 'XA'], writes=['bpX'])
                P.op('act', lambda e: e.activation(out=WA[:].rearrange("p a h k -> p h a k"), in_=pX, func=AF.Copy), reads=['bpX'], writes=['WA'])

                def rm(e):
                    for cc in range(2):
                        for hh in range(2):
                            h = cc * 2 + hh
                            hp = hh * 64
                            ins = e.matmul(pG[0][hp:hp + 64, cc, 0, :], lhsT=WA[:, 1, h, :], rhs=U1[:, h, 1, :], start=True, stop=True)
                    return ins
                P.op('pe', rm, reads=['WA', 'U1'], writes=['bpG0'])
                P.op('dve', lambda e, cs=cs: e.tensor_tensor(out=RtT[:], in0=pG[0][:, :, 0, :], in1=arT[:, :, 1, cs], op=ALU.add),
                     reads=['bpG0', 'arT'], writes=['RtT'])
                for c in range(2):
                    pc = slice(c * 64, (c + 1) * 64)
                    pMc = pG[1 - c][:].rearrange("p a b t -> p (a b) t")

                    def mmm(e, c=c, pc=pc, pMc=pMc):
                        for cc in range(2):
                            ins = e.matmul(pMc[:, cc, :], lhsT=WA[pc, 1, cc * 2:cc * 2 + 2, :].rearrange("p h k -> p (h k)"),
                                           rhs=tk[pc, 1, cc * 128:(cc + 1) * 128], start=True, stop=True)
                        return ins
                    P.op('pe', mmm, reads=['WA', 'tk'], writes=[f'bpG{1 - c}'])
                    for cc in range(2):
                        P.op('dve', lambda e, c=c, cc=cc, tt=tt, pMc=pMc: e.scalar_tensor_tensor(
                            out=Msb[:, cc, c, :], in0=ident[:], scalar=gC[:, cc, tt * 2 + c:tt * 2 + c + 1], in1=pMc[:, cc, :],
                            op0=ALU.mult, op1=ALU.add), reads=[f'bpG{1 - c}', 'gC', 'ident'], writes=['Msb'])
                if not is_ctx:
                    def y0m(e, tt=tt):
                        for h in range(4):
                            e.matmul(pY[:, h, :], lhsT=U1[:, h, 1, :], rhs=WA[:, 0, h, :], start=(h == 0), stop=False, skip_group_check=True)
                            ins = e.matmul(pY[:, h, :], lhsT=U2[:, h, 1, :], rhs=vtok[:, tt, h * 64:(h + 1) * 64], start=False, stop=False,
                                           skip_group_check=True)
                        return ins
                    P.op('pe', y0m, reads=['U1', 'U2', 'WA', 'vtok'], writes=['bpY'])
                for ci, c in enumerate(corder):
                    pc = slice(c * 64, (c + 1) * 64)
                    if not is_ctx:
                        def ycm(e, pc=pc, ci=ci):
                            for cc in range(2):
                                ins = e.matmul(pYf[pc, cc * 128:(cc + 1) * 128], lhsT=RtT[:, cc, pc], rhs=Sbd[:, cc, :], start=False,
                                               stop=(ci == 1), skip_group_check=True)
                            return ins
                        P.op('pe', ycm, reads=['RtT', 'Sbd'], writes=['bpY'])

                    def scm(e, pc=pc, c=c, tt=tt):
                        for cc in range(2):
                            o_ = pSf[:, cc * 128:(cc + 1) * 128]
                            e.matmul(o_, lhsT=tk[pc, 1, cc * 128:(cc + 1) * 128], rhs=WA[pc, 0, cc * 2:cc * 2 + 2, :].rearrange("p h k -> p (h k)"),
                                     start=(cc == 0), stop=False, skip_group_check=True)
                            e.matmul(o_, lhsT=tk[pc, 2, cc * 128:(cc + 1) * 128], rhs=vtok[pc, tt, cc * 128:(cc + 1) * 128],
                                     start=False, stop=False, skip_group_check=True)
                            ins = e.matmul(o_, lhsT=Msb[:, cc, c, :], rhs=Sbd[:, cc, :], start=False, stop=True, skip_group_check=True)
                        return ins
                    P.op('pe', scm, reads=['tk', 'WA', 'vtok', 'Msb', 'Sbd'], writes=['bpS'])
                    pS3 = pSf[:, 0:256].rearrange("p (c x) -> p c x", c=2)
                    P.op('act', lambda e, pS3=pS3: e.activation(out=Sbd[0:64, :, 0:64], in_=pS3[0:64, :, 0:64], func=AF.Copy),
                         reads=['bpS'], writes=['Sbd'])
                    P.op('dve', lambda e, pS3=pS3: e.tensor_copy(out=Sbd[64:128, :, 64:128], in_=pS3[64:128, :, 64:128]),
                         reads=['bpS'], writes=['Sbd'])
                if is_ctx:
                    continue
                row0 = tok0 - 256 + tt * 128
                if not final:
                    P.op('dve', lambda e: e.tensor_copy(out=ysb[:], in_=pYf[:, 0:256]), reads=['bpY'], writes=['ysb'])
                    P.dma('sp', lambda e, row0=row0: e.dma_start(out=T['y0'][row0:row0 + 128, :], in_=ysb[:]), reads=['ysb'], writes=['y0'])
                    P.dma('sp', lambda e, row0=row0, tt=tt: e.dma_start(out=T['bon0'][row0:row0 + 128, :], in_=bon[:, tt, :]), reads=['bon'], writes=['bon0'])
                else:
                    P.dma('sp', lambda e, row0=row0: e.dma_start(out=y0t[:], in_=T['y0'][row0:row0 + 128, :]), reads=['y0'], writes=['y0t'])
                    P.dma('sp', lambda e, row0=row0: e.dma_start(out=b0t[:], in_=T['bon0'][row0:row0 + 128, :]), reads=['bon0'], writes=['b0t'])
                    P.op('dve', lambda e: e.tensor_tensor(out=ysb[:], in0=pYf[:, 0:256], in1=y0t[:], op=ALU.add),
                         reads=['bpY', 'y0t'], writes=['ysb'])
                    P.op('dve', lambda e, tt=tt: e.tensor_tensor(out=b0t[:], in0=b0t[:], in1=bon[:, tt, :], op=ALU.add), reads=['b0t', 'bon'], writes=['b0t'])
                    y3 = ysb[:].rearrange("p (h v) -> p h v", h=4)
                    P.op('dve', lambda e: e.tensor_reduce(out=st8[:, 0:4], in_=y3, axis=AX.X, op=ALU.add), reads=['ysb'], writes=['st8'])
                    P.op('dve', lambda e: e.tensor_scalar(out=st8[:, 0:4], in0=st8[:, 0:4], scalar1=1.0 / 64, scalar2=None, op0=ALU.mult),
                         reads=['st8'], writes=['st8'])
                    P.op('dve', lambda e: e.tensor_tensor(out=y3, in0=y3, in1=st8[:, 0:4].unsqueeze(2).broadcast_to([128, 4, 64]), op=ALU.subtract),
                         reads=['ysb', 'st8'], writes=['ysb'])
                    P.op('dve', lambda e: e.tensor_tensor(out=osb[:], in0=ysb[:], in1=ysb[:], op=ALU.mult), reads=['ysb'], writes=['osb'])
                    P.op('dve', lambda e: e.tensor_reduce(out=st8[:, 4:8], in_=osb[:].rearrange("p (h v) -> p h v", h=4), axis=AX.X, op=ALU.add),
                         reads=['osb'], writes=['st8'])
                    P.op('act', lambda e: e.activation(out=st8[:, 4:8], in_=st8[:, 4:8], func=AF.Ln, scale=1.0 / 64, bias=64e-5), reads=['st8'], writes=['st8'])
                    P.op('act', lambda e: e.activation(out=st8[:, 4:8], in_=st8[:, 4:8], func=AF.Exp, scale=-0.5), reads=['st8'], writes=['st8'])
                    P.op('dve', lambda e: e.tensor_tensor(out=y3, in0=y3, in1=st8[:, 4:8].unsqueeze(2).broadcast_to([128, 4, 64]), op=ALU.mult),
                         reads=['ysb', 'st8'], writes=['ysb'])
                    P.op('dve', lambda e: e.tensor_tensor(out=ysb[:], in0=ysb[:], in1=lnrep[:, 0, :], op=ALU.mult), reads=['ysb', 'lnrep'], writes=['ysb'])
                    P.op('dve', lambda e: e.tensor_tensor(out=ysb[:], in0=ysb[:], in1=lnrep[:, 1, :], op=ALU.add), reads=['ysb', 'lnrep'], writes=['ysb'])
                    P.op('dve', lambda e, tt=tt: e.tensor_tensor(
                        out=osb[:].rearrange("p (h v) -> p h v", h=4), in0=vtok[:, tt, :].rearrange("p (h v) -> p h v", h=4),
                        in1=b0t[:, 0:4].unsqueeze(2).broadcast_to([128, 4, 64]), op=ALU.mult), reads=['vtok', 'b0t'], writes=['osb'])
                    P.op('dve', lambda e: e.tensor_tensor(out=osb[:], in0=osb[:], in1=ysb[:], op=ALU.add), reads=['osb', 'ysb'], writes=['osb'])
                    P.op('dve', lambda e, tt=tt: e.tensor_tensor(out=osb[:], in0=osb[:], in1=gtok[:, tt, :], op=ALU.mult), reads=['osb', 'gtok'], writes=['osb'])
                    P.dma('sp', lambda e, row0=row0: e.dma_start(out=T['og'][row0:row0 + 128, :], in_=osb[:]), reads=['osb'], writes=['og'])

        for d in range(2):
            P.op('dve', lambda e: e.memset(Sbd[:], 0.0), writes=['Sbd'])
            do_segment(0, 256, True, False, False, d, d == 1)
            segs = list(range(NL)) if d == 0 else list(range(NL - 1, -1, -1))
            for si in segs:
                do_segment(256 + si * 512, 512, False, si > 0, si < NL - 1, d, d == 1)
        P.barrier()


def build_B(NL):
    nc = bass.Bass("TRN2", target_bir_lowering=False)
    def din(name, shape):
        return nc.dram_tensor(name, shape, F32, kind="ExternalInput").ap()
    T = {}
    NTOK = 256 + NL * 512
    T['xs'] = din("xs", [NTOK, 1024])
    T['xmixT'] = din("xmixT", [128, 8, 6])
    T['wrkv'] = din("wrkv", [1024, 768])
    T['w1cat'] = din("w1cat", [1024, 128])
    T['a1cat'] = din("a1cat", [1024, 128])
    T['g1'] = din("g1", [1024, 160])
    T['w2cat'] = din("w2cat", [128, 256])
    T['a2cat'] = din("a2cat", [128, 256])
    T['g2a'] = din("g2a", [128, 256])
    T['g2b'] = din("g2b", [32, 256])
    T['colvec'] = din("colvec", [128, 2, 6])
    T['rkw'] = din("rkw", [128, 2, 2])
    T['lnrep'] = din("lnrep", [128, 2, 256])
    T['mk'] = din("mk", [10, 128, 128])
    cT = din("cT", [128, 8, 2])
    ada_w = din("ada_w", [1024, 6144])
    ada_bT = din("ada_bT", [128, 48])
    adab_rep = din("adab_rep", [128, 2, 1024])
    gmixT = din("gmixT", [128, 8])
    gffnT = din("gffnT", [128, 8])
    dout = lambda name, shape: nc.dram_tensor(name, shape, F32, kind="ExternalOutput").ap()
    T['y0'] = dout("y0", [NL * 512, 256])
    T['bon0'] = dout("bon0", [NL * 512, 4])
    T['og'] = dout("og", [NL * 512, 256])
    T['gdram'] = dout("gdram", [2, 128, 2, 1024])
    P = Prog(nc)
    P.psum_keys = PSUM_KEYS
    with ExitStack() as st:
        sb = _sb(nc, st)
        C = make_consts(P, nc, sb)
        M = mod_setup(P, nc, st, C, cT, ada_w, ada_bT, adab_rep, gmixT, gffnT, T['gdram'])
        rwkv_phase(P, nc, C, M, T, NL)
        P.final_wait('sp')
        P.emit(st)
    print("B ops:", P.nops, {e: len(P.streams[e]) for e in P.ENG})
    return nc


def host_B(inp, xs_b, core):
    b, hg = core // 4, core % 4
    cols = slice(hg * 256, (hg + 1) * 256)
    m = {}
    m['xs'] = xs_b
    m['xmixT'] = inp['rwkv_x_mix'][0].reshape(6, 8, 128).transpose(2, 1, 0)
    m['wrkv'] = np.concatenate([inp['rwkv_w_r'][0][:, cols], inp['rwkv_w_k'][0][:, cols], inp['rwkv_w_v'][0][:, cols]], 1)
    m['w1cat'] = np.concatenate([inp['rwkv_decay_w1'][0, 0], inp['rwkv_decay_w1'][0, 1]], 1)
    m['a1cat'] = np.concatenate([inp['rwkv_iclr_a1'][0, 0], inp['rwkv_iclr_a1'][0, 1]], 1)
    m['g1'] = inp['rwkv_gate_g1'][0]
    m['w2cat'] = np.concatenate([inp['rwkv_decay_w2'][0, 0][:, cols], inp['rwkv_decay_w2'][0, 1][:, cols]], 0)
    m['a2cat'] = np.concatenate([inp['rwkv_iclr_a2'][0, 0][:, cols], inp['rwkv_iclr_a2'][0, 1][:, cols]], 0)
    g2 = inp['rwkv_gate_g2'][0][:, cols]
    m['g2a'] = g2[0:128]
    m['g2b'] = g2[128:160]
    vecs = [inp['rwkv_decay_w0'][0, 0], inp['rwkv_decay_w0'][0, 1], inp['rwkv_iclr_a0'][0, 0], inp['rwkv_iclr_a0'][0, 1],
            inp['rwkv_k_k'][0], inp['rwkv_k_a'][0]]
    cv = np.stack([v[cols].reshape(2, 128) for v in vecs], 2)
    m['colvec'] = cv.transpose(1, 0, 2)
    rkw = np.zeros((128, 2, 2), np.float32)
    for cc in range(2):
        for hh in range(2):
            rkw[hh * 64:(hh + 1) * 64, cc, hh] = inp['rwkv_r_k'][0][hg * 4 + cc * 2 + hh]
    m['rkw'] = rkw
    m['lnrep'] = np.broadcast_to(np.stack([inp['rwkv_ln_g'][0][cols], inp['rwkv_ln_b'][0][cols]])[None], (128, 2, 256))
    r = np.arange(128)[:, None]
    c = np.arange(128)[None, :]
    b64 = (r // 64) == (c // 64)
    b32 = (r // 32) == (c // 32)
    b16 = (r // 16) == (c // 16)
    S64 = (r < c) & b64
    I64 = (r <= c) & b64
    S16 = (r < c) & b16
    O1 = (r < c) & b32 & ~b16
    O2 = (r < c) & b64 & ~b32
    m['mk'] = np.stack([S64, I64, S64.T, I64.T, S16, S16.T, O1, O1.T, O2, O2.T]).astype(np.float32)
    m.update(host_mod(inp, b, 1))
    return {k_: np.ascontiguousarray(v, dtype=np.float32) for k_, v in m.items()}


def wo_phase(P, nc, C, T, NT):
    with ExitStack() as st:
        sb = _sb(nc, st)
        ps = _ps(nc, st)
        wo = sb("wo", [128, 8, 1024], BF16)
        stg = [sb(f"cstg{i}", [128, 1024], F32) for i in range(2)]
        g2rep = sb("cg2rep", [128, 1024], F32)
        ot32 = [sb(f"ot32_{i}", [128, 8, 128], F32) for i in range(2)]
        otb = [sb(f"otb_{i}", [128, 8, 128], BF16) for i in range(2)]
        xt = [sb(f"cxt{i}", [128, 1024], F32) for i in range(2)]
        ty = [sb(f"cty{i}", [128, 1024], F32) for i in range(2)]
        pA = [ps(f"cpA{i}", [128, 512], F32) for i in range(2)]
        P.dma('sp', lambda e: e.dma_start(out=g2rep[:], in_=T['gdram'][0, :, 0, :]), reads=['g_dram'], writes=['cg2rep'])
        wsrc = T['wo'].rearrange("(c p) n -> p c n", p=128)
        for k in range(8):
            P.dma('sp', lambda e, k=k: e.dma_start(out=stg[k % 2][:], in_=wsrc[:, k, :]), writes=[f'cstg{k % 2}'])
            P.op('pool', lambda e, k=k: e.tensor_copy(out=wo[:, k, :], in_=stg[k % 2][:]), reads=[f'cstg{k % 2}'], writes=['wo'])
        osrc = T['oT'].rearrange("(c p) n -> p c n", p=128)
        for t in range(NT):
            i = t % 2
            P.dma('sp', lambda e, t=t, i=i: e.dma_start(out=ot32[i][:], in_=osrc[:, :, t * 128:(t + 1) * 128]), writes=[f'ot32_{i}'])
            P.dma('sp', lambda e, t=t, i=i: e.dma_start(out=xt[i][:], in_=T['xin'][t * 128:(t + 1) * 128, :]), writes=[f'cxt{i}'])
            P.op('pool', lambda e, i=i: e.tensor_copy(out=otb[i][:], in_=ot32[i][:]), reads=[f'ot32_{i}'], writes=[f'otb_{i}'])
            for half in range(2):
                pa = pA[half]

                def om(e, i=i, half=half, pa=pa):
                    for k in range(8):
                        ins = e.matmul(pa[:], lhsT=otb[i][:, k, :], rhs=wo[:, k, half * 512:(half + 1) * 512], start=(k == 0), stop=(k == 7))
                    return ins
                P.op('pe', om, reads=[f'otb_{i}', 'wo'], writes=[f'cpA{half}'])
                P.op('dve', lambda e, i=i, half=half, pa=pa: e.tensor_tensor(
                    out=ty[i][:, half * 512:(half + 1) * 512], in0=pa[:], in1=g2rep[:, half * 512:(half + 1) * 512], op=ALU.mult),
                    reads=[f'cpA{half}', 'cg2rep'], writes=[f'cty{i}'])
            P.op('dve', lambda e, i=i: e.tensor_tensor(out=ty[i][:], in0=ty[i][:], in1=xt[i][:], op=ALU.add),
                 reads=[f'cty{i}', f'cxt{i}'], writes=[f'cty{i}'])
            P.dma('sp', lambda e, t=t, i=i: e.dma_start(out=T['x3'][t * 128:(t + 1) * 128, :], in_=ty[i][:]), reads=[f'cty{i}'], writes=['x3'])
        P.barrier()


def build_C(NT, n_exp=16):
    nc = bass.Bass("TRN2", target_bir_lowering=False)
    def din(name, shape):
        return nc.dram_tensor(name, shape, F32, kind="ExternalInput").ap()
    dout = lambda name, shape: nc.dram_tensor(name, shape, F32, kind="ExternalOutput").ap()
    T = {}
    T['oT'] = din("oT", [1024, NT * 128])
    T['wo'] = din("wo", [1024, 1024])
    T['xin'] = din("xin", [NT * 128, 1024])
    cT = din("cT", [128, 8, 2])
    ada_w = din("ada_w", [1024, 6144])
    ada_bT = din("ada_bT", [128, 48])
    adab_rep = din("adab_rep", [128, 2, 1024])
    gmixT = din("gmixT", [128, 8])
    gffnT = din("gffnT", [128, 8])
    T['rw'] = din("rw", [128, 8, 16])
    T['rbias'] = din("rbias", [128, 16])
    T['w1'] = din("w1", [n_exp, 1024, 1024])
    T['w3'] = din("w3", [n_exp, 1024, 1024])
    T['w2'] = din("w2", [n_exp, 1024, 1024])
    T['fng'] = din("fng", [128, 1024])
    T['x3'] = dout("x3", [NT * 128, 1024])
    T['out'] = dout("out", [NT * 128, 1024])
    T['gdram'] = dout("gdram", [2, 128, 2, 1024])
    P = Prog(nc)
    P.psum_keys = PSUM_KEYS
    with ExitStack() as st:
        sb = _sb(nc, st)
        C = make_consts(P, nc, sb)
        M = mod_setup(P, nc, st, C, cT, ada_w, ada_bT, adab_rep, gmixT, gffnT, T['gdram'])
        wo_phase(P, nc, C, T, NT)
        T['x_in'] = T['x3']
        T['x_out'] = T['out']
        moe_phase(P, nc, C, M, T, NT, lambda t: 0, "m", final_norm=True)
        P.final_wait('sp')
        P.emit(st)
    print("C ops:", P.nops, {e: len(P.streams[e]) for e in P.ENG})
    return nc


def host_C(inp, o_own, xl2_own, b):
    m = {}
    m['oT'] = o_own.T
    m['wo'] = inp['rwkv_w_o'][0]
    m['xin'] = xl2_own
    m['fng'] = np.broadcast_to(inp['final_norm_g'][None], (128, 1024))
    m.update(host_mod(inp, b, 1))
    m.update(host_moe(inp, 1))
    return {k_: np.ascontiguousarray(v, dtype=np.float32) for k_, v in m.items()}


def kernel(**inp):
    inp = {k: np.asarray(v) for k, v in inp.items()}
    S = 16384
    cfg = Cfg(S)
    OWN = cfg.OWN
    cores = list(range(8))
    ncA = build_A(cfg)
    mapsA = [host_A(inp, cfg, c) for c in cores]
    resA = run_bass_kernel_spmd(ncA, mapsA, core_ids=cores).results
    x2 = [np.asarray(resA[c]['x2']) for c in cores]
    del mapsA, resA
    xs = [np.concatenate([x2[b * 4][:256]] + [x2[b * 4 + q][256:] for q in range(4)], 0) for b in range(2)]
    ncB = build_B(S // 512)
    mapsB = [host_B(inp, xs[c // 4], c) for c in cores]
    resB = run_bass_kernel_spmd(ncB, mapsB, core_ids=cores).results
    o = [np.concatenate([np.asarray(resB[b * 4 + hg]['og']) for hg in range(4)], 1) for b in range(2)]
    del mapsB, resB
    ncC = build_C(OWN // 128)
    mapsC = [host_C(inp, o[c // 4][(c % 4) * OWN:(c % 4 + 1) * OWN], x2[c][256:], c // 4) for c in cores]
    resC = run_bass_kernel_spmd(ncC, mapsC, core_ids=cores).results
    out = np.stack([np.concatenate([np.asarray(resC[b * 4 + q]['out']) for q in range(4)], 0) for b in range(2)])
    return np.ascontiguousarray(out, dtype=np.float32)
```
